# Optimizing a Trainium2 kernel written in Bass

```python
import jax, jax.numpy as jnp
from jax import lax
import numpy as np

D_MODEL = 2048
BATCH = 8
SEQ = 2048
DEPTH = 4

GRID_W = 64
CTX_LEN = 256
N_MIXERS = 2

ATT_HEADS = 32
ATT_KV_HEADS = 4
ATT_HEAD_DIM = 64
ATT_WINDOW = 128
ATT_BLOCK = 128
ATT_Q_DIM = ATT_HEADS * ATT_HEAD_DIM
ATT_KV_DIM = ATT_KV_HEADS * ATT_HEAD_DIM
ATT_QKV_DIM = ATT_Q_DIM + 2 * ATT_KV_DIM
ROPE_AXIS_DIM = ATT_HEAD_DIM // 2
ROPE_BASE = 10000.0

DN_QK_HEADS = 16
DN_V_HEADS = 32
DN_HEAD_DIM = 128
DN_CONV_W = 5
DN_CHUNK = 64
DN_K_DIM = DN_QK_HEADS * DN_HEAD_DIM
DN_V_DIM = DN_V_HEADS * DN_HEAD_DIM
DN_CONV_DIM = 2 * DN_K_DIM + DN_V_DIM
DN_IN_DIM = DN_CONV_DIM + DN_V_DIM + 4 * DN_V_HEADS

N_EXPERTS = 32
TOP_K = 4
D_EXPERT = 768
SWIGLU_LIMIT = 7.0
SWIGLU_ALPHA = 1.702
MOE_BLOCK = 128

LN_EPS = 1e-5
RMS_EPS = 1e-6
L2_EPS = 1e-6
DEEPNORM_ALPHA = (2.0 * DEPTH) ** 0.25
DEEPNORM_BETA = (8.0 * DEPTH) ** -0.25
N_ATT_LAYERS = (DEPTH + 1) // 2
N_DN_LAYERS = DEPTH // 2

kernel_name = "hybrid_swa_deltanet_moe_dit"


def layer_norm(t, g, b):
    tf = t.astype(jnp.float32)
    mu = jnp.mean(tf, axis=-1, keepdims=True)
    var = jnp.mean(jnp.square(tf - mu), axis=-1, keepdims=True)
    return ((tf - mu) * lax.rsqrt(var + LN_EPS) * g + b).astype(t.dtype)


def axial_rope_tables(rows):
    r, col = jnp.meshgrid(jnp.arange(rows), jnp.arange(GRID_W), indexing="ij")
    pos = jnp.stack([r.reshape(-1), col.reshape(-1)], axis=-1).astype(jnp.float32)
    inv_freq = ROPE_BASE ** (-jnp.arange(0, ROPE_AXIS_DIM, 2, dtype=jnp.float32) / ROPE_AXIS_DIM)
    ang = pos[:, :, None] * inv_freq
    return jnp.cos(ang), jnp.sin(ang)


def apply_axial_rope(t, cos, sin):
    B, n, H, hd = t.shape
    tf = t.astype(jnp.float32).reshape(B, n, H, 2, 2, hd // 4)
    t1, t2 = tf[..., 0, :], tf[..., 1, :]
    cs, sn = cos[None, :, None], sin[None, :, None]
    out = jnp.stack([t1 * cs - t2 * sn, t2 * cs + t1 * sn], axis=-2)
    return out.reshape(B, n, H, hd).astype(t.dtype)


def softmax_with_sink(scores, sink):
    sink_col = jnp.broadcast_to(sink.astype(jnp.float32)[None, :, :, None, None], scores.shape[:-1] + (1,))
    p = jax.nn.softmax(jnp.concatenate([scores, sink_col], axis=-1), axis=-1)
    return p[..., :-1]


def windowed_gqa_sink(u_lat, u_ctx, rope_cos, rope_sin, w_qkv, b_qkv, sink, w_o, b_o, need_ctx_out):
    B, S, _ = u_lat.shape
    G = ATT_HEADS // ATT_KV_HEADS
    scale = ATT_HEAD_DIM ** -0.5
    sink = sink.reshape(ATT_KV_HEADS, G)

    def project(u, rotary):
        n = u.shape[1]
        q, k, v = jnp.split(u @ w_qkv + b_qkv, [ATT_Q_DIM, ATT_Q_DIM + ATT_KV_DIM], axis=-1)
        q = q.reshape(B, n, ATT_HEADS, ATT_HEAD_DIM)
        k = k.reshape(B, n, ATT_KV_HEADS, ATT_HEAD_DIM)
        v = v.reshape(B, n, ATT_KV_HEADS, ATT_HEAD_DIM)
        if rotary:
            q = apply_axial_rope(q, rope_cos, rope_sin)
            k = apply_axial_rope(k, rope_cos, rope_sin)
        return q.reshape(B, n, ATT_KV_HEADS, G, ATT_HEAD_DIM), k, v

    def out_proj(o):
        return o.reshape(o.shape[0], o.shape[1], ATT_Q_DIM) @ w_o + b_o

    q_l, k_l, v_l = project(u_lat, True)
    q_c, k_c, v_c = project(u_ctx, False)

    n_blocks = S // ATT_BLOCK
    span = ATT_BLOCK + 2 * ATT_WINDOW
    pad = ((0, 0), (ATT_WINDOW, ATT_WINDOW), (0, 0), (0, 0))
    k_pad, v_pad = jnp.pad(k_l, pad), jnp.pad(v_l, pad)
    q_blocks = jnp.moveaxis(q_l.reshape(B, n_blocks, ATT_BLOCK, ATT_KV_HEADS, G, ATT_HEAD_DIM), 1, 0)

    def latent_block(args):
        blk, q_b = args
        start = blk * ATT_BLOCK
        k_b = lax.dynamic_slice_in_dim(k_pad, start, span, axis=1)
        v_b = lax.dynamic_slice_in_dim(v_pad, start, span, axis=1)
        q_pos = start + jnp.arange(ATT_BLOCK)
        k_pos = start - ATT_WINDOW + jnp.arange(span)
        valid = (jnp.abs(q_pos[:, None] - k_pos[None, :]) <= ATT_WINDOW) & (k_pos >= 0) & (k_pos < S)
        s_loc = jnp.einsum("bqkgd,bskd->bkgqs", q_b, k_b).astype(jnp.float32) * scale
        s_loc = jnp.where(valid, s_loc, -jnp.inf)
        s_ctx = jnp.einsum("bqkgd,bckd->bkgqc", q_b, k_c).astype(jnp.float32) * scale
        p = softmax_with_sink(jnp.concatenate([s_loc, s_ctx], axis=-1), sink).astype(v_b.dtype)
        return (jnp.einsum("bkgqs,bskd->bqkgd", p[..., :span], v_b)
                + jnp.einsum("bkgqc,bckd->bqkgd", p[..., span:], v_c))

    o_lat = lax.map(latent_block, (jnp.arange(n_blocks), q_blocks))
    o_lat = jnp.moveaxis(o_lat, 0, 1).reshape(B, S, ATT_KV_HEADS, G, ATT_HEAD_DIM)
    y_lat = out_proj(o_lat)
    if not need_ctx_out:
        return y_lat, None
    s_cc = jnp.einsum("bqkgd,bckd->bkgqc", q_c, k_c).astype(jnp.float32) * scale
    p_cc = softmax_with_sink(s_cc, sink).astype(v_c.dtype)
    y_ctx = out_proj(jnp.einsum("bkgqc,bckd->bqkgd", p_cc, v_c))
    return y_lat, y_ctx


def centred_depthwise_conv_silu(t, w):
    half = DN_CONV_W // 2
    y = lax.conv_general_dilated(t, w[:, None, :].astype(t.dtype), window_strides=(1,),
                                 padding=[(half, half)], dimension_numbers=("NWC", "WIO", "NWC"),
                                 feature_group_count=t.shape[-1])
    return jax.nn.silu(y)


def l2_normalise(t):
    tf = t.astype(jnp.float32)
    return tf * lax.rsqrt(jnp.sum(tf * tf, axis=-1, keepdims=True) + L2_EPS)


def chunk_gated_delta(q, k, v, g, beta, state0, with_output):
    B, n, H, dk = q.shape
    dv = v.shape[-1]
    nc = n // DN_CHUNK

    def to_chunks(t):
        t = t.astype(jnp.float32).reshape((B, nc, DN_CHUNK, H, -1))
        return t.transpose(1, 0, 3, 2, 4)

    qc, kc, vc = to_chunks(q), to_chunks(k), to_chunks(v)
    gc = jnp.cumsum(to_chunks(g[..., None])[..., 0], axis=-1)
    bc = to_chunks(beta[..., None])[..., 0]
    idx = jnp.arange(DN_CHUNK)
    incl = idx[:, None] >= idx[None, :]
    decay = jnp.exp(jnp.where(incl, gc[..., :, None] - gc[..., None, :], -jnp.inf))
    m = jnp.where(idx[:, None] > idx[None, :],
                  bc[..., :, None] * jnp.einsum("zbhid,zbhjd->zbhij", kc, kc) * decay, 0.0)
    rhs = jnp.concatenate([vc * bc[..., None], kc * (bc * jnp.exp(gc))[..., None]], axis=-1)
    sol = lax.linalg.triangular_solve(m + jnp.eye(DN_CHUNK, dtype=jnp.float32), rhs,
                                      left_side=True, lower=True, unit_diagonal=True)
    u, w = sol[..., :dv], sol[..., dv:]
    qk = jnp.einsum("zbhid,zbhjd->zbhij", qc, kc) * decay if with_output else None

    def step(S, xs):
        q_i, k_i, u_i, w_i, g_i, qk_i = xs
        v_new = u_i - jnp.einsum("bhcd,bhde->bhce", w_i, S)
        g_last = g_i[..., -1]
        S_next = (S * jnp.exp(g_last)[..., None, None]
                  + jnp.einsum("bhcd,bhce->bhde", k_i * jnp.exp(g_last[..., None] - g_i)[..., None], v_new))
        if not with_output:
            return S_next, None
        o = (jnp.einsum("bhcd,bhde->bhce", q_i * jnp.exp(g_i)[..., None], S)
             + jnp.einsum("bhij,bhje->bhie", qk_i, v_new))
        return S_next, o

    S_final, o = lax.scan(step, state0, (qc, kc, u, w, gc, qk))
    if with_output:
        o = o.transpose(1, 0, 3, 2, 4).reshape(B, n, H, dv)
    return o, S_final


def gated_deltanet(u_lat, u_ctx, w_in, conv_w, a_log, dt_bias, norm_w, w_o, need_ctx_out):
    B = u_lat.shape[0]
    rep = DN_V_HEADS // DN_QK_HEADS

    def project(u):
        n = u.shape[1]
        qkv, z, ab = jnp.split(u @ w_in, [DN_CONV_DIM, DN_CONV_DIM + DN_V_DIM], axis=-1)
        qkv = centred_depthwise_conv_silu(qkv, conv_w)
        q, k, v = jnp.split(qkv, [DN_K_DIM, 2 * DN_K_DIM], axis=-1)
        q = l2_normalise(q.reshape(B, n, DN_QK_HEADS, DN_HEAD_DIM)) * DN_HEAD_DIM ** -0.5
        k = l2_normalise(k.reshape(B, n, DN_QK_HEADS, DN_HEAD_DIM))
        q, k = jnp.repeat(q, rep, axis=2), jnp.repeat(k, rep, axis=2)
        v = v.reshape(B, n, DN_V_HEADS, DN_HEAD_DIM)
        ab = ab.astype(jnp.float32).reshape(B, n, 2, 2, DN_V_HEADS)
        g = -jnp.exp(a_log.astype(jnp.float32)) * jax.nn.softplus(ab[:, :, :, 0] + dt_bias.astype(jnp.float32))
        beta = jax.nn.sigmoid(ab[:, :, :, 1])
        return q, k, v, z, g, beta

    def gated_out(o, z):
        bsz, n = o.shape[:2]
        o = o * lax.rsqrt(jnp.mean(o * o, axis=-1, keepdims=True) + RMS_EPS) * norm_w.astype(jnp.float32)
        o = o * jax.nn.silu(z.astype(jnp.float32).reshape(bsz, n, DN_V_HEADS, DN_HEAD_DIM))
        return o.reshape(bsz, n, DN_V_DIM).astype(z.dtype) @ w_o

    qc, kc, vc, zc, gc, bc = project(u_ctx)
    ql, kl, vl, zl, gl, bl = project(u_lat)
    state0 = jnp.zeros((B, DN_V_HEADS, DN_HEAD_DIM, DN_HEAD_DIM), jnp.float32)
    o_lat, o_ctx = [], []
    for d in range(2):
        flip = (lambda t: jnp.flip(t, axis=1)) if d == 1 else (lambda t: t)
        oc, s_ctx = chunk_gated_delta(flip(qc), flip(kc), flip(vc), flip(gc[:, :, d]), flip(bc[:, :, d]),
                                      state0, need_ctx_out)
        ol, _ = chunk_gated_delta(flip(ql), flip(kl), flip(vl), flip(gl[:, :, d]), flip(bl[:, :, d]),
                                  s_ctx, True)
        o_lat.append(flip(ol))
        if need_ctx_out:
            o_ctx.append(flip(oc))
    y_lat = gated_out(o_lat[0] + o_lat[1], zl)
    if not need_ctx_out:
        return y_lat, None
    return y_lat, gated_out(o_ctx[0] + o_ctx[1], zc)


def moe_ffn(h, w_router, b_router, w_gu, b_gu, w_down, b_down):
    T, D = h.shape
    A = T * TOP_K
    n_blocks = -(-A // MOE_BLOCK) + N_EXPERTS
    logits = (h @ w_router + b_router).astype(jnp.float32)
    top_logit, top_idx = lax.top_k(logits, TOP_K)
    top_w = jax.nn.softmax(top_logit, axis=-1)
    flat_e = top_idx.reshape(A)
    order = jnp.argsort(flat_e)
    e_sorted = flat_e[order]
    tok_sorted = order // TOP_K
    w_sorted = top_w.reshape(A)[order]
    counts = jnp.zeros((N_EXPERTS,), jnp.int32).at[flat_e].add(1)
    padded = (counts + MOE_BLOCK - 1) // MOE_BLOCK * MOE_BLOCK
    pad_end = jnp.cumsum(padded)
    pad_start = pad_end - padded
    start = jnp.cumsum(counts) - counts
    dest = pad_start[e_sorted] + jnp.arange(A, dtype=jnp.int32) - start[e_sorted]
    row_tok = jnp.full((n_blocks * MOE_BLOCK,), T, jnp.int32).at[dest].set(tok_sorted)
    block_e = jnp.minimum(jnp.searchsorted(pad_end, jnp.arange(n_blocks) * MOE_BLOCK, side="right"),
                          N_EXPERTS - 1)
    h_pad = jnp.concatenate([h, jnp.zeros((1, D), h.dtype)], axis=0)
    xb = h_pad[row_tok].reshape(n_blocks, MOE_BLOCK, D)

    def expert_block(args):
        xe, e = args
        gu = xe @ w_gu[e] + b_gu[e]
        gate = jnp.minimum(gu[..., ::2], SWIGLU_LIMIT)
        up = jnp.clip(gu[..., 1::2], -SWIGLU_LIMIT, SWIGLU_LIMIT)
        act = (up + 1.0) * gate * jax.nn.sigmoid(SWIGLU_ALPHA * gate)
        return act @ w_down[e] + b_down[e]

    yb = lax.map(expert_block, (xb, block_e)).reshape(n_blocks * MOE_BLOCK, D)
    return jnp.zeros_like(h).at[tok_sorted].add(w_sorted[:, None].astype(h.dtype) * yb[dest])


def setup_inputs(seed: int = 0) -> dict:
    key = jax.random.key(seed)
    ks = jax.random.split(key, 25)
    D = D_MODEL

    def nrm(k, shape, s):
        return jax.random.normal(k, shape, jnp.float32) * s

    dt = jnp.exp(jax.random.uniform(ks[16], (N_DN_LAYERS, 2, DN_V_HEADS), jnp.float32,
                                    float(np.log(1e-3)), float(np.log(1e-1))))
    return {
        "x": nrm(ks[0], (BATCH, SEQ, D), 1.0),
        "c": nrm(ks[1], (BATCH, D), 1.0),
        "ctx": nrm(ks[2], (BATCH, CTX_LEN, D), 1.0),
        "c_ctx": nrm(ks[3], (D,), 1.0),
        "w_mod": nrm(ks[4], (DEPTH, D, 6 * D), 0.5 * D ** -0.5),
        "b_mod": nrm(ks[5], (DEPTH, 6 * D), 0.02),
        "ln_g": 1.0 + nrm(ks[6], (DEPTH, 2, D), 0.02),
        "ln_b": nrm(ks[7], (DEPTH, 2, D), 0.02),
        "att_w_qkv": nrm(ks[8], (N_ATT_LAYERS, D, ATT_QKV_DIM), D ** -0.5),
        "att_b_qkv": nrm(ks[9], (N_ATT_LAYERS, ATT_QKV_DIM), 0.02),
        "att_sink": nrm(ks[10], (N_ATT_LAYERS, ATT_HEADS), 0.5),
        "att_w_o": nrm(ks[11], (N_ATT_LAYERS, ATT_Q_DIM, D), DEEPNORM_BETA * ATT_Q_DIM ** -0.5),
        "att_b_o": nrm(ks[12], (N_ATT_LAYERS, D), 0.02),
        "dn_w_in": nrm(ks[13], (N_DN_LAYERS, D, DN_IN_DIM), D ** -0.5),
        "dn_conv_w": nrm(ks[14], (N_DN_LAYERS, DN_CONV_W, DN_CONV_DIM), DN_CONV_W ** -0.5),
        "dn_a_log": jnp.log(jax.random.uniform(ks[15], (N_DN_LAYERS, 2, DN_V_HEADS), jnp.float32, 1.0, 16.0)),
        "dn_dt_bias": dt + jnp.log(-jnp.expm1(-dt)),
        "dn_norm_w": 1.0 + nrm(ks[17], (N_DN_LAYERS, DN_HEAD_DIM), 0.02),
        "dn_w_o": nrm(ks[18], (N_DN_LAYERS, DN_V_DIM, D), DEEPNORM_BETA * DN_V_DIM ** -0.5),
        "moe_w_router": nrm(ks[19], (DEPTH, D, N_EXPERTS), D ** -0.5),
        "moe_b_router": nrm(ks[20], (DEPTH, N_EXPERTS), 0.01),
        "moe_w_gu": nrm(ks[21], (DEPTH, N_EXPERTS, D, 2 * D_EXPERT), D ** -0.5),
        "moe_b_gu": nrm(ks[22], (DEPTH, N_EXPERTS, 2 * D_EXPERT), 0.02),
        "moe_w_down": nrm(ks[23], (DEPTH, N_EXPERTS, D_EXPERT, D), DEEPNORM_BETA * D_EXPERT ** -0.5),
        "moe_b_down": nrm(ks[24], (DEPTH, N_EXPERTS, D), 0.02),
    }


def reference(x, c, ctx, c_ctx, w_mod, b_mod, ln_g, ln_b, att_w_qkv, att_b_qkv, att_sink, att_w_o, att_b_o,
              dn_w_in, dn_conv_w, dn_a_log, dn_dt_bias, dn_norm_w, dn_w_o, moe_w_router, moe_b_router,
              moe_w_gu, moe_b_gu, moe_w_down, moe_b_down):
    B, S, D = x.shape
    L = ctx.shape[1]
    rows = S // GRID_W
    rope_cos, rope_sin = axial_rope_tables(rows)
    h_lat, h_ctx = x, ctx
    for i in range(DEPTH):
        last = i == DEPTH - 1
        j = i // N_MIXERS
        m_lat = jnp.split((jax.nn.silu(c) @ w_mod[i] + b_mod[i])[:, None, :], 6, axis=-1)
        m_ctx = jnp.split(jax.nn.silu(c_ctx) @ w_mod[i] + b_mod[i], 6, axis=-1)
        u_lat = h_lat * (1.0 + m_lat[1]) + m_lat[0]
        u_ctx = h_ctx * (1.0 + m_ctx[1]) + m_ctx[0]
        if i % N_MIXERS == 0:
            y_lat, y_ctx = windowed_gqa_sink(u_lat, u_ctx, rope_cos, rope_sin, att_w_qkv[j], att_b_qkv[j],
                                             att_sink[j], att_w_o[j], att_b_o[j], not last)
        else:
            y_lat, y_ctx = gated_deltanet(u_lat, u_ctx, dn_w_in[j], dn_conv_w[j], dn_a_log[j], dn_dt_bias[j],
                                          dn_norm_w[j], dn_w_o[j], not last)
        h_lat = layer_norm(DEEPNORM_ALPHA * h_lat + m_lat[2] * y_lat, ln_g[i, 0], ln_b[i, 0])
        u_lat = h_lat * (1.0 + m_lat[4]) + m_lat[3]
        moe_args = (moe_w_router[i], moe_b_router[i], moe_w_gu[i], moe_b_gu[i], moe_w_down[i], moe_b_down[i])
        if last:
            y_lat = moe_ffn(u_lat.reshape(B * S, D), *moe_args).reshape(B, S, D)
        else:
            h_ctx = layer_norm(DEEPNORM_ALPHA * h_ctx + m_ctx[2] * y_ctx, ln_g[i, 0], ln_b[i, 0])
            u_ctx = h_ctx * (1.0 + m_ctx[4]) + m_ctx[3]
            y = moe_ffn(jnp.concatenate([u_lat.reshape(B * S, D), u_ctx.reshape(B * L, D)], axis=0), *moe_args)
            y_lat, y_ctx = y[:B * S].reshape(B, S, D), y[B * S:].reshape(B, L, D)
            h_ctx = layer_norm(DEEPNORM_ALPHA * h_ctx + m_ctx[5] * y_ctx, ln_g[i, 1], ln_b[i, 1])
        h_lat = layer_norm(DEEPNORM_ALPHA * h_lat + m_lat[5] * y_lat, ln_g[i, 1], ln_b[i, 1])
    return h_lat
```

```python
import contextlib
import numpy as np
import concourse.bass as bass
import concourse.mybir as mybir
from concourse.bass_utils import run_bass_kernel_spmd

F32 = mybir.dt.float32
BF16 = mybir.dt.bfloat16
ALU = mybir.AluOpType
AF = mybir.ActivationFunctionType
AX = mybir.AxisListType

D = 2048
NCH = 16
CTX = 256
SEQ = 2048
T = CTX + SEQ
NT = T // 128
DEPTH = 4
ALPHA = (2.0 * DEPTH) ** 0.25
LN_EPS = 1e-5
GROUPS = [(0, 256), (256, 512), (768, 512), (1280, 512), (1792, 512)]
NE = 32
DEXP = 768

EPOCH = 30000
N_DMA_SEMS = 12


class Buf:
    __slots__ = ("name", "lw", "rd", "excl")

    def __init__(self, name="", excl=False):
        self.name = name
        self.lw = None
        self.rd = {}
        self.excl = excl


class Sched:
    def __init__(self, nc):
        self.nc = nc
        self.ops = {"pe": [], "act": [], "dve": [], "pool": [], "sp": []}
        self.cnt = {e: 0 for e in self.ops}
        self.epoch = {e: 0 for e in self.ops}
        self.seen = {e: {} for e in self.ops}
        self.semkeys = set()
        self.dma_tot = {}
        self.dma_rr = {"sp": 0, "pool": 0, "act": 0}
        self.n_ops = 0

    def _deps(self, eng, reads, writes):
        deps = {}
        for b in reads:
            if b.lw is not None and deps.get(b.lw[0], 0) < b.lw[1]:
                deps[b.lw[0]] = b.lw[1]
            if b.excl:
                for k, v in b.rd.items():
                    if k[1] != eng and deps.get(k, 0) < v:
                        deps[k] = v
        for b in writes:
            if b.lw is not None and deps.get(b.lw[0], 0) < b.lw[1]:
                deps[b.lw[0]] = b.lw[1]
            for k, v in b.rd.items():
                if deps.get(k, 0) < v:
                    deps[k] = v
        out = []
        seen = self.seen[eng]
        for k, v in deps.items():
            if eng == "pe" and k[0] == "e" and k[1] == "pe":
                continue
            if seen.get(k, 0) >= v:
                continue
            seen[k] = v
            out.append((k, v))
        return out

    def op(self, eng, fn, reads=(), writes=()):
        if self.cnt[eng] >= EPOCH:
            self.epoch[eng] += 1
            self.cnt[eng] = 0
        waits = self._deps(eng, reads, writes)
        key = ("e", eng, self.epoch[eng])
        self.semkeys.add(key)
        self.cnt[eng] += 1
        val = self.cnt[eng]
        self.ops[eng].append((waits, fn, key, 1))
        for b in writes:
            b.lw = (key, val)
            b.rd = {}
        for b in reads:
            if b.rd.get(key, 0) < val:
                b.rd[key] = val
        self.n_ops += 1

    def dma(self, q, out_ap, in_ap, reads=(), writes=()):
        i = self.dma_rr[q]
        self.dma_rr[q] = (i + 1) % N_DMA_SEMS
        key = ("d", q, i)
        self.semkeys.add(key)
        prev = self.dma_tot.get(key, 0)
        waits = self._deps(q, reads, writes)
        if prev > 0 and self.seen[q].get(key, 0) < prev:
            self.seen[q][key] = prev
            waits.append((key, prev))
        tot = prev + 16
        self.dma_tot[key] = tot

        def fn(e, out_ap=out_ap, in_ap=in_ap):
            return e.dma_start(out=out_ap, in_=in_ap)
        self.ops[q].append((waits, fn, key, 16))
        for b in writes:
            b.lw = (key, tot)
            b.rd = {}
        for b in reads:
            if b.rd.get(key, 0) < tot:
                b.rd[key] = tot
        self.n_ops += 1

    def fence(self):
        allk = []
        for x in self.ops:
            if self.cnt[x] > 0:
                allk.append((("e", x, self.epoch[x]), self.cnt[x]))
        for k, v in self.dma_tot.items():
            allk.append((k, v))
        for e in self.ops:
            waits = []
            for k, v in allk:
                if self.seen[e].get(k, 0) < v:
                    self.seen[e][k] = v
                    waits.append((k, v))
            if waits:
                self.ops[e].append((waits, None, None, 0))

    def emit(self):
        nc = self.nc
        with contextlib.ExitStack() as st:
            sems = {}
            for k in sorted(self.semkeys):
                sems[k] = st.enter_context(nc.semaphore("s_" + "_".join(str(x) for x in k)))
            block = st.enter_context(nc.Block())

            def run(eng_name):
                def body(e):
                    for waits, fn, key, inc in self.ops[eng_name]:
                        for (k, v) in waits:
                            e.wait_ge(sems[k], v)
                        if fn is not None:
                            fn(e).then_inc(sems[key], inc)
                return body
            block.sync(run("sp"))
            block.tensor(run("pe"))
            block.scalar(run("act"))
            block.vector(run("dve"))
            block.gpsimd(run("pool"))


class Rot:
    def __init__(self, items):
        self.items = items
        self.i = 0

    def next(self):
        it = self.items[self.i]
        self.i = (self.i + 1) % len(self.items)
        return it


class K:
    def __init__(self, cfg):
        self.cfg = cfg
        self.nc = bass.Bass("TRN2", target_bir_lowering=False)
        self.S = Sched(self.nc)
        self.uid = 0
        self.base = 16640
        self.off = 16640
        self.LIMIT = 229000

    def sb(self, shape, dtype, persistent=False):
        self.uid += 1
        esz = 2 if dtype == BF16 else 4
        n = 1
        for s in shape[1:]:
            n *= s
        nbytes = (n * esz + 63) // 64 * 64
        t = self.nc.alloc_sbuf_tensor_at("sb%d" % self.uid, list(shape), dtype, offset=self.off)
        self.off += nbytes
        assert self.off <= self.LIMIT, ("SBUF overflow", self.off)
        if persistent:
            self.base = self.off
        return t

    def sbb(self, shape, dtype):
        return self.sb(shape, dtype), Buf()

    def rot(self, n, shape, dtype):
        return Rot([self.sbb(shape, dtype) for _ in range(n)])

    def stage(self):
        self.S.fence()
        self.off = self.base

    def dram(self, name, shape, dtype):
        return self.nc.dram_tensor(name, list(shape), dtype).ap()

    def ext_in(self, name, shape, dtype=F32):
        return self.nc.dram_tensor(name, list(shape), dtype, kind="ExternalInput").ap()

    def ext_out(self, name, shape, dtype=F32):
        return self.nc.dram_tensor(name, list(shape), dtype, kind="ExternalOutput").ap()

    def mm(self, out, lhsT, rhs, start, stop, reads, writes):
        self.S.op("pe", lambda e: e.matmul(out, lhsT, rhs, start=start, stop=stop), reads, writes)

    def tr(self, out, in_, reads, writes):
        ident = self.ident
        n = in_.shape[0]
        self.S.op("pe", lambda e: e.transpose(out, in_, ident[0:n, 0:n]), list(reads) + [self.B_const], writes)

    def act(self, out, in_, func, reads, writes, bias=0.0, scale=1.0):
        self.S.op("act", lambda e: e.activation(out=out, in_=in_, func=func, bias=bias, scale=scale), reads, writes)

    def ts(self, eng, out, in0, s1, s2, op0, op1, reads, writes):
        if op1 is None:
            self.S.op(eng, lambda e: e.tensor_scalar(out=out, in0=in0, scalar1=s1, scalar2=None, op0=op0), reads, writes)
        else:
            self.S.op(eng, lambda e: e.tensor_scalar(out=out, in0=in0, scalar1=s1, scalar2=s2, op0=op0, op1=op1), reads, writes)

    def tt(self, eng, out, in0, in1, op, reads, writes):
        self.S.op(eng, lambda e: e.tensor_tensor(out=out, in0=in0, in1=in1, op=op), reads, writes)

    def stt(self, eng, out, in0, scalar, in1, op0, op1, reads, writes):
        self.S.op(eng, lambda e: e.scalar_tensor_tensor(out=out, in0=in0, scalar=scalar, in1=in1, op0=op0, op1=op1), reads, writes)

    def cp(self, eng, out, in_, reads, writes):
        if eng == "act":
            self.S.op("act", lambda e: e.copy(out=out, in_=in_), reads, writes)
        else:
            self.S.op(eng, lambda e: e.tensor_copy(out=out, in_=in_), reads, writes)

    def setup(self):
        nc, S = self.nc, self.S
        self.ps = [nc.alloc_psum_tensor("ps%d" % i, [128, 512], F32) for i in range(8)]
        self.psB = [Buf("ps%d" % i, excl=True) for i in range(8)]
        self.B_const = Buf("const")
        self.ident = self.sb([128, 128], F32, True)
        self.ones = self.sb([128, 128], F32, True)
        ident, ones = self.ident, self.ones
        self.onesrow = self.sb([1, 512], F32, True)
        onesrow = self.onesrow
        S.op("pool", lambda e: e.memset(onesrow[:], 1.0), (), [self.B_const])
        S.op("pool", lambda e: e.memset(ones[:], 1.0), (), [self.B_const])
        S.op("pool", lambda e: e.memset(ident[:], 1.0), (), [self.B_const])
        S.op("pool", lambda e: e.affine_select(out=ident[:], in_=ident[:], pattern=[[-1, 128]],
                                               compare_op=ALU.is_equal, fill=0.0, base=0, channel_multiplier=1),
             [self.B_const], [self.B_const])
        self.m_le = self.sb([128, 128], F32, True)
        self.m_ge = self.sb([128, 128], F32, True)
        self.m_lt = self.sb([128, 128], F32, True)
        self.m_gt = self.sb([128, 128], F32, True)
        for m, step, cm, base in ((self.m_le, 1, -1, 0), (self.m_ge, -1, 1, 0), (self.m_lt, 1, -1, -1), (self.m_gt, -1, 1, -1)):
            S.op("pool", lambda e, m=m: e.memset(m[:], 1.0), (), [self.B_const])
            S.op("pool", lambda e, m=m, step=step, cm=cm, base=base: e.affine_select(
                out=m[:], in_=m[:], pattern=[[step, 128]], compare_op=ALU.is_ge, fill=0.0, base=base, channel_multiplier=cm),
                 [self.B_const], [self.B_const])
        self.MT = self.sb([128, DEPTH, 96, 2], F32, True)
        self.B_MT = Buf("MT")
        self.lnG = self.sb([128, 128], F32, True)
        self.lnB = self.sb([128, 128], F32, True)
        self.B_ln = Buf("ln")

    def prologue_mod(self, c_in, cctx_in, w_mod, b_mod, ln_g, ln_b):
        S = self.S
        self.stage()
        P = self.ps
        cs, B_cs = self.sbb([32, 128], F32)
        csT, B_csT = self.sbb([128, 32], F32)
        bm, B_bm = self.sbb([128, 3, 128], F32)
        bmT, B_bmT = self.sbb([128, 384], F32)
        lt, B_lt = self.sbb([128, 2, 128], F32)
        S.dma("sp", cs[0:16, :], c_in, (), [B_cs])
        S.dma("sp", cs[16:32, :], cctx_in, (), [B_cs])
        self.act(cs[:], cs[:], AF.Silu, [B_cs], [B_cs])
        self.tr(P[0][:, 0:32], cs[:], [B_cs], [self.psB[0]])
        self.cp("dve", csT[:], P[0][:, 0:32], [self.psB[0]], [B_csT])
        bview = b_mod.rearrange("l (f p) -> (l f) p", p=128)
        for i in range(3):
            S.dma("sp", bm[:, i, :], bview[i * 128:(i + 1) * 128, :], (), [B_bm])
        for i in range(3):
            self.tr(P[1][:, i * 128:(i + 1) * 128], bm[:, i, :], [B_bm], [self.psB[1]])
        self.cp("dve", bmT[:], P[1][:, 0:384], [self.psB[1]], [B_bmT])
        S.dma("sp", lt[:, 0, :], ln_g.rearrange("l s (k p) -> (l s k) p", p=128), (), [B_lt])
        S.dma("sp", lt[:, 1, :], ln_b.rearrange("l s (k p) -> (l s k) p", p=128), (), [B_lt])
        self.tr(P[2][:, 0:128], lt[:, 0, :], [B_lt], [self.psB[2]])
        self.tr(P[2][:, 128:256], lt[:, 1, :], [B_lt], [self.psB[2]])
        self.cp("dve", self.lnG[:], P[2][:, 0:128], [self.psB[2]], [self.B_ln])
        self.cp("dve", self.lnB[:], P[2][:, 128:256], [self.psB[2]], [self.B_ln])
        wrot = self.rot(3, [128, 16, 512], F32)
        csv = csT[:].rearrange("p (j k) -> p j k", j=2)
        pi = 0
        rowrot = self.rot(2, [2, 512], F32)
        for l in range(DEPTH):
            wv = w_mod[l].rearrange("(k p) n -> p k n", p=128)
            for nb in range(24):
                w, B_w = wrot.next()
                S.dma("sp", w[:], wv[:, :, nb * 512:(nb + 1) * 512], (), [B_w])
                pr_, B_pr = P[3 + pi], self.psB[3 + pi]
                pt, B_pt = P[5 + pi], self.psB[5 + pi]
                pi = (pi + 1) % 2
                for k in range(16):
                    self.mm(pr_[0:2, :], csv[:, :, k], w[:, k, :], k == 0, k == 15, [B_w, B_csT], [B_pr])
                row, B_row = rowrot.next()
                self.cp("act", row[:], pr_[0:2, :], [B_pr], [B_row])
                for f in range(4):
                    self.tr(pt[:, f * 2:f * 2 + 2], row[0:2, f * 128:(f + 1) * 128], [B_row], [B_pt])
                a = l * 96 + nb * 4
                self.tt("dve", self.MT[:, l, nb * 4:nb * 4 + 4, :], pt[:, 0:8].rearrange("p (f j) -> p f j", j=2),
                        bmT[:, a:a + 4].unsqueeze(2).broadcast_to([128, 4, 2]), ALU.add, [B_pt, B_bmT], [self.B_MT])
        for l in range(DEPTH):
            for sec in (1, 4):
                v = self.MT[:, l, sec * 16:(sec + 1) * 16, :]
                self.ts("dve", v, v, 1.0, None, ALU.add, None, [self.B_MT], [self.B_MT])
            for sec in (2, 5):
                v = self.MT[:, l, sec * 16:(sec + 1) * 16, :]
                self.ts("dve", v, v, 1.0 / ALPHA, None, ALU.mult, None, [self.B_MT], [self.B_MT])

    def prologue_x(self, x_in, ctx_in, HT, HTB):
        S = self.S
        self.stage()
        P = self.ps
        xrot = self.rot(2, [128, D], F32)
        srot = self.rot(2, [128, NCH, 128], F32)
        HTv = HT.rearrange("c p t -> p c t")
        pi = 0
        for tt in range(NT):
            xt, B_x = xrot.next()
            src = ctx_in[tt * 128:(tt + 1) * 128, :] if tt < 2 else x_in[(tt - 2) * 128:(tt - 1) * 128, :]
            S.dma("sp", xt[:], src, (), [B_x])
            st, B_st = srot.next()
            for cq in range(4):
                pt, B_pt = P[pi], self.psB[pi]
                pi = (pi + 1) % 8
                for j in range(4):
                    c = cq * 4 + j
                    self.tr(pt[:, j * 128:(j + 1) * 128], xt[:, c * 128:(c + 1) * 128], [B_x], [B_pt])
                self.cp("act" if cq % 2 else "dve", st[:, cq * 4:(cq + 1) * 4, :],
                        pt[:].rearrange("p (j t) -> p j t", j=4), [B_pt], [B_st])
            g = self.group_of(tt * 128)
            S.dma("sp", HTv[:, :, tt * 128:(tt + 1) * 128], st[:], [B_st], [HTB[g]])

    @staticmethod
    def group_of(t):
        for g, (s, n) in enumerate(GROUPS):
            if s <= t < s + n:
                return g
        raise ValueError

    def ln_mod(self, l, HT, HTB, YT, YTB, gate_sec, ln_idx, mod_sec, UT, UTB, router=None, l_mod=None):
        S = self.S
        self.stage()
        P, PB = self.ps, self.psB
        has_ln = YT is not None
        zrot = Rot([(self.sb([128, NCH, 512], F32), [Buf() for _ in range(NCH)]) for _ in range(2)])
        yrot = self.rot(4, [128, 512], F32)
        qrot = self.rot(2, [128, 512], F32)
        urot = self.rot(2, [128, NCH, 512], BF16)
        ufrot = self.rot(2, [128, 512], F32)
        mean, B_mean = self.sbb([128, 512], F32)
        msq, B_msq = self.sbb([128, 512], F32)
        rstd, B_rstd = self.sbb([128, 512], F32)
        HTv = HT.rearrange("c p t -> p c t")
        UTv = UT.rearrange("c p t -> p c t")
        if router is not None:
            w_r, b_r, GT, GTB = router
            wr, B_wr = self.sbb([128, NCH, NE], F32)
            br, B_br = self.sbb([1, NE], F32)
            S.dma("sp", wr[:], w_r.rearrange("(k p) e -> p k e", p=128), (), [B_wr])
            S.dma("sp", br[:], b_r, (), [B_br])
            lg, B_lg = self.sbb([128, 4, NE], F32)
            ex, B_ex = self.sbb([128, 4, NE], F32)
            mk, B_mk = self.sbb([128, 4, NE], F32)
            m8, B_m8 = self.sbb([128, 4, 8], F32)
            sm, B_sm = self.sbb([128, 4, 4], F32)
            gts, B_gts = self.sbb([NE, 512], F32)
        sh_sec, sc_sec = mod_sec
        lm = l if l_mod is None else l_mod
        eps = LN_EPS / (ALPHA * ALPHA)
        for g, (t0, n) in enumerate(GROUPS):
            j = 1 if g == 0 else 0
            z, BZ = zrot.next()
            u, B_u = urot.next()
            S.dma("sp", z[:, :, 0:n], HTv[:, :, t0:t0 + n], [HTB[g]], BZ)
            if has_ln:
                for c in range(NCH):
                    B_z = BZ[c]
                    y, B_y = yrot.next()
                    S.dma("sp", y[:, 0:n], YT[c][:, t0:t0 + n], [YTB[g]], [B_y])
                    self.stt("dve", z[:, c, 0:n], y[:, 0:n], self.MT[:, l, gate_sec * 16 + c, j:j + 1], z[:, c, 0:n],
                             ALU.mult, ALU.add, [B_y, B_z, self.B_MT], [B_z])
                    q, B_q = qrot.next()
                    self.act(q[:, 0:n], z[:, c, 0:n], AF.Square, [B_z], [B_q])
                    self.mm(P[0][:, 0:n], self.ones[:], z[:, c, 0:n], c == 0, c == NCH - 1, [B_z, self.B_const], [PB[0]])
                    self.mm(P[1][:, 0:n], self.ones[:], q[:, 0:n], c == 0, c == NCH - 1, [B_q, self.B_const], [PB[1]])
                self.ts("dve", mean[:, 0:n], P[0][:, 0:n], 1.0 / D, None, ALU.mult, None, [PB[0]], [B_mean])
                self.tt("pool", msq[:, 0:n], mean[:, 0:n], mean[:, 0:n], ALU.mult, [B_mean], [B_msq])
                self.stt("dve", rstd[:, 0:n], P[1][:, 0:n], 1.0 / D, msq[:, 0:n], ALU.mult, ALU.subtract, [PB[1], B_msq], [B_rstd])
                self.act(rstd[:, 0:n], rstd[:, 0:n], AF.Sqrt, [B_rstd], [B_rstd], bias=eps)
                S.op("dve", lambda e, n=n: e.reciprocal(out=rstd[:, 0:n], in_=rstd[:, 0:n]), [B_rstd], [B_rstd])
            for c in range(NCH):
                B_z = BZ[c]
                zc = z[:, c, 0:n]
                if has_ln:
                    self.tt("pool", zc, zc, mean[:, 0:n], ALU.subtract, [B_z, B_mean], [B_z])
                    self.tt("dve", zc, zc, rstd[:, 0:n], ALU.mult, [B_z, B_rstd], [B_z])
                    col = l * 32 + ln_idx * 16 + c
                    self.act(zc, zc, AF.Identity, [B_z, self.B_ln], [B_z], bias=self.lnB[:, col:col + 1], scale=self.lnG[:, col:col + 1])
                sc = self.MT[:, lm, sc_sec * 16 + c, j:j + 1]
                sh = self.MT[:, lm, sh_sec * 16 + c, j:j + 1]
                self.act(u[:, c, 0:n], zc, AF.Identity, [B_z, self.B_MT], [B_u], bias=sh, scale=sc)
                if router is not None:
                    uf, B_uf = ufrot.next()
                    self.ts("dve", uf[:, 0:n], zc, sc, sh, ALU.mult, ALU.add, [B_z, self.B_MT], [B_uf])
                    for tl in range(n // 128):
                        self.mm(P[2 + tl][:, 0:NE], uf[:, tl * 128:(tl + 1) * 128], wr[:, c, :],
                                c == 0, False, [B_uf, B_wr], [PB[2 + tl]])
            if has_ln:
                S.dma("sp", HTv[:, :, t0:t0 + n], z[:, :, 0:n], BZ, [HTB[g]])
            S.dma("sp", UTv[:, :, t0:t0 + n], u[:, :, 0:n], [B_u], [UTB[g]])
            if router is not None:
                ntl = n // 128
                for tl in range(ntl):
                    self.mm(P[2 + tl][:, 0:NE], self.ones[0:1, :], br[:], False, True, [B_br, self.B_const], [PB[2 + tl]])
                    self.cp("dve", lg[:, tl, :], P[2 + tl][:, 0:NE], [PB[2 + tl]], [B_lg])
                for tl in range(ntl):
                    S.op("dve", lambda e, tl=tl: e.max(out=m8[:, tl, :], in_=lg[:, tl, :]), [B_lg], [B_m8])
                self.ts("dve", sm[:, 0:ntl, 0:1], m8[:, 0:ntl, 0:1], -1.0, None, ALU.mult, None, [B_m8], [B_sm])
                for tl in range(ntl):
                    self.act(ex[:, tl, :], lg[:, tl, :], AF.Exp, [B_lg, B_sm], [B_ex], bias=sm[:, tl, 0:1])
                    self.ts("dve", mk[:, tl, :], lg[:, tl, :], m8[:, tl, 3:4], None, ALU.is_ge, None, [B_lg, B_m8], [B_mk])
                self.tt("dve", ex[:, 0:ntl, :], ex[:, 0:ntl, :], mk[:, 0:ntl, :], ALU.mult, [B_ex, B_mk], [B_ex])
                S.op("dve", lambda e, ntl=ntl: e.tensor_reduce(out=sm[:, 0:ntl, 1:2], in_=ex[:, 0:ntl, :], axis=AX.X, op=ALU.add),
                     [B_ex], [B_sm])
                S.op("dve", lambda e, ntl=ntl: e.reciprocal(out=sm[:, 0:ntl, 2:3], in_=sm[:, 0:ntl, 1:2]), [B_sm], [B_sm])
                for tl in range(ntl):
                    self.ts("dve", ex[:, tl, :], ex[:, tl, :], sm[:, tl, 2:3], None, ALU.mult, None, [B_ex, B_sm], [B_ex])
                    self.tr(P[6][0:NE, tl * 128:(tl + 1) * 128], ex[:, tl, :], [B_ex], [PB[6]])
                self.cp("dve", gts[:, 0:n], P[6][0:NE, 0:n], [PB[6]], [B_gts])
                S.dma("sp", GT[:, t0:t0 + n], gts[:, 0:n], [B_gts], [GTB[g]])

    def moe(self, l, UT, UTB, GT, GTB, YT, YTB, wgu, bgu, wdn, bdn, skip_ctx=False):
        S = self.S
        self.stage()
        P, PB = self.ps, self.psB
        TG = 768
        SG = 384
        ug, B_ug = self.sbb([128, NCH, TG], BF16)
        acc, B_acc = self.sbb([128, NCH, TG], F32)
        gt, B_gt = self.sbb([NE, TG], F32)
        sel, B_sel = self.sbb([NE, NE, 128], F32)
        bdt, B_bdt = self.sbb([NE, D], F32)
        bgT, B_bgT = self.sbb([128, 12, NE], F32)
        bgl, B_bgl = self.sbb([NE, 1536], F32)
        wrot = self.rot(3, [128, 16, 256], BF16)
        drot = self.rot(3, [128, 6, 1024], BF16)
        arot = self.rot(2, [128, 6, TG], BF16)
        t1rot = self.rot(2, [128, SG], F32)
        t2rot = self.rot(2, [128, SG], F32)
        t3rot = self.rot(2, [128, SG], F32)
        S.op("dve", lambda e: e.tensor_copy(out=sel[:], in_=self.ident[0:NE, 0:NE].unsqueeze(2).broadcast_to([NE, NE, 128])),
             [self.B_const], [B_sel])
        S.dma("sp", bdt[:], bdn, (), [B_bdt])
        S.dma("sp", bgl[:], bgu, (), [B_bgl])
        for fc in range(12):
            self.tr(P[0][:, fc * NE:(fc + 1) * NE], bgl[:, fc * 128:(fc + 1) * 128], [B_bgl], [PB[0]])
        self.cp("dve", bgT[:], P[0][:, 0:12 * NE].rearrange("p (f e) -> p f e", e=NE), [PB[0]], [B_bgT])
        UTv = UT.rearrange("c p t -> p c t")
        YTv = YT.rearrange("c p t -> p c t")
        pgu = 0
        py = 0
        for tg in range(T // TG):
            t0 = tg * TG
            rg = sorted(set(self.group_of(t0 + a) for a in range(0, TG, 128)))
            S.dma("sp", ug[:], UTv[:, :, t0:t0 + TG], [UTB[g] for g in rg], [B_ug])
            S.dma("sp", gt[:], GT[:, t0:t0 + TG], [GTB[g] for g in rg], [B_gt])
            sgs = [(256, 256), (512, 256)] if (skip_ctx and tg == 0) else [(0, SG), (SG, SG)]
            if skip_ctx and tg == 0:
                S.op("pool", lambda e: e.memset(acc[:, :, 0:256], 0.0), (), [B_acc])
            for ex in range(NE):
                for sg, (so, sn) in enumerate(sgs):
                    self.mm(P[4 + sg][:, 0:sn], sel[:, ex, :], gt[:, so:so + sn], True, True,
                            [B_sel, B_gt], [PB[4 + sg]])
                a, B_a = arot.next()
                for j in range(6):
                    w, B_w = wrot.next()
                    S.dma("pool", w[:], wgu[ex, j], (), [B_w])
                    for sg, (so, sn) in enumerate(sgs):
                        pg, B_pg = P[pgu], PB[pgu]
                        pu, B_pu = P[pgu + 1], PB[pgu + 1]
                        pgu = (pgu + 2) % 4
                        for k in range(16):
                            self.mm(pg[:, 0:sn], w[:, k, 0:128], ug[:, k, so:so + sn], k == 0, k == 15, [B_w, B_ug], [B_pg])
                        for k in range(16):
                            self.mm(pu[:, 0:sn], w[:, k, 128:256], ug[:, k, so:so + sn], k == 0, k == 15, [B_w, B_ug], [B_pu])
                        g1, B_g1 = t1rot.next()
                        u1, B_u1 = t2rot.next()
                        s1, B_s1 = t3rot.next()
                        self.ts("dve", g1[:, 0:sn], pg[:, 0:sn], bgT[:, j, ex:ex + 1], 7.0, ALU.add, ALU.min, [B_pg, B_bgT], [B_g1])
                        self.ts("dve", u1[:, 0:sn], pu[:, 0:sn], bgT[:, 6 + j, ex:ex + 1], 7.0, ALU.add, ALU.min, [B_pu, B_bgT], [B_u1])
                        self.act(s1[:, 0:sn], g1[:, 0:sn], AF.Sigmoid, [B_g1], [B_s1], scale=1.702)
                        self.ts("dve", u1[:, 0:sn], u1[:, 0:sn], -7.0, 1.0, ALU.max, ALU.add, [B_u1], [B_u1])
                        self.tt("dve", g1[:, 0:sn], g1[:, 0:sn], s1[:, 0:sn], ALU.mult, [B_g1, B_s1], [B_g1])
                        self.tt("dve", g1[:, 0:sn], g1[:, 0:sn], u1[:, 0:sn], ALU.mult, [B_g1, B_u1], [B_g1])
                        self.tt("dve", a[:, j, so:so + sn], g1[:, 0:sn], P[4 + sg][:, 0:sn], ALU.mult, [B_g1, PB[4 + sg]], [B_a])
                for dh in range(2):
                    wd, B_wd = drot.next()
                    S.dma("pool", wd[:], wdn[ex].rearrange("(f p) d -> p f d", p=128)[:, :, dh * 1024:(dh + 1) * 1024], (), [B_wd])
                    for dc in range(8):
                        cc = dh * 8 + dc
                        for sg, (so, sn) in enumerate(sgs):
                            pyb, B_py = P[6 + py], PB[6 + py]
                            py = (py + 1) % 2
                            if ex == 0:
                                self.mm(pyb[:, 0:sn], bdt[:, cc * 128:(cc + 1) * 128], gt[:, so:so + sn], True, False,
                                        [B_bdt, B_gt], [B_py])
                            for f in range(6):
                                self.mm(pyb[:, 0:sn], wd[:, f, dc * 128:(dc + 1) * 128], a[:, f, so:so + sn],
                                        (f == 0 and ex != 0), f == 5, [B_wd, B_a], [B_py])
                            av = acc[:, cc, so:so + sn]
                            if ex == 0:
                                self.cp("dve", av, pyb[:, 0:sn], [B_py], [B_acc])
                            else:
                                self.tt("dve", av, av, pyb[:, 0:sn], ALU.add, [B_py, B_acc], [B_acc])
            S.dma("sp", YTv[:, :, t0:t0 + TG], acc[:], [B_acc], [YTB[g] for g in rg])

    def epilogue(self, HT, HTB, out):
        S = self.S
        self.stage()
        P, PB = self.ps, self.psB
        hrot = self.rot(2, [128, NCH, 128], F32)
        orot = self.rot(2, [128, D], F32)
        HTv = HT.rearrange("c p t -> p c t")
        B_out = Buf("out")
        pi = 0
        for tt in range(2, NT):
            h, B_h = hrot.next()
            g = self.group_of(tt * 128)
            S.dma("sp", h[:], HTv[:, :, tt * 128:(tt + 1) * 128], [HTB[g]], [B_h])
            o, B_o = orot.next()
            for cq in range(4):
                pt, B_pt = P[pi], PB[pi]
                pi = (pi + 1) % 8
                for j in range(4):
                    self.tr(pt[:, j * 128:(j + 1) * 128], h[:, cq * 4 + j, :], [B_h], [B_pt])
                self.cp("act" if cq % 2 else "dve", o[:, cq * 512:(cq + 1) * 512], pt[:], [B_pt], [B_o])
            S.dma("sp", out[(tt - 2) * 128:(tt - 1) * 128, :], o[:], [B_o], [B_out])
        return B_out

    def dump(self, src, dst, srcB):
        B = Buf("dump")
        self.S.dma("sp", dst, src, srcB, [B])
        return B

    def finish(self, bufs):
        S = self.S
        waits = S._deps("sp", bufs, ())
        S.ops["sp"].append((waits, None, None, 0))
        S.fence()
        S.emit()


def rope_tables():
    inv = (np.float32(10000.0) ** (-np.arange(0, 32, 2, dtype=np.float32) / np.float32(32))).astype(np.float32)
    t = np.arange(SEQ)
    pos = np.stack([t // 64, t % 64], 0).astype(np.float32)
    cosT = np.zeros((128, SEQ), np.float32)
    sinT = np.zeros((128, SEQ), np.float32)
    pmat = np.zeros((128, 128), np.float32)
    for p in range(128):
        d = p % 64
        axis, pair, f = d // 32, (d % 32) // 16, d % 16
        ang = (pos[axis] * inv[f]).astype(np.float32)
        cosT[p] = np.cos(ang)
        sinT[p] = np.sin(ang) * (-1.0 if pair == 0 else 1.0)
        partner = p + 16 if pair == 0 else p - 16
        pmat[partner, p] = 1.0
    return cosT, sinT, pmat


def attn_proj(self, UT, UTB, w_qkv, b_qkv, cos_in, sin_in, pmat_in, QTd, KTd, VAd, B_att):
    S = self.S
    self.stage()
    P, PB = self.ps, self.psB
    ug, B_ug = self.sbb([128, NCH, T], BF16)
    UTv = UT.rearrange("c p t -> p c t")
    for g, (t0, n) in enumerate(GROUPS):
        S.dma("sp", ug[:, :, t0:t0 + n], UTv[:, :, t0:t0 + n], [UTB[g]], [B_ug])
    cosT, B_tab = self.sbb([128, SEQ], F32)
    sinT = self.sb([128, SEQ], F32)
    pm = self.sb([128, 128], F32)
    S.dma("sp", cosT[:], cos_in, (), [B_tab])
    S.dma("sp", sinT[:], sin_in, (), [B_tab])
    S.dma("sp", pm[:], pmat_in, (), [B_tab])
    brow, B_brow = self.sbb([1, 2560], F32)
    S.dma("sp", brow[:], b_qkv, (), [B_brow])
    bkd, B_bkd = self.sbb([1, 4, 128], F32)
    for kv in range(4):
        for hh in range(2):
            S.dma("sp", bkd[0:1, kv, hh * 64:(hh + 1) * 64], b_qkv[0:1, 2048 + kv * 64:2048 + (kv + 1) * 64], (), [B_bkd])
    wrot = self.rot(2, [128, NCH, 512], BF16)
    wkd, B_wkd = self.sbb([128, NCH, 4, 128], BF16)
    q32rot = self.rot(2, [128, 512], F32)
    t1rot = self.rot(2, [128, 512], F32)
    qsrot = self.rot(3, [128, 512], BF16)
    vaug, B_va = self.sbb([128, NT, 4, 65], BF16)
    S.op("pool", lambda e: e.memset(vaug[:, :, :, 64:65], 1.0), (), [B_va])
    wv = w_qkv.rearrange("(k p) n -> p k n", p=128)
    pi = 0

    def project(lhs_fn, bias_ap, dst, c, Bw):
        nonlocal pi
        for g, (t0, n) in enumerate(GROUPS):
            pt, B_pt = P[pi], PB[pi]
            pi = (pi + 1) % 4
            for k in range(NCH):
                self.mm(pt[:, 0:n], lhs_fn(k), ug[:, k, t0:t0 + n], k == 0, False, [Bw, B_ug], [B_pt])
            self.mm(pt[:, 0:n], bias_ap, self.onesrow[0:1, 0:n], False, True, [B_brow, B_bkd, self.B_const], [B_pt])
            qs, B_qs = qsrot.next()
            if g == 0:
                self.cp("act", qs[:, 0:n], pt[:, 0:n], [B_pt], [B_qs])
            else:
                q32, B_q32 = q32rot.next()
                t1, B_t1 = t1rot.next()
                self.cp("act", q32[:, 0:n], pt[:, 0:n], [B_pt], [B_q32])
                p2, B_p2 = P[4 + (pi % 2)], PB[4 + (pi % 2)]
                self.mm(p2[:, 0:n], pm[:], q32[:, 0:n], True, True, [B_tab, B_q32], [B_p2])
                l0 = t0 - CTX
                self.tt("pool", t1[:, 0:n], q32[:, 0:n], cosT[:, l0:l0 + n], ALU.mult, [B_q32, B_tab], [B_t1])
                self.tt("dve", q32[:, 0:n], p2[:, 0:n], sinT[:, l0:l0 + n], ALU.mult, [B_p2, B_tab], [B_q32])
                self.tt("pool", qs[:, 0:n], t1[:, 0:n], q32[:, 0:n], ALU.add, [B_t1, B_q32], [B_qs])
            S.dma("sp", dst[c][:, t0:t0 + n], qs[:, 0:n], [B_qs], [B_att])

    for wb in range(5):
        w, B_w = wrot.next()
        S.dma("pool", w[:], wv[:, :, wb * 512:(wb + 1) * 512], (), [B_w])
        if wb < 4:
            for f in range(4):
                c = wb * 4 + f
                project(lambda k, w=w, f=f: w[:, k, f * 128:(f + 1) * 128], brow[0:1, c * 128:(c + 1) * 128], QTd, c, B_w)
        else:
            for hh in range(2):
                self.cp("dve", wkd[:, :, :, hh * 64:(hh + 1) * 64], w[:, :, 0:256].rearrange("p k (v d) -> p k v d", d=64),
                        [B_w], [B_wkd])
            for kv in range(4):
                project(lambda k, kv=kv: wkd[:, k, kv, :], bkd[0:1, kv, :], KTd, kv, B_wkd)
            for tt in range(NT):
                pt, B_pt = P[6 + tt % 2], PB[6 + tt % 2]
                for k in range(NCH):
                    self.mm(pt[:, 0:256], ug[:, k, tt * 128:(tt + 1) * 128], w[:, k, 256:512], k == 0, False, [B_w, B_ug], [B_pt])
                self.mm(pt[:, 0:256], self.ones[0:1, :], brow[0:1, 2304:2560], False, True, [B_brow, self.B_const], [B_pt])
                self.cp("act", vaug[:, tt, :, 0:64], pt[:, 0:256].rearrange("p (v d) -> p v d", d=64), [B_pt], [B_va])
            S.dma("sp", VAd, vaug[:].rearrange("p t v d -> p (t v d)"), [B_va], [B_att])


def attn_core(self, QTd, KTd, VAd, B_att, sink, w_o, b_o, YT, YTB):
    S = self.S
    self.stage()
    P, PB = self.ps, self.psB
    kt, B_kt = self.sbb([128, 4, T], BF16)
    S.dma("sp", kt[:], KTd.rearrange("v p t -> p v t"), [B_att], [B_kt])
    vaug, B_va = self.sbb([128, NT, 4, 65], BF16)
    S.dma("sp", vaug[:].rearrange("p t v d -> p (t v d)"), VAd, [B_att], [B_va])
    mask3, B_mk = self.sbb([128, 384], BF16)
    self.cp("dve", mask3[:, 0:128], self.m_ge[:], [self.B_const], [B_mk])
    self.cp("dve", mask3[:, 128:256], self.ones[:], [self.B_const], [B_mk])
    self.cp("dve", mask3[:, 256:384], self.m_le[:], [self.B_const], [B_mk])
    srow, B_srow = self.sbb([1, 32], F32)
    S.dma("sp", srow[:], sink, (), [B_srow])
    esink, B_es = self.sbb([128, 32], F32)
    self.mm(P[0][:, 0:32], self.ones[0:1, :], srow[:], True, True, [B_srow, self.B_const], [PB[0]])
    self.act(esink[:], P[0][:, 0:32], AF.Exp, [PB[0]], [B_es])
    borow, B_bo = self.sbb([1, D], F32)
    S.dma("sp", borow[:], b_o, (), [B_bo])
    qrot = self.rot(2, [128, NCH, 512], BF16)
    pcrot = self.rot(3, [128, 2, 512], BF16)
    plrot = self.rot(9, [128, 384], BF16)
    ot, B_ot = self.sbb([128, 4, D], F32)
    otg, B_otg = self.sbb([128, NCH, 512], BF16)
    worot = self.rot(3, [128, NCH, 128], BF16)
    yrot = self.rot(2, [128, 512], F32)
    den, B_den = self.sbb([128, 8], F32)
    QTv = QTd.rearrange("c p t -> p c t")
    wov = w_o.rearrange("(k p) n -> p k n", p=128)
    i_c = i_l = i_o = i_t = 0
    for g, (t0, n) in enumerate(GROUPS):
        nb = n // 128
        qg, B_qg = qrot.next()
        S.dma("sp", qg[:, :, 0:n], QTv[:, :, t0:t0 + n], [B_att], [B_qg])
        def stage1(h):
            nonlocal i_c, i_l
            qc, r0, kv = h // 2, (h % 2) * 64, h // 8
            pc, B_pc = pcrot.next()
            for ct in range(2):
                pt, B_pt = P[i_c], PB[i_c]
                i_c = (i_c + 1) % 2
                self.mm(pt[:, 0:n], kt[r0:r0 + 64, kv, ct * 128:(ct + 1) * 128], qg[r0:r0 + 64, qc, 0:n], True, True, [B_kt, B_qg], [B_pt])
                self.act(pc[:, ct, 0:n], pt[:, 0:n], AF.Exp, [B_pt], [B_pc], scale=0.125)
            pls = []
            for j in range(nb):
                if g > 0:
                    qb = (g - 1) * 4 + j
                    slots = [s_ for s_ in range(3) if 0 <= qb + s_ - 1 <= 15]
                    pt, B_pt = P[2 + i_l], PB[2 + i_l]
                    i_l = (i_l + 1) % 2
                    for s_ in slots:
                        k0 = CTX + (qb + s_ - 1) * 128
                        self.mm(pt[:, s_ * 128:(s_ + 1) * 128], kt[r0:r0 + 64, kv, k0:k0 + 128], qg[r0:r0 + 64, qc, j * 128:(j + 1) * 128],
                                True, True, [B_kt, B_qg], [B_pt])
                    pl, B_pl = plrot.next()
                    a_, b_ = slots[0] * 128, (slots[-1] + 1) * 128
                    self.act(pl[:, a_:b_], pt[:, a_:b_], AF.Exp, [B_pt], [B_pl], scale=0.125)
                    self.tt("pool", pl[:, a_:b_], pl[:, a_:b_], mask3[:, a_:b_], ALU.mult, [B_pl, B_mk], [B_pl])
                    pls.append((pl, B_pl, slots, qb))
                else:
                    pls.append(None)
            return (h, pc, B_pc, pls)

        def stage2(st):
            nonlocal i_o
            h, pc, B_pc, pls = st
            kv = h // 8
            po, B_po = P[4 + i_o], PB[4 + i_o]
            i_o = (i_o + 1) % 2
            for j in range(nb):
                mms = []
                if pls[j] is not None:
                    pl, B_pl, slots, qb = pls[j]
                    for s_ in slots:
                        mms.append((pl[:, s_ * 128:(s_ + 1) * 128], vaug[:, 2 + qb + s_ - 1, kv, :], B_pl))
                for ct in range(2):
                    mms.append((pc[:, ct, j * 128:(j + 1) * 128], vaug[:, ct, kv, :], B_pc))
                for i, (l_, r_, B_) in enumerate(mms):
                    self.mm(po[:, j * 65:(j + 1) * 65], l_, r_, i == 0, i == len(mms) - 1, [B_, B_va], [B_po])
            pov = po[:, 0:nb * 65].rearrange("p (j d) -> p j d", d=65)
            self.ts("dve", den[:, 0:nb], pov[:, :, 64], esink[:, h:h + 1], None, ALU.add, None, [B_po, B_es], [B_den])
            S.op("dve", lambda e, nb=nb: e.reciprocal(out=den[:, 0:nb], in_=den[:, 0:nb]), [B_den], [B_den])
            self.tt("dve", ot[:, 0:nb, h * 64:(h + 1) * 64], pov[:, :, 0:64], den[:, 0:nb].unsqueeze(2).broadcast_to([128, nb, 64]),
                    ALU.mult, [B_po, B_den], [B_ot])

        prev = None
        for h in range(33):
            cur_st = stage1(h) if h < 32 else None
            if prev is not None:
                stage2(prev)
            prev = cur_st
        for j in range(nb):
            for cq in range(4):
                pt, B_pt = P[6 + i_t], PB[6 + i_t]
                i_t = (i_t + 1) % 2
                for f in range(4):
                    c = cq * 4 + f
                    self.tr(pt[:, f * 128:(f + 1) * 128], ot[:, j, c * 128:(c + 1) * 128], [B_ot], [B_pt])
                self.cp("act" if cq % 2 else "dve", otg[:, cq * 4:(cq + 1) * 4, j * 128:(j + 1) * 128],
                        pt[:].rearrange("p (f t) -> p f t", f=4), [B_pt], [B_otg])
        for dc in range(NCH):
            wo, B_wo = worot.next()
            S.dma("pool", wo[:], wov[:, :, dc * 128:(dc + 1) * 128], (), [B_wo])
            pt, B_pt = P[6 + i_t], PB[6 + i_t]
            i_t = (i_t + 1) % 2
            for c in range(NCH):
                self.mm(pt[:, 0:n], wo[:, c, :], otg[:, c, 0:n], c == 0, False, [B_wo, B_otg], [B_pt])
            self.mm(pt[:, 0:n], borow[0:1, dc * 128:(dc + 1) * 128], self.onesrow[0:1, 0:n], False, True, [B_bo, self.B_const], [B_pt])
            y, B_y = yrot.next()
            self.cp("act", y[:, 0:n], pt[:, 0:n], [B_pt], [B_y])
            S.dma("sp", YT[dc][:, t0:t0 + n], y[:, 0:n], [B_y], [YTB[g]])


K.attn_proj = attn_proj
K.attn_core = attn_core


L2_EPS = 1e-6
RMS_EPS = 1e-6


def dn_proj(self, UT, UTB, w_in, conv_w, a_log, dt_bias, QTn, KTn, KTOK, VTOK, ZSd, GBd, B_dn):
    S = self.S
    self.stage()
    P, PB = self.ps, self.psB
    ug, B_ug = self.sbb([128, NCH, T], BF16)
    UTv = UT.rearrange("c p t -> p c t")
    for g, (t0, n) in enumerate(GROUPS):
        S.dma("sp", ug[:, :, t0:t0 + n], UTv[:, :, t0:t0 + n], [UTB[g]], [B_ug])
    w5l, B_w5l = self.sbb([128, 3, 128], F32)
    w5T, B_w5 = self.sbb([128, 320], F32)
    cwv = conv_w.rearrange("j (c p) -> (j c) p", p=128)
    for i, (a, b) in enumerate(((0, 128), (128, 256), (256, 320))):
        S.dma("sp", w5l[0:b - a, i, :], cwv[a:b, :], (), [B_w5l])
        self.tr(P[0][:, a:b], w5l[0:b - a, i, :], [B_w5l], [PB[0]])
    self.cp("dve", w5T[:], P[0][:, 0:320], [PB[0]], [B_w5])
    wv = w_in.rearrange("(k p) n -> p k n", p=128)
    wrot = self.rot(2, [128, NCH, 512], BF16)
    xcrot = self.rot(2, [128, T], F32)
    ycrot = self.rot(2, [128, T], F32)
    sqrot = self.rot(2, [128, T], F32)
    rnrot = self.rot(2, [128, 512], F32)
    qsrot = self.rot(2, [128, 512], BF16)
    ktok, B_ktok = self.sbb([128, NT, 128], BF16)
    vtok, B_vtok = self.sbb([128, NT, 128], F32)
    zsrot = self.rot(2, [128, 512], F32)
    pi = 0
    for wb in range(16):
        w, B_w = wrot.next()
        S.dma("pool", w[:], wv[:, :, wb * 512:(wb + 1) * 512], (), [B_w])
        for f in range(4):
            cc = wb * 4 + f
            xc, B_xc = xcrot.next()
            yc, B_yc = ycrot.next()
            sq, B_sq = sqrot.next()
            for g, (t0, n) in enumerate(GROUPS):
                pt, B_pt = P[pi], PB[pi]
                pi = (pi + 1) % 4
                for k in range(NCH):
                    self.mm(pt[:, 0:n], w[:, k, f * 128:(f + 1) * 128], ug[:, k, t0:t0 + n], k == 0, k == NCH - 1, [B_w, B_ug], [B_pt])
                self.cp("act", xc[:, t0:t0 + n], pt[:, 0:n], [B_pt], [B_xc])
            ce = "dve"
            for (s0, s1) in ((0, CTX), (CTX, T)):
                self.ts(ce, yc[:, s0:s1], xc[:, s0:s1], w5T[:, 2 * 64 + cc:2 * 64 + cc + 1], None, ALU.mult, None, [B_xc, B_w5], [B_yc])
                for j in (0, 1, 3, 4):
                    sh = j - 2
                    a, b = s0 + max(0, -sh), s1 - max(0, sh)
                    self.stt(ce, yc[:, a:b], xc[:, a + sh:b + sh], w5T[:, j * 64 + cc:j * 64 + cc + 1], yc[:, a:b], ALU.mult, ALU.add,
                             [B_xc, B_w5, B_yc], [B_yc])
            self.act(yc[:], yc[:], AF.Silu, [B_yc], [B_yc])
            if cc < 32:
                hq = cc % 16
                isq = cc < 16
                self.act(sq[:], yc[:], AF.Square, [B_yc], [B_sq])
                for g, (t0, n) in enumerate(GROUPS):
                    pt, B_pt = P[4 + g % 2], PB[4 + g % 2]
                    self.mm(pt[:, 0:n], self.ones[:], sq[:, t0:t0 + n], True, True, [B_sq, self.B_const], [B_pt])
                    rn, B_rn = rnrot.next()
                    self.act(rn[:, 0:n], pt[:, 0:n], AF.Sqrt, [B_pt], [B_rn], bias=L2_EPS)
                    S.op("dve", lambda e, rn=rn, n=n: e.reciprocal(out=rn[:, 0:n], in_=rn[:, 0:n]), [B_rn], [B_rn])
                    if isq:
                        self.stt("dve", rn[:, 0:n], yc[:, t0:t0 + n], 128.0 ** -0.5, rn[:, 0:n], ALU.mult, ALU.mult, [B_yc, B_rn], [B_rn])
                    else:
                        self.tt("dve", rn[:, 0:n], yc[:, t0:t0 + n], rn[:, 0:n], ALU.mult, [B_yc, B_rn], [B_rn])
                    qs, B_qs = qsrot.next()
                    self.cp("act", qs[:, 0:n], rn[:, 0:n], [B_rn], [B_qs])
                    S.dma("sp", (QTn if isq else KTn)[hq][:, t0:t0 + n], qs[:, 0:n], [B_qs], [B_dn])
                    if not isq:
                        pt2, B_pt2 = P[6 + g % 2], PB[6 + g % 2]
                        for tl in range(n // 128):
                            self.tr(pt2[:, tl * 128:(tl + 1) * 128], rn[:, tl * 128:(tl + 1) * 128], [B_rn], [B_pt2])
                        tt0 = t0 // 128
                        self.cp("dve", ktok[:, tt0:tt0 + n // 128, :], pt2[:, 0:n].rearrange("p (t d) -> p t d", d=128), [B_pt2], [B_ktok])
                if not isq:
                    S.dma("sp", KTOK[hq], ktok[:], [B_ktok], [B_dn])
            else:
                hv = cc - 32
                for g, (t0, n) in enumerate(GROUPS):
                    pt2, B_pt2 = P[6 + g % 2], PB[6 + g % 2]
                    for tl in range(n // 128):
                        self.tr(pt2[:, tl * 128:(tl + 1) * 128], yc[:, t0 + tl * 128:t0 + (tl + 1) * 128], [B_yc], [B_pt2])
                    tt0 = t0 // 128
                    self.cp("dve", vtok[:, tt0:tt0 + n // 128, :], pt2[:, 0:n].rearrange("p (t d) -> p t d", d=128), [B_pt2], [B_vtok])
                S.dma("sp", VTOK[hv], vtok[:], [B_vtok], [B_dn])
    ZSv = ZSd.rearrange("h p t e -> p h t e")
    for zb in range(8):
        w, B_w = wrot.next()
        S.dma("pool", w[:], wv[:, :, 8192 + zb * 512:8192 + (zb + 1) * 512], (), [B_w])
        for tt in range(NT):
            pt, B_pt = P[pi], PB[pi]
            pi = (pi + 1) % 4
            for k in range(NCH):
                self.mm(pt[:], ug[:, k, tt * 128:(tt + 1) * 128], w[:, k, :], k == 0, k == NCH - 1, [B_w, B_ug], [B_pt])
            zs, B_zs = zsrot.next()
            self.act(zs[:], pt[:], AF.Silu, [B_pt], [B_zs])
            S.dma("sp", ZSv[:, zb * 4:(zb + 1) * 4, tt, :], zs[:].rearrange("p (h e) -> p h e", e=128), [B_zs], [B_dn])
    w, B_w = wrot.next()
    S.dma("pool", w[:, :, 0:128], wv[:, :, 12288:12416], (), [B_w])
    arow, B_arow = self.sbb([1, 128], F32)
    S.dma("sp", arow[0:1, 0:64], a_log, (), [B_arow])
    S.dma("sp", arow[0:1, 64:128], dt_bias, (), [B_arow])
    abc, B_abc = self.sbb([128, 128], F32)
    self.mm(P[4][:, 0:128], self.ones[0:1, :], arow[:], True, True, [B_arow, self.B_const], [PB[4]])
    self.act(abc[:, 0:64], P[4][:, 0:64], AF.Exp, [PB[4]], [B_abc])
    self.ts("dve", abc[:, 0:64], abc[:, 0:64], -1.0, None, ALU.mult, None, [B_abc], [B_abc])
    self.cp("dve", abc[:, 64:128], P[4][:, 64:128], [PB[4]], [B_abc])
    gt_, B_g = self.sbb([128, NT, 64], F32)
    bt_, B_b = self.sbb([128, NT, 64], F32)
    tmp, B_tmp = self.sbb([128, 2, 2, 32], F32)
    for tt in range(NT):
        pt, B_pt = P[pi], PB[pi]
        pi = (pi + 1) % 4
        for k in range(NCH):
            self.mm(pt[:, 0:128], ug[:, k, tt * 128:(tt + 1) * 128], w[:, k, 0:128], k == 0, k == NCH - 1, [B_w, B_ug], [B_pt])
        pv = pt[:, 0:128].rearrange("p (d a h) -> p d a h", d=2, a=2)
        self.tt("dve", tmp[:, :, 0, :], pv[:, :, 0, :], abc[:, 64:128].rearrange("p (d h) -> p d h", d=2), ALU.add, [B_pt, B_abc], [B_tmp])
        self.act(tmp[:, :, 0, :], tmp[:, :, 0, :], AF.Exp, [B_tmp], [B_tmp])
        self.act(tmp[:, :, 0, :], tmp[:, :, 0, :], AF.Ln, [B_tmp], [B_tmp], bias=1.0)
        self.tt("dve", gt_[:, tt, :].rearrange("p (d h) -> p d h", d=2), tmp[:, :, 0, :], abc[:, 0:64].rearrange("p (d h) -> p d h", d=2),
                ALU.mult, [B_tmp, B_abc], [B_g])
        self.act(tmp[:, :, 1, :], pv[:, :, 1, :], AF.Exp, [B_pt], [B_tmp], scale=-1.0)
        self.ts("dve", tmp[:, :, 1, :], tmp[:, :, 1, :], 1.0, None, ALU.add, None, [B_tmp], [B_tmp])
        S.op("dve", lambda e, tt=tt: e.reciprocal(out=bt_[:, tt, :].rearrange("p (d h) -> p d h", d=2), in_=tmp[:, :, 1, :]), [B_tmp], [B_b])
    S.dma("sp", GBd[0], gt_[:], [B_g], [B_dn])
    S.dma("sp", GBd[1], bt_[:], [B_b], [B_dn])


def dn_core(self, QTn, KTn, KTOK, VTOK, ZSd, GBd, B_dn, norm_w, OGd, B_og):
    S = self.S
    self.stage()
    P, PB = self.ps, self.psB
    G, B_G = self.sbb([128, NT, 64], F32)
    Bt, B_Bt = self.sbb([128, NT, 64], F32)
    S.dma("sp", G[:], GBd[0], [B_dn], [B_G])
    S.dma("sp", Bt[:], GBd[1], [B_dn], [B_Bt])
    GC, B_GC = self.sbb([128, NT, 64], F32)
    EG, B_EG = self.sbb([128, NT, 64], F32)
    NEG, B_NEG = self.sbb([128, NT, 64], F32)
    BDL, B_BDL = self.sbb([128, NT, 64], F32)
    EGL, B_EGL = self.sbb([128, NT, 64], F32)
    STOP = self.cfg.get("dn_stop", 99)
    if STOP <= 1:
        return
    nwr, B_nwr = self.sbb([1, 128], F32)
    S.dma("sp", nwr[:], norm_w, (), [B_nwr])
    nw, B_nw = self.sbb([128, 128], F32)
    self.mm(P[0][:, 0:128], self.ones[0:1, :], nwr[:], True, True, [B_nwr, self.B_const], [PB[0]])
    self.cp("dve", nw[:], P[0][:, 0:128], [PB[0]], [B_nw])
    if STOP <= 2:
        return
    for ch in range(NT):
        pa, B_pa = P[1 + ch % 2], PB[1 + ch % 2]
        pb, B_pb = P[3 + ch % 2], PB[3 + ch % 2]
        self.mm(pa[:, 0:32], self.m_le[:], G[:, ch, 0:32], True, True, [B_G, self.B_const], [B_pa])
        self.mm(pa[:, 32:64], self.m_ge[:], G[:, ch, 32:64], True, True, [B_G, self.B_const], [B_pa])
        self.mm(pb[:, 0:64], self.ones[:], G[:, ch, :], True, True, [B_G, self.B_const], [B_pb])
        self.cp("dve", GC[:, ch, :], pa[:, 0:64], [B_pa], [B_GC])
        self.act(EG[:, ch, :], pa[:, 0:64], AF.Exp, [B_pa], [B_EG])
        self.act(EGL[:, ch, :], pb[:, 0:64], AF.Exp, [B_pb], [B_EGL])
        self.tt("dve", BDL[:, ch, :], pb[:, 0:64], GC[:, ch, :], ALU.subtract, [B_pb, B_GC], [B_BDL])
    if STOP <= 3:
        return
    self.ts("dve", NEG[:], EG[:], -1.0, None, ALU.mult, None, [B_EG], [B_NEG])
    self.act(BDL[:], BDL[:], AF.Exp, [B_BDL], [B_BDL])
    self.tt("dve", BDL[:], BDL[:], Bt[:], ALU.mult, [B_BDL, B_Bt], [B_BDL])

    if STOP <= 4:
        return
    qT, B_qT = self.sbb([128, T], BF16)
    kT, B_kT = self.sbb([128, T], BF16)
    ktok, B_ktok = self.sbb([128, NT, 128], BF16)
    vtok, B_vtok = self.sbb([128, 2, NT, 128], F32)
    AT, B_AT = self.sbb([128, NT, 4, 128], BF16)
    QKD, B_QKD = self.sbb([128, NT, 4, 128], BF16)
    O, B_O = self.sbb([128, 2, NT, 128], F32)
    ss, B_ss = self.sbb([128, NT], F32)
    ogT, B_ogT = self.sbb([128, T], BF16)
    NCHAIN = 4
    ch_t = []
    setup_t = []
    off_chain = self.off
    for i in range(2):
        d = {}
        for nm in ("KQ", "Gm", "E"):
            shape = [128, 2, 256] if nm == "KQ" else [128, 4, 128]
            d[nm] = self.sbb(shape, F32)
        setup_t.append(d)
    for i in range(NCHAIN):
        d = dict(setup_t[i % 2])
        for nm in ("PRa", "PRb"):
            d[nm] = self.sbb([128, 4, 2, 128], F32)
        for nm in ("PTa", "PTb"):
            d[nm] = self.sbb([128, 4, 128], F32)
        ch_t.append(d)
    off_end = self.off
    self.off = off_chain
    zs, B_zs = self.sbb([128, NT, 128], F32)
    sq, B_sq = self.sbb([128, NT, 128], F32)
    assert self.off <= off_end
    self.off = off_end
    psi = [0]

    def nps():
        i = psi[0]
        psi[0] = (i + 1) % 8
        return P[i], PB[i]
    St, B_S = self.sbb([128, 4, 128], F32)
    Sb, B_Sb = self.sbb([128, 4, 128], BF16)
    rp, B_rp = self.sbb([128, 4, 128], BF16)
    vn, B_vn = self.sbb([128, 4, 128], BF16)
    vd, B_vd = self.sbb([128, 4, 128], BF16)
    strict = (self.m_lt, self.m_gt)
    incl = (self.m_le, self.m_ge)
    aft_k = (self.m_gt, self.m_lt)
    atb_k = (self.m_le, self.m_ge)
    identb = self.ident[:].unsqueeze(1).broadcast_to([128, 4, 128])
    order = ([c for c in range(NT)], [1, 0] + [c for c in range(NT - 1, 1, -1)])
    PH = self.cfg.get("dn_phases", "ABC")
    for hq in range(self.cfg.get("dn_nhq", 16)):
        S.fence()
        S.dma("sp", qT[:], QTn[hq], [B_dn], [B_qT])
        S.dma("sp", kT[:], KTn[hq], [B_dn], [B_kT])
        S.dma("sp", ktok[:], KTOK[hq], [B_dn], [B_ktok])
        for hvl in range(2):
            S.dma("sp", vtok[:, hvl], VTOK[2 * hq + hvl], [B_dn], [B_vtok])
        for c0 in (range(0, NT, NCHAIN) if "A" in PH else []):
            chains = list(range(c0, min(NT, c0 + NCHAIN)))
            for ci, ch in enumerate(chains):
                tmpd = ch_t[ci]
                cs = slice(ch * 128, (ch + 1) * 128)
                KQ, B_KQ = tmpd["KQ"]
                Gm, B_Gm = tmpd["Gm"]
                E, B_E = tmpd["E"]
                PRa_, B_X = tmpd["PRa"]
                X = PRa_[:, :, 0, :]
                pk, B_pk = nps()
                self.mm(pk[:, 0:128], kT[:, cs], kT[:, cs], True, True, [B_kT], [B_pk])
                self.mm(pk[:, 128:256], kT[:, cs], qT[:, cs], True, True, [B_kT, B_qT], [B_pk])
                for d in range(2):
                    self.tt("dve", KQ[:, d, 0:128], pk[:, 0:128], strict[d][:], ALU.mult, [B_pk, self.B_const], [B_KQ])
                    self.tt("dve", KQ[:, d, 128:256], pk[:, 128:256], incl[d][:], ALU.mult, [B_pk, self.B_const], [B_KQ])
                for q in range(4):
                    d, hvl = q // 2, q % 2
                    col = d * 32 + 2 * hq + hvl
                    self.ts("pool", Gm[:, q, :], aft_k[d][:], G[:, ch, col:col + 1], None, ALU.mult, None, [B_G, self.B_const], [B_Gm])
                pd, B_pd = nps()
                for q in range(4):
                    self.mm(pd[:, q * 128:(q + 1) * 128], Gm[:, q, :], atb_k[q // 2][:], True, True, [B_Gm, self.B_const], [B_pd])
                self.act(E[:].rearrange("p q i -> p (q i)"), pd[:], AF.Exp, [B_pd], [B_E])
                for q in range(4):
                    d, hvl = q // 2, q % 2
                    col = d * 32 + 2 * hq + hvl
                    self.stt("dve", X[:, q, :], E[:, q, :], Bt[:, ch, col:col + 1], KQ[:, d, 0:128], ALU.mult, ALU.mult,
                             [B_E, B_Bt, B_KQ], [B_X])
                for d in range(2):
                    self.tt("pool", QKD[:, ch, 2 * d:2 * d + 2, :], E[:, 2 * d:2 * d + 2, :],
                            KQ[:, d, 128:256].unsqueeze(1).broadcast_to([128, 2, 128]), ALU.mult, [B_E, B_KQ], [B_QKD])
            for ci, ch in enumerate(chains):
                tmpd = ch_t[ci]
                PRa_, B_PRa = tmpd["PRa"]
                PTa_, B_PTa = tmpd["PTa"]
                pt, B_pt = nps()
                for q in range(4):
                    self.tr(pt[:, q * 128:(q + 1) * 128], PRa_[:, q, 0, :], [B_PRa], [B_pt])
                self.cp("act", PTa_[:].rearrange("p q i -> p (q i)"), pt[:], [B_pt], [B_PTa])
            for s_ in range(7):
                for ci, ch in enumerate(chains):
                    tmpd = ch_t[ci]
                    (PRc, B_PRc), (PTc, B_PTc) = (tmpd["PRa"], tmpd["PTa"]) if s_ % 2 == 0 else (tmpd["PRb"], tmpd["PTb"])
                    (PRn, B_PRn), (PTn, B_PTn) = (tmpd["PRb"], tmpd["PTb"]) if s_ % 2 == 0 else (tmpd["PRa"], tmpd["PTa"])
                    lo, hi = (0, 128) if s_ == 0 else ((128, 256) if s_ >= 5 else (0, 256))
                    banks = [nps(), nps()]
                    for q in range(4):
                        pb_, B_pb_ = banks[q // 2]
                        o0 = (q % 2) * 256
                        self.mm(pb_[:, o0 + lo:o0 + hi], PTc[:, q, :], PRc[:, q].rearrange("p a i -> p (a i)")[:, lo:hi], True, True,
                                [B_PTc, B_PRc], [B_pb_])
                    if s_ < 6:
                        pq, B_pq = nps()
                        for q in range(4):
                            self.mm(pq[:, q * 128:(q + 1) * 128], PRc[:, q, 0, :], PTc[:, q, :], True, True, [B_PRc, B_PTc], [B_pq])
                    for hb in range(2):
                        pb_, B_pb_ = banks[hb]
                        pv = pb_[:].rearrange("p (q a i) -> p q a i", q=2, a=2)
                        qs = slice(2 * hb, 2 * hb + 2)
                        if s_ < 5:
                            self.cp("act", PRn[:, qs, 0, :], pv[:, :, 0, :], [B_pb_], [B_PRn])
                        if s_ == 0:
                            self.tt("pool", PRn[:, qs, 1, :], self.ident[:].unsqueeze(1).broadcast_to([128, 2, 128]), PRc[:, qs, 0, :],
                                    ALU.subtract, [B_PRc, self.B_const], [B_PRn])
                        elif s_ < 6:
                            self.tt("dve", PRn[:, qs, 1, :], PRc[:, qs, 1, :], pv[:, :, 1, :], ALU.add, [B_pb_, B_PRc], [B_PRn])
                        else:
                            self.tt("dve", AT[:, ch, qs, :], PRc[:, qs, 1, :], pv[:, :, 1, :], ALU.add, [B_pb_, B_PRc], [B_AT])
                    if s_ < 6:
                        self.cp("act" if s_ % 2 else "dve", PTn[:].rearrange("p q i -> p (q i)"), pq[:], [B_pq], [B_PTn])
        S.op("pool", lambda e: e.memset(St[:], 0.0), (), [B_S])
        S.op("pool", lambda e: e.memset(Sb[:], 0.0), (), [B_Sb])
        S.op("pool", lambda e: e.memset(O[:], 0.0), (), [B_O])
        for s in (range(NT) if "B" in PH else []):
            pA, B_pA = P[0], PB[0]
            pB_, B_pB = P[1], PB[1]
            pC, B_pC = P[2], PB[2]
            pD, B_pD = P[3], PB[3]
            pE, B_pE = P[6], PB[6]
            info = []
            for q in range(4):
                d, hvl = q // 2, q % 2
                ch = order[d][s]
                info.append((d, hvl, ch, d * 32 + 2 * hq + hvl, slice(ch * 128, (ch + 1) * 128)))
            for q, (d, hvl, ch, col, cs) in enumerate(info):
                self.mm(pA[:, q * 128:(q + 1) * 128], kT[:, cs], Sb[:, q, :], True, True, [B_kT, B_Sb], [B_pA])
            for q, (d, hvl, ch, col, cs) in enumerate(info):
                self.mm(pB_[:, q * 128:(q + 1) * 128], qT[:, cs], Sb[:, q, :], True, True, [B_qT, B_Sb], [B_pB])
            for q, (d, hvl, ch, col, cs) in enumerate(info):
                self.stt("dve", rp[:, q, :], pA[:, q * 128:(q + 1) * 128], NEG[:, ch, col:col + 1], vtok[:, hvl, ch, :], ALU.mult, ALU.add,
                         [B_pA, B_NEG, B_vtok], [B_rp])
            for q, (d, hvl, ch, col, cs) in enumerate(info):
                self.mm(pC[:, q * 128:(q + 1) * 128], AT[:, ch, q, :], rp[:, q, :], True, True, [B_AT, B_rp], [B_pC])
            for q, (d, hvl, ch, col, cs) in enumerate(info):
                self.act(vn[:, q, :], pC[:, q * 128:(q + 1) * 128], AF.Identity, [B_pC, B_Bt], [B_vn], scale=Bt[:, ch, col:col + 1])
                self.ts("dve", vd[:, q, :], pC[:, q * 128:(q + 1) * 128], BDL[:, ch, col:col + 1], None, ALU.mult, None, [B_pC, B_BDL], [B_vd])
            for q, (d, hvl, ch, col, cs) in enumerate(info):
                self.mm(pD[:, q * 128:(q + 1) * 128], ktok[:, ch, :], vd[:, q, :], True, True, [B_ktok, B_vd], [B_pD])
            for q, (d, hvl, ch, col, cs) in enumerate(info):
                self.mm(pE[:, q * 128:(q + 1) * 128], QKD[:, ch, q, :], vn[:, q, :], True, True, [B_QKD, B_vn], [B_pE])
            for q, (d, hvl, ch, col, cs) in enumerate(info):
                self.stt("dve", St[:, q, :], St[:, q, :], EGL[:, ch, col:col + 1], pD[:, q * 128:(q + 1) * 128], ALU.mult, ALU.add,
                         [B_S, B_EGL, B_pD], [B_S])
            self.cp("act", Sb[:], St[:], [B_S], [B_Sb])
            for q, (d, hvl, ch, col, cs) in enumerate(info):
                self.stt("dve", O[:, hvl, ch, :], pB_[:, q * 128:(q + 1) * 128], EG[:, ch, col:col + 1], O[:, hvl, ch, :], ALU.mult, ALU.add,
                         [B_pB, B_EG, B_O], [B_O])
                self.tt("dve", O[:, hvl, ch, :], O[:, hvl, ch, :], pE[:, q * 128:(q + 1) * 128], ALU.add, [B_pE, B_O], [B_O])
        S.fence()
        for hvl in (range(2) if "C" in PH else []):
            hv = 2 * hq + hvl
            S.dma("sp", zs[:], ZSd[hv], [B_dn], [B_zs])
            Ov = O[:, hvl]
            self.tt("pool", sq[:], Ov, Ov, ALU.mult, [B_O], [B_sq])
            S.op("dve", lambda e: e.tensor_reduce(out=ss[:], in_=sq[:], axis=AX.X, op=ALU.add), [B_sq], [B_ss])
            self.act(ss[:], ss[:], AF.Sqrt, [B_ss], [B_ss], bias=RMS_EPS, scale=1.0 / 128.0)
            S.op("dve", lambda e: e.reciprocal(out=ss[:], in_=ss[:]), [B_ss], [B_ss])
            self.tt("dve", sq[:], Ov, ss[:].unsqueeze(2).broadcast_to([128, NT, 128]), ALU.mult, [B_O, B_ss], [B_sq])
            self.tt("pool", sq[:], sq[:], nw[:].unsqueeze(1).broadcast_to([128, NT, 128]), ALU.mult, [B_sq, B_nw], [B_sq])
            self.tt("dve", sq[:], sq[:], zs[:], ALU.mult, [B_sq, B_zs], [B_sq])
            for tq in range(0, NT, 4):
                nt_ = min(4, NT - tq)
                pt, B_pt = P[4 + (tq // 4) % 2], PB[4 + (tq // 4) % 2]
                for i in range(nt_):
                    self.tr(pt[:, i * 128:(i + 1) * 128], sq[:, tq + i, :], [B_sq], [B_pt])
                self.cp("act", ogT[:, tq * 128:(tq + nt_) * 128], pt[:, 0:nt_ * 128], [B_pt], [B_ogT])
            S.dma("sp", OGd[hv], ogT[:], [B_ogT], [B_og])


def dn_out(self, OGd, B_og, w_o, YT, YTB):
    S = self.S
    self.stage()
    P, PB = self.ps, self.psB
    grot = self.rot(2, [128, 32, 512], BF16)
    worot = self.rot(3, [128, 32, 128], BF16)
    yrot = self.rot(3, [128, 512], F32)
    OGv = OGd.rearrange("h p t -> p h t")
    wov = w_o.rearrange("(k p) n -> p k n", p=128)
    pi = 0
    for g, (t0, n) in enumerate(GROUPS):
        og, B_g = grot.next()
        S.dma("sp", og[:, :, 0:n], OGv[:, :, t0:t0 + n], [B_og], [B_g])
        for dc in range(NCH):
            wo, B_wo = worot.next()
            S.dma("pool", wo[:], wov[:, :, dc * 128:(dc + 1) * 128], (), [B_wo])
            pt, B_pt = P[pi], PB[pi]
            pi = (pi + 1) % 4
            for k in range(32):
                self.mm(pt[:, 0:n], wo[:, k, :], og[:, k, 0:n], k == 0, k == 31, [B_wo, B_g], [B_pt])
            y, B_y = yrot.next()
            self.cp("act" if dc % 2 else "dve", y[:, 0:n], pt[:, 0:n], [B_pt], [B_y])
            S.dma("sp", YT[dc][:, t0:t0 + n], y[:, 0:n], [B_y], [YTB[g]])


K.dn_proj = dn_proj
K.dn_core = dn_core
K.dn_out = dn_out


def build_program():
    k = K({})
    I = {}
    I["c"] = k.ext_in("c", [16, 128]); I["c_ctx"] = k.ext_in("c_ctx", [16, 128])
    I["x"] = k.ext_in("x", [SEQ, D]); I["ctx"] = k.ext_in("ctx", [CTX, D])
    I["w_mod"] = k.ext_in("w_mod", [DEPTH, D, 6 * D]); I["b_mod"] = k.ext_in("b_mod", [DEPTH, 6 * D])
    I["ln_g"] = k.ext_in("ln_g", [DEPTH, 2, D]); I["ln_b"] = k.ext_in("ln_b", [DEPTH, 2, D])
    I["att_w_qkv"] = k.ext_in("att_w_qkv", [2, D, 2560]); I["att_b_qkv"] = k.ext_in("att_b_qkv", [2, 1, 2560])
    I["att_sink"] = k.ext_in("att_sink", [2, 1, 32]); I["att_w_o"] = k.ext_in("att_w_o", [2, D, D]); I["att_b_o"] = k.ext_in("att_b_o", [2, 1, D])
    I["cosT"] = k.ext_in("cosT", [128, SEQ]); I["sinT"] = k.ext_in("sinT", [128, SEQ]); I["pmat"] = k.ext_in("pmat", [128, 128])
    I["dn_w_in"] = k.ext_in("dn_w_in", [2, D, 12416]); I["dn_conv_w"] = k.ext_in("dn_conv_w", [2, 5, 8192])
    I["dn_a_log"] = k.ext_in("dn_a_log", [2, 1, 64]); I["dn_dt_bias"] = k.ext_in("dn_dt_bias", [2, 1, 64])
    I["dn_norm_w"] = k.ext_in("dn_norm_w", [2, 1, 128]); I["dn_w_o"] = k.ext_in("dn_w_o", [2, 4096, D])
    I["moe_w_r"] = k.ext_in("moe_w_r", [DEPTH, D, NE]); I["moe_b_r"] = k.ext_in("moe_b_r", [DEPTH, 1, NE])
    I["moe_wgu"] = k.ext_in("moe_wgu", [DEPTH, NE, 6, 128, 16, 256]); I["moe_bgu"] = k.ext_in("moe_bgu", [DEPTH, NE, 1536])
    I["moe_wdn"] = k.ext_in("moe_wdn", [DEPTH, NE, DEXP, D]); I["moe_bdn"] = k.ext_in("moe_bdn", [DEPTH, NE, D])
    out = k.ext_out("out", [SEQ, D])
    HT = k.dram("HT", [NCH, 128, T], F32); YT = k.dram("YT", [NCH, 128, T], F32); UT = k.dram("UT", [NCH, 128, T], BF16)
    GT = k.dram("GT", [NE, T], F32)
    QTd = k.dram("QTd", [NCH, 128, T], BF16); KTd = k.dram("KTd", [4, 128, T], BF16); VAd = k.dram("VAd", [128, NT * 4 * 65], BF16)
    QTn = k.dram("QTn", [16, 128, T], BF16); KTn = k.dram("KTn", [16, 128, T], BF16); KTOK = k.dram("KTOK", [16, 128, NT, 128], BF16)
    VTOK = k.dram("VTOK", [32, 128, NT, 128], F32); ZSd = k.dram("ZSd", [32, 128, NT, 128], F32); GBd = k.dram("GBd", [2, 128, NT, 64], F32)
    OGd = k.dram("OGd", [32, 128, T], BF16)
    HTB = [Buf() for _ in GROUPS]; YTB = [Buf() for _ in GROUPS]; UTB = [Buf() for _ in GROUPS]; GTB = [Buf() for _ in GROUPS]
    B_att = Buf(); B_dn = Buf(); B_og = Buf()
    k.setup()
    k.prologue_mod(I["c"], I["c_ctx"], I["w_mod"], I["b_mod"], I["ln_g"], I["ln_b"])
    k.prologue_x(I["x"], I["ctx"], HT, HTB)
    k.ln_mod(0, HT, HTB, None, None, None, None, (0, 1), UT, UTB)
    for i in range(DEPTH):
        j = i // 2
        if i % 2 == 0:
            k.attn_proj(UT, UTB, I["att_w_qkv"][j], I["att_b_qkv"][j], I["cosT"], I["sinT"], I["pmat"], QTd, KTd, VAd, B_att)
            k.attn_core(QTd, KTd, VAd, B_att, I["att_sink"][j], I["att_w_o"][j], I["att_b_o"][j], YT, YTB)
        else:
            k.dn_proj(UT, UTB, I["dn_w_in"][j], I["dn_conv_w"][j], I["dn_a_log"][j], I["dn_dt_bias"][j], QTn, KTn, KTOK, VTOK, ZSd, GBd, B_dn)
            k.dn_core(QTn, KTn, KTOK, VTOK, ZSd, GBd, B_dn, I["dn_norm_w"][j], OGd, B_og)
            k.dn_out(OGd, B_og, I["dn_w_o"][j], YT, YTB)
        k.ln_mod(i, HT, HTB, YT, YTB, 2, 0, (3, 4), UT, UTB, router=(I["moe_w_r"][i], I["moe_b_r"][i], GT, GTB))
        k.moe(i, UT, UTB, GT, GTB, YT, YTB, I["moe_wgu"][i], I["moe_bgu"][i], I["moe_wdn"][i], I["moe_bdn"][i], skip_ctx=(i == DEPTH - 1))
        k.ln_mod(i, HT, HTB, YT, YTB, 5, 1, (0, 1), UT, UTB, l_mod=min(i + 1, DEPTH - 1))
    B_out = k.epilogue(HT, HTB, out)
    k.finish([B_out])
    return k


_PROG = None


def kernel(x, c, ctx, c_ctx, w_mod, b_mod, ln_g, ln_b, att_w_qkv, att_b_qkv, att_sink, att_w_o, att_b_o,
           dn_w_in, dn_conv_w, dn_a_log, dn_dt_bias, dn_norm_w, dn_w_o, moe_w_router, moe_b_router,
           moe_w_gu, moe_b_gu, moe_w_down, moe_b_down):
    global _PROG
    if _PROG is None:
        _PROG = build_program()
    k = _PROG
    f = lambda a: np.ascontiguousarray(np.asarray(a, dtype=np.float32))
    x, c, ctx, c_ctx = f(x), f(c), f(ctx), f(c_ctx)
    B = x.shape[0]
    cosT, sinT, pmat = rope_tables()
    wg = np.asarray(moe_w_gu, dtype=np.float32)
    g = wg[..., 0::2].reshape(DEPTH, NE, 16, 128, 6, 128)
    u = wg[..., 1::2].reshape(DEPTH, NE, 16, 128, 6, 128)
    wgu = np.ascontiguousarray(np.concatenate([g, u], axis=-1).transpose(0, 1, 4, 3, 2, 5))
    del g, u, wg
    bg = np.asarray(moe_b_gu, dtype=np.float32)
    bgu = np.ascontiguousarray(np.concatenate([bg[..., 0::2], bg[..., 1::2]], axis=-1))
    shared = {
        "c_ctx": c_ctx.reshape(16, 128), "w_mod": f(w_mod), "b_mod": f(b_mod), "ln_g": f(ln_g), "ln_b": f(ln_b),
        "att_w_qkv": f(att_w_qkv), "att_b_qkv": f(att_b_qkv).reshape(2, 1, 2560), "att_sink": f(att_sink).reshape(2, 1, 32),
        "att_w_o": f(att_w_o), "att_b_o": f(att_b_o).reshape(2, 1, D), "cosT": cosT, "sinT": sinT, "pmat": pmat,
        "dn_w_in": f(dn_w_in), "dn_conv_w": f(dn_conv_w), "dn_a_log": f(dn_a_log).reshape(2, 1, 64),
        "dn_dt_bias": f(dn_dt_bias).reshape(2, 1, 64), "dn_norm_w": f(dn_norm_w).reshape(2, 1, 128), "dn_w_o": f(dn_w_o),
        "moe_w_r": f(moe_w_router), "moe_b_r": f(moe_b_router).reshape(DEPTH, 1, NE), "moe_wgu": wgu, "moe_bgu": bgu,
        "moe_wdn": f(moe_w_down), "moe_bdn": f(moe_b_down),
    }
    in_maps = []
    for b in range(B):
        m = dict(shared)
        m["x"] = x[b]
        m["ctx"] = ctx[b]
        m["c"] = c[b].reshape(16, 128)
        in_maps.append(m)
    res = run_bass_kernel_spmd(k.nc, in_maps, core_ids=list(range(B)))
    return np.stack([np.asarray(r["out"], dtype=np.float32) for r in res.results], axis=0)
```

```python
import contextlib
import numpy as np
import concourse.bass as bass
import concourse.mybir as mybir
from concourse.bass_utils import run_bass_kernel_spmd

F32 = mybir.dt.float32
BF16 = mybir.dt.bfloat16
ALU = mybir.AluOpType
AF = mybir.ActivationFunctionType
AX = mybir.AxisListType

D = 2048
NCH = 16
CTX = 256
SEQ = 2048
T = CTX + SEQ
NT = T // 128
DEPTH = 4
ALPHA = (2.0 * DEPTH) ** 0.25
LN_EPS = 1e-5
GROUPS = [(0, 256), (256, 512), (768, 512), (1280, 512), (1792, 512)]
NE = 32
DEXP = 768

EPOCH = 30000
N_DMA_SEMS = 12


class Buf:
    __slots__ = ("name", "lw", "rd", "excl")

    def __init__(self, name="", excl=False):
        self.name = name
        self.lw = None
        self.rd = {}
        self.excl = excl


class Sched:
    def __init__(self, nc):
        self.nc = nc
        self.ops = {"pe": [], "act": [], "dve": [], "pool": [], "sp": []}
        self.cnt = {e: 0 for e in self.ops}
        self.epoch = {e: 0 for e in self.ops}
        self.seen = {e: {} for e in self.ops}
        self.semkeys = set()
        self.dma_tot = {}
        self.dma_rr = {"sp": 0, "pool": 0, "act": 0}
        self.n_ops = 0

    def _deps(self, eng, reads, writes):
        deps = {}
        for b in reads:
            if b.lw is not None and deps.get(b.lw[0], 0) < b.lw[1]:
                deps[b.lw[0]] = b.lw[1]
            if b.excl:
                for k, v in b.rd.items():
                    if k[1] != eng and deps.get(k, 0) < v:
                        deps[k] = v
        for b in writes:
            if b.lw is not None and deps.get(b.lw[0], 0) < b.lw[1]:
                deps[b.lw[0]] = b.lw[1]
            for k, v in b.rd.items():
                if deps.get(k, 0) < v:
                    deps[k] = v
        out = []
        seen = self.seen[eng]
        for k, v in deps.items():
            if eng == "pe" and k[0] == "e" and k[1] == "pe":
                continue
            if seen.get(k, 0) >= v:
                continue
            seen[k] = v
            out.append((k, v))
        return out

    def op(self, eng, fn, reads=(), writes=()):
        if self.cnt[eng] >= EPOCH:
            self.epoch[eng] += 1
            self.cnt[eng] = 0
        waits = self._deps(eng, reads, writes)
        key = ("e", eng, self.epoch[eng])
        self.semkeys.add(key)
        self.cnt[eng] += 1
        val = self.cnt[eng]
        self.ops[eng].append((waits, fn, key, 1))
        for b in writes:
            b.lw = (key, val)
            b.rd = {}
        for b in reads:
            if b.rd.get(key, 0) < val:
                b.rd[key] = val
        self.n_ops += 1

    def dma(self, q, out_ap, in_ap, reads=(), writes=()):
        i = self.dma_rr[q]
        self.dma_rr[q] = (i + 1) % N_DMA_SEMS
        key = ("d", q, i)
        self.semkeys.add(key)
        prev = self.dma_tot.get(key, 0)
        waits = self._deps(q, reads, writes)
        if prev > 0 and self.seen[q].get(key, 0) < prev:
            self.seen[q][key] = prev
            waits.append((key, prev))
        tot = prev + 16
        self.dma_tot[key] = tot

        def fn(e, out_ap=out_ap, in_ap=in_ap):
            return e.dma_start(out=out_ap, in_=in_ap)
        self.ops[q].append((waits, fn, key, 16))
        for b in writes:
            b.lw = (key, tot)
            b.rd = {}
        for b in reads:
            if b.rd.get(key, 0) < tot:
                b.rd[key] = tot
        self.n_ops += 1

    def fence(self):
        allk = []
        for x in self.ops:
            if self.cnt[x] > 0:
                allk.append((("e", x, self.epoch[x]), self.cnt[x]))
        for k, v in self.dma_tot.items():
            allk.append((k, v))
        for e in self.ops:
            waits = []
            for k, v in allk:
                if self.seen[e].get(k, 0) < v:
                    self.seen[e][k] = v
                    waits.append((k, v))
            if waits:
                self.ops[e].append((waits, None, None, 0))

    def emit(self):
        nc = self.nc
        with contextlib.ExitStack() as st:
            sems = {}
            for k in sorted(self.semkeys):
                sems[k] = st.enter_context(nc.semaphore("s_" + "_".join(str(x) for x in k)))
            block = st.enter_context(nc.Block())

            def run(eng_name):
                def body(e):
                    for waits, fn, key, inc in self.ops[eng_name]:
                        for (k, v) in waits:
                            e.wait_ge(sems[k], v)
                        if fn is not None:
                            fn(e).then_inc(sems[key], inc)
                return body
            block.sync(run("sp"))
            block.tensor(run("pe"))
            block.scalar(run("act"))
            block.vector(run("dve"))
            block.gpsimd(run("pool"))


class Rot:
    def __init__(self, items):
        self.items = items
        self.i = 0

    def next(self):
        it = self.items[self.i]
        self.i = (self.i + 1) % len(self.items)
        return it


class K:
    def __init__(self, cfg):
        self.cfg = cfg
        self.nc = bass.Bass("TRN2", target_bir_lowering=False)
        self.S = Sched(self.nc)
        self.uid = 0
        self.base = 16640
        self.off = 16640
        self.LIMIT = 229000

    def sb(self, shape, dtype, persistent=False):
        self.uid += 1
        esz = 2 if dtype == BF16 else 4
        n = 1
        for s in shape[1:]:
            n *= s
        nbytes = (n * esz + 63) // 64 * 64
        t = self.nc.alloc_sbuf_tensor_at("sb%d" % self.uid, list(shape), dtype, offset=self.off)
        self.off += nbytes
        assert self.off <= self.LIMIT, ("SBUF overflow", self.off)
        if persistent:
            self.base = self.off
        return t

    def sbb(self, shape, dtype):
        return self.sb(shape, dtype), Buf()

    def rot(self, n, shape, dtype):
        return Rot([self.sbb(shape, dtype) for _ in range(n)])

    def stage(self):
        self.S.fence()
        self.off = self.base

    def dram(self, name, shape, dtype):
        return self.nc.dram_tensor(name, list(shape), dtype).ap()

    def ext_in(self, name, shape, dtype=F32):
        return self.nc.dram_tensor(name, list(shape), dtype, kind="ExternalInput").ap()

    def ext_out(self, name, shape, dtype=F32):
        return self.nc.dram_tensor(name, list(shape), dtype, kind="ExternalOutput").ap()

    def mm(self, out, lhsT, rhs, start, stop, reads, writes):
        self.S.op("pe", lambda e: e.matmul(out, lhsT, rhs, start=start, stop=stop), reads, writes)

    def tr(self, out, in_, reads, writes):
        ident = self.ident
        n = in_.shape[0]
        self.S.op("pe", lambda e: e.transpose(out, in_, ident[0:n, 0:n]), list(reads) + [self.B_const], writes)

    def act(self, out, in_, func, reads, writes, bias=0.0, scale=1.0):
        self.S.op("act", lambda e: e.activation(out=out, in_=in_, func=func, bias=bias, scale=scale), reads, writes)

    def ts(self, eng, out, in0, s1, s2, op0, op1, reads, writes):
        if op1 is None:
            self.S.op(eng, lambda e: e.tensor_scalar(out=out, in0=in0, scalar1=s1, scalar2=None, op0=op0), reads, writes)
        else:
            self.S.op(eng, lambda e: e.tensor_scalar(out=out, in0=in0, scalar1=s1, scalar2=s2, op0=op0, op1=op1), reads, writes)

    def tt(self, eng, out, in0, in1, op, reads, writes):
        self.S.op(eng, lambda e: e.tensor_tensor(out=out, in0=in0, in1=in1, op=op), reads, writes)

    def stt(self, eng, out, in0, scalar, in1, op0, op1, reads, writes):
        self.S.op(eng, lambda e: e.scalar_tensor_tensor(out=out, in0=in0, scalar=scalar, in1=in1, op0=op0, op1=op1), reads, writes)

    def cp(self, eng, out, in_, reads, writes):
        if eng == "act":
            self.S.op("act", lambda e: e.copy(out=out, in_=in_), reads, writes)
        else:
            self.S.op(eng, lambda e: e.tensor_copy(out=out, in_=in_), reads, writes)

    def setup(self):
        nc, S = self.nc, self.S
        self.ps = [nc.alloc_psum_tensor("ps%d" % i, [128, 512], F32) for i in range(8)]
        self.psB = [Buf("ps%d" % i, excl=True) for i in range(8)]
        self.B_const = Buf("const")
        self.ident = self.sb([128, 128], F32, True)
        self.ones = self.sb([128, 128], F32, True)
        ident, ones = self.ident, self.ones
        self.onesrow = self.sb([1, 512], F32, True)
        onesrow = self.onesrow
        S.op("pool", lambda e: e.memset(onesrow[:], 1.0), (), [self.B_const])
        S.op("pool", lambda e: e.memset(ones[:], 1.0), (), [self.B_const])
        S.op("pool", lambda e: e.memset(ident[:], 1.0), (), [self.B_const])
        S.op("pool", lambda e: e.affine_select(out=ident[:], in_=ident[:], pattern=[[-1, 128]],
                                               compare_op=ALU.is_equal, fill=0.0, base=0, channel_multiplier=1),
             [self.B_const], [self.B_const])
        self.m_le = self.sb([128, 128], F32, True)
        self.m_ge = self.sb([128, 128], F32, True)
        self.m_lt = self.sb([128, 128], F32, True)
        self.m_gt = self.sb([128, 128], F32, True)
        for m, step, cm, base in ((self.m_le, 1, -1, 0), (self.m_ge, -1, 1, 0), (self.m_lt, 1, -1, -1), (self.m_gt, -1, 1, -1)):
            S.op("pool", lambda e, m=m: e.memset(m[:], 1.0), (), [self.B_const])
            S.op("pool", lambda e, m=m, step=step, cm=cm, base=base: e.affine_select(
                out=m[:], in_=m[:], pattern=[[step, 128]], compare_op=ALU.is_ge, fill=0.0, base=base, channel_multiplier=cm),
                 [self.B_const], [self.B_const])
        self.MT = self.sb([128, DEPTH, 96, 2], F32, True)
        self.B_MT = Buf("MT")
        self.lnG = self.sb([128, 128], F32, True)
        self.lnB = self.sb([128, 128], F32, True)
        self.B_ln = Buf("ln")

    def prologue_mod(self, c_in, cctx_in, w_mod, b_mod, ln_g, ln_b):
        S = self.S
        self.stage()
        P = self.ps
        cs, B_cs = self.sbb([32, 128], F32)
        csT, B_csT = self.sbb([128, 32], F32)
        bm, B_bm = self.sbb([128, 3, 128], F32)
        bmT, B_bmT = self.sbb([128, 384], F32)
        lt, B_lt = self.sbb([128, 2, 128], F32)
        S.dma("sp", cs[0:16, :], c_in, (), [B_cs])
        S.dma("sp", cs[16:32, :], cctx_in, (), [B_cs])
        self.act(cs[:], cs[:], AF.Silu, [B_cs], [B_cs])
        self.tr(P[0][:, 0:32], cs[:], [B_cs], [self.psB[0]])
        self.cp("dve", csT[:], P[0][:, 0:32], [self.psB[0]], [B_csT])
        bview = b_mod.rearrange("l (f p) -> (l f) p", p=128)
        for i in range(3):
            S.dma("sp", bm[:, i, :], bview[i * 128:(i + 1) * 128, :], (), [B_bm])
        for i in range(3):
            self.tr(P[1][:, i * 128:(i + 1) * 128], bm[:, i, :], [B_bm], [self.psB[1]])
        self.cp("dve", bmT[:], P[1][:, 0:384], [self.psB[1]], [B_bmT])
        S.dma("sp", lt[:, 0, :], ln_g.rearrange("l s (k p) -> (l s k) p", p=128), (), [B_lt])
        S.dma("sp", lt[:, 1, :], ln_b.rearrange("l s (k p) -> (l s k) p", p=128), (), [B_lt])
        self.tr(P[2][:, 0:128], lt[:, 0, :], [B_lt], [self.psB[2]])
        self.tr(P[2][:, 128:256], lt[:, 1, :], [B_lt], [self.psB[2]])
        self.cp("dve", self.lnG[:], P[2][:, 0:128], [self.psB[2]], [self.B_ln])
        self.cp("dve", self.lnB[:], P[2][:, 128:256], [self.psB[2]], [self.B_ln])
        wrot = self.rot(3, [128, 16, 512], F32)
        csv = csT[:].rearrange("p (j k) -> p j k", j=2)
        pi = 0
        rowrot = self.rot(2, [2, 512], F32)
        for l in range(DEPTH):
            wv = w_mod[l].rearrange("(k p) n -> p k n", p=128)
            for nb in range(24):
                w, B_w = wrot.next()
                S.dma("sp", w[:], wv[:, :, nb * 512:(nb + 1) * 512], (), [B_w])
                pr_, B_pr = P[3 + pi], self.psB[3 + pi]
                pt, B_pt = P[5 + pi], self.psB[5 + pi]
                pi = (pi + 1) % 2
                for k in range(16):
                    self.mm(pr_[0:2, :], csv[:, :, k], w[:, k, :], k == 0, k == 15, [B_w, B_csT], [B_pr])
                row, B_row = rowrot.next()
                self.cp("act", row[:], pr_[0:2, :], [B_pr], [B_row])
                for f in range(4):
                    self.tr(pt[:, f * 2:f * 2 + 2], row[0:2, f * 128:(f + 1) * 128], [B_row], [B_pt])
                a = l * 96 + nb * 4
                self.tt("dve", self.MT[:, l, nb * 4:nb * 4 + 4, :], pt[:, 0:8].rearrange("p (f j) -> p f j", j=2),
                        bmT[:, a:a + 4].unsqueeze(2).broadcast_to([128, 4, 2]), ALU.add, [B_pt, B_bmT], [self.B_MT])
        for l in range(DEPTH):
            for sec in (1, 4):
                v = self.MT[:, l, sec * 16:(sec + 1) * 16, :]
                self.ts("dve", v, v, 1.0, None, ALU.add, None, [self.B_MT], [self.B_MT])
            for sec in (2, 5):
                v = self.MT[:, l, sec * 16:(sec + 1) * 16, :]
                self.ts("dve", v, v, 1.0 / ALPHA, None, ALU.mult, None, [self.B_MT], [self.B_MT])

    def prologue_x(self, x_in, ctx_in, HT, HTB):
        S = self.S
        self.stage()
        P = self.ps
        xrot = self.rot(2, [128, D], F32)
        srot = self.rot(2, [128, NCH, 128], F32)
        HTv = HT.rearrange("c p t -> p c t")
        pi = 0
        for tt in range(NT):
            xt, B_x = xrot.next()
            src = ctx_in[tt * 128:(tt + 1) * 128, :] if tt < 2 else x_in[(tt - 2) * 128:(tt - 1) * 128, :]
            S.dma("sp", xt[:], src, (), [B_x])
            st, B_st = srot.next()
            for cq in range(4):
                pt, B_pt = P[pi], self.psB[pi]
                pi = (pi + 1) % 8
                for j in range(4):
                    c = cq * 4 + j
                    self.tr(pt[:, j * 128:(j + 1) * 128], xt[:, c * 128:(c + 1) * 128], [B_x], [B_pt])
                self.cp("act" if cq % 2 else "dve", st[:, cq * 4:(cq + 1) * 4, :],
                        pt[:].rearrange("p (j t) -> p j t", j=4), [B_pt], [B_st])
            g = self.group_of(tt * 128)
            S.dma("sp", HTv[:, :, tt * 128:(tt + 1) * 128], st[:], [B_st], [HTB[g]])

    @staticmethod
    def group_of(t):
        for g, (s, n) in enumerate(GROUPS):
            if s <= t < s + n:
                return g
        raise ValueError

    def ln_mod(self, l, HT, HTB, YT, YTB, gate_sec, ln_idx, mod_sec, UT, UTB, router=None, l_mod=None):
        S = self.S
        self.stage()
        P, PB = self.ps, self.psB
        has_ln = YT is not None
        zrot = Rot([(self.sb([128, NCH, 512], F32), [Buf() for _ in range(NCH)]) for _ in range(2)])
        yrot = self.rot(4, [128, 512], F32)
        qrot = self.rot(2, [128, 512], F32)
        urot = self.rot(2, [128, NCH, 512], BF16)
        ufrot = self.rot(2, [128, 512], F32)
        mean, B_mean = self.sbb([128, 512], F32)
        msq, B_msq = self.sbb([128, 512], F32)
        rstd, B_rstd = self.sbb([128, 512], F32)
        HTv = HT.rearrange("c p t -> p c t")
        UTv = UT.rearrange("c p t -> p c t")
        if router is not None:
            w_r, b_r, GT, GTB = router
            wr, B_wr = self.sbb([128, NCH, NE], F32)
            br, B_br = self.sbb([1, NE], F32)
            S.dma("sp", wr[:], w_r.rearrange("(k p) e -> p k e", p=128), (), [B_wr])
            S.dma("sp", br[:], b_r, (), [B_br])
            lg, B_lg = self.sbb([128, 4, NE], F32)
            ex, B_ex = self.sbb([128, 4, NE], F32)
            mk, B_mk = self.sbb([128, 4, NE], F32)
            m8, B_m8 = self.sbb([128, 4, 8], F32)
            sm, B_sm = self.sbb([128, 4, 4], F32)
            gts, B_gts = self.sbb([NE, 512], F32)
        sh_sec, sc_sec = mod_sec
        lm = l if l_mod is None else l_mod
        eps = LN_EPS / (ALPHA * ALPHA)
        for g, (t0, n) in enumerate(GROUPS):
            j = 1 if g == 0 else 0
            z, BZ = zrot.next()
            u, B_u = urot.next()
            S.dma("sp", z[:, :, 0:n], HTv[:, :, t0:t0 + n], [HTB[g]], BZ)
            if has_ln:
                for c in range(NCH):
                    B_z = BZ[c]
                    y, B_y = yrot.next()
                    S.dma("sp", y[:, 0:n], YT[c][:, t0:t0 + n], [YTB[g]], [B_y])
                    self.stt("dve", z[:, c, 0:n], y[:, 0:n], self.MT[:, l, gate_sec * 16 + c, j:j + 1], z[:, c, 0:n],
                             ALU.mult, ALU.add, [B_y, B_z, self.B_MT], [B_z])
                    q, B_q = qrot.next()
                    self.act(q[:, 0:n], z[:, c, 0:n], AF.Square, [B_z], [B_q])
                    self.mm(P[0][:, 0:n], self.ones[:], z[:, c, 0:n], c == 0, c == NCH - 1, [B_z, self.B_const], [PB[0]])
                    self.mm(P[1][:, 0:n], self.ones[:], q[:, 0:n], c == 0, c == NCH - 1, [B_q, self.B_const], [PB[1]])
                self.ts("dve", mean[:, 0:n], P[0][:, 0:n], 1.0 / D, None, ALU.mult, None, [PB[0]], [B_mean])
                self.tt("pool", msq[:, 0:n], mean[:, 0:n], mean[:, 0:n], ALU.mult, [B_mean], [B_msq])
                self.stt("dve", rstd[:, 0:n], P[1][:, 0:n], 1.0 / D, msq[:, 0:n], ALU.mult, ALU.subtract, [PB[1], B_msq], [B_rstd])
                self.act(rstd[:, 0:n], rstd[:, 0:n], AF.Sqrt, [B_rstd], [B_rstd], bias=eps)
                S.op("dve", lambda e, n=n: e.reciprocal(out=rstd[:, 0:n], in_=rstd[:, 0:n]), [B_rstd], [B_rstd])
            for c in range(NCH):
                B_z = BZ[c]
                zc = z[:, c, 0:n]
                if has_ln:
                    self.tt("pool", zc, zc, mean[:, 0:n], ALU.subtract, [B_z, B_mean], [B_z])
                    self.tt("dve", zc, zc, rstd[:, 0:n], ALU.mult, [B_z, B_rstd], [B_z])
                    col = l * 32 + ln_idx * 16 + c
                    self.act(zc, zc, AF.Identity, [B_z, self.B_ln], [B_z], bias=self.lnB[:, col:col + 1], scale=self.lnG[:, col:col + 1])
                sc = self.MT[:, lm, sc_sec * 16 + c, j:j + 1]
                sh = self.MT[:, lm, sh_sec * 16 + c, j:j + 1]
                self.act(u[:, c, 0:n], zc, AF.Identity, [B_z, self.B_MT], [B_u], bias=sh, scale=sc)
                if router is not None:
                    uf, B_uf = ufrot.next()
                    self.ts("dve", uf[:, 0:n], zc, sc, sh, ALU.mult, ALU.add, [B_z, self.B_MT], [B_uf])
                    for tl in range(n // 128):
                        self.mm(P[2 + tl][:, 0:NE], uf[:, tl * 128:(tl + 1) * 128], wr[:, c, :],
                                c == 0, False, [B_uf, B_wr], [PB[2 + tl]])
            if has_ln:
                S.dma("sp", HTv[:, :, t0:t0 + n], z[:, :, 0:n], BZ, [HTB[g]])
            S.dma("sp", UTv[:, :, t0:t0 + n], u[:, :, 0:n], [B_u], [UTB[g]])
            if router is not None:
                ntl = n // 128
                for tl in range(ntl):
                    self.mm(P[2 + tl][:, 0:NE], self.ones[0:1, :], br[:], False, True, [B_br, self.B_const], [PB[2 + tl]])
                    self.cp("dve", lg[:, tl, :], P[2 + tl][:, 0:NE], [PB[2 + tl]], [B_lg])
                for tl in range(ntl):
                    S.op("dve", lambda e, tl=tl: e.max(out=m8[:, tl, :], in_=lg[:, tl, :]), [B_lg], [B_m8])
                self.ts("dve", sm[:, 0:ntl, 0:1], m8[:, 0:ntl, 0:1], -1.0, None, ALU.mult, None, [B_m8], [B_sm])
                for tl in range(ntl):
                    self.act(ex[:, tl, :], lg[:, tl, :], AF.Exp, [B_lg, B_sm], [B_ex], bias=sm[:, tl, 0:1])
                    self.ts("dve", mk[:, tl, :], lg[:, tl, :], m8[:, tl, 3:4], None, ALU.is_ge, None, [B_lg, B_m8], [B_mk])
                self.tt("dve", ex[:, 0:ntl, :], ex[:, 0:ntl, :], mk[:, 0:ntl, :], ALU.mult, [B_ex, B_mk], [B_ex])
                S.op("dve", lambda e, ntl=ntl: e.tensor_reduce(out=sm[:, 0:ntl, 1:2], in_=ex[:, 0:ntl, :], axis=AX.X, op=ALU.add),
                     [B_ex], [B_sm])
                S.op("dve", lambda e, ntl=ntl: e.reciprocal(out=sm[:, 0:ntl, 2:3], in_=sm[:, 0:ntl, 1:2]), [B_sm], [B_sm])
                for tl in range(ntl):
                    self.ts("dve", ex[:, tl, :], ex[:, tl, :], sm[:, tl, 2:3], None, ALU.mult, None, [B_ex, B_sm], [B_ex])
                    self.tr(P[6][0:NE, tl * 128:(tl + 1) * 128], ex[:, tl, :], [B_ex], [PB[6]])
                self.cp("dve", gts[:, 0:n], P[6][0:NE, 0:n], [PB[6]], [B_gts])
                S.dma("sp", GT[:, t0:t0 + n], gts[:, 0:n], [B_gts], [GTB[g]])

    def moe(self, l, UT, UTB, GT, GTB, YT, YTB, wgu, bgu, wdn, bdn, skip_ctx=False):
        S = self.S
        self.stage()
        P, PB = self.ps, self.psB
        TG = 768
        SG = 384
        ug, B_ug = self.sbb([128, NCH, TG], BF16)
        acc, B_acc = self.sbb([128, NCH, TG], F32)
        gt, B_gt = self.sbb([NE, TG], F32)
        sel, B_sel = self.sbb([NE, NE, 128], F32)
        bdt, B_bdt = self.sbb([NE, D], F32)
        bgT, B_bgT = self.sbb([128, 12, NE], F32)
        bgl, B_bgl = self.sbb([NE, 1536], F32)
        wrot = self.rot(3, [128, 16, 256], BF16)
        drot = self.rot(3, [128, 6, 1024], BF16)
        arot = self.rot(2, [128, 6, TG], BF16)
        t1rot = self.rot(2, [128, SG], F32)
        t2rot = self.rot(2, [128, SG], F32)
        t3rot = self.rot(2, [128, SG], F32)
        S.op("dve", lambda e: e.tensor_copy(out=sel[:], in_=self.ident[0:NE, 0:NE].unsqueeze(2).broadcast_to([NE, NE, 128])),
             [self.B_const], [B_sel])
        S.dma("sp", bdt[:], bdn, (), [B_bdt])
        S.dma("sp", bgl[:], bgu, (), [B_bgl])
        for fc in range(12):
            self.tr(P[0][:, fc * NE:(fc + 1) * NE], bgl[:, fc * 128:(fc + 1) * 128], [B_bgl], [PB[0]])
        self.cp("dve", bgT[:], P[0][:, 0:12 * NE].rearrange("p (f e) -> p f e", e=NE), [PB[0]], [B_bgT])
        UTv = UT.rearrange("c p t -> p c t")
        YTv = YT.rearrange("c p t -> p c t")
        pgu = 0
        py = 0
        for tg in range(T // TG):
            t0 = tg * TG
            rg = sorted(set(self.group_of(t0 + a) for a in range(0, TG, 128)))
            S.dma("sp", ug[:], UTv[:, :, t0:t0 + TG], [UTB[g] for g in rg], [B_ug])
            S.dma("sp", gt[:], GT[:, t0:t0 + TG], [GTB[g] for g in rg], [B_gt])
            sgs = [(256, 256), (512, 256)] if (skip_ctx and tg == 0) else [(0, SG), (SG, SG)]
            if skip_ctx and tg == 0:
                S.op("pool", lambda e: e.memset(acc[:, :, 0:256], 0.0), (), [B_acc])
            for ex in range(NE):
                for sg, (so, sn) in enumerate(sgs):
                    self.mm(P[4 + sg][:, 0:sn], sel[:, ex, :], gt[:, so:so + sn], True, True,
                            [B_sel, B_gt], [PB[4 + sg]])
                a, B_a = arot.next()
                for j in range(6):
                    w, B_w = wrot.next()
                    S.dma("pool", w[:], wgu[ex, j], (), [B_w])
                    for sg, (so, sn) in enumerate(sgs):
                        pg, B_pg = P[pgu], PB[pgu]
                        pu, B_pu = P[pgu + 1], PB[pgu + 1]
                        pgu = (pgu + 2) % 4
                        for k in range(16):
                            self.mm(pg[:, 0:sn], w[:, k, 0:128], ug[:, k, so:so + sn], k == 0, k == 15, [B_w, B_ug], [B_pg])
                        for k in range(16):
                            self.mm(pu[:, 0:sn], w[:, k, 128:256], ug[:, k, so:so + sn], k == 0, k == 15, [B_w, B_ug], [B_pu])
                        g1, B_g1 = t1rot.next()
                        u1, B_u1 = t2rot.next()
                        s1, B_s1 = t3rot.next()
                        self.ts("dve", g1[:, 0:sn], pg[:, 0:sn], bgT[:, j, ex:ex + 1], 7.0, ALU.add, ALU.min, [B_pg, B_bgT], [B_g1])
                        self.ts("dve", u1[:, 0:sn], pu[:, 0:sn], bgT[:, 6 + j, ex:ex + 1], 7.0, ALU.add, ALU.min, [B_pu, B_bgT], [B_u1])
                        self.act(s1[:, 0:sn], g1[:, 0:sn], AF.Sigmoid, [B_g1], [B_s1], scale=1.702)
                        self.ts("dve", u1[:, 0:sn], u1[:, 0:sn], -7.0, 1.0, ALU.max, ALU.add, [B_u1], [B_u1])
                        self.tt("dve", g1[:, 0:sn], g1[:, 0:sn], s1[:, 0:sn], ALU.mult, [B_g1, B_s1], [B_g1])
                        self.tt("dve", g1[:, 0:sn], g1[:, 0:sn], u1[:, 0:sn], ALU.mult, [B_g1, B_u1], [B_g1])
                        self.tt("dve", a[:, j, so:so + sn], g1[:, 0:sn], P[4 + sg][:, 0:sn], ALU.mult, [B_g1, PB[4 + sg]], [B_a])
                for dh in range(2):
                    wd, B_wd = drot.next()
                    S.dma("pool", wd[:], wdn[ex].rearrange("(f p) d -> p f d", p=128)[:, :, dh * 1024:(dh + 1) * 1024], (), [B_wd])
                    for dc in range(8):
                        cc = dh * 8 + dc
                        for sg, (so, sn) in enumerate(sgs):
                            pyb, B_py = P[6 + py], PB[6 + py]
                            py = (py + 1) % 2
                            if ex == 0:
                                self.mm(pyb[:, 0:sn], bdt[:, cc * 128:(cc + 1) * 128], gt[:, so:so + sn], True, False,
                                        [B_bdt, B_gt], [B_py])
                            for f in range(6):
                                self.mm(pyb[:, 0:sn], wd[:, f, dc * 128:(dc + 1) * 128], a[:, f, so:so + sn],
                                        (f == 0 and ex != 0), f == 5, [B_wd, B_a], [B_py])
                            av = acc[:, cc, so:so + sn]
                            if ex == 0:
                                self.cp("dve", av, pyb[:, 0:sn], [B_py], [B_acc])
                            else:
                                self.tt("dve", av, av, pyb[:, 0:sn], ALU.add, [B_py, B_acc], [B_acc])
            S.dma("sp", YTv[:, :, t0:t0 + TG], acc[:], [B_acc], [YTB[g] for g in rg])

    def epilogue(self, HT, HTB, out):
        S = self.S
        self.stage()
        P, PB = self.ps, self.psB
        hrot = self.rot(2, [128, NCH, 128], F32)
        orot = self.rot(2, [128, D], F32)
        HTv = HT.rearrange("c p t -> p c t")
        B_out = Buf("out")
        pi = 0
        for tt in range(2, NT):
            h, B_h = hrot.next()
            g = self.group_of(tt * 128)
            S.dma("sp", h[:], HTv[:, :, tt * 128:(tt + 1) * 128], [HTB[g]], [B_h])
            o, B_o = orot.next()
            for cq in range(4):
                pt, B_pt = P[pi], PB[pi]
                pi = (pi + 1) % 8
                for j in range(4):
                    self.tr(pt[:, j * 128:(j + 1) * 128], h[:, cq * 4 + j, :], [B_h], [B_pt])
                self.cp("act" if cq % 2 else "dve", o[:, cq * 512:(cq + 1) * 512], pt[:], [B_pt], [B_o])
            S.dma("sp", out[(tt - 2) * 128:(tt - 1) * 128, :], o[:], [B_o], [B_out])
        return B_out

    def dump(self, src, dst, srcB):
        B = Buf("dump")
        self.S.dma("sp", dst, src, srcB, [B])
        return B

    def finish(self, bufs):
        S = self.S
        waits = S._deps("sp", bufs, ())
        S.ops["sp"].append((waits, None, None, 0))
        S.fence()
        S.emit()


def rope_tables():
    inv = (np.float32(10000.0) ** (-np.arange(0, 32, 2, dtype=np.float32) / np.float32(32))).astype(np.float32)
    t = np.arange(SEQ)
    pos = np.stack([t // 64, t % 64], 0).astype(np.float32)
    cosT = np.zeros((128, SEQ), np.float32)
    sinT = np.zeros((128, SEQ), np.float32)
    pmat = np.zeros((128, 128), np.float32)
    for p in range(128):
        d = p % 64
        axis, pair, f = d // 32, (d % 32) // 16, d % 16
        ang = (pos[axis] * inv[f]).astype(np.float32)
        cosT[p] = np.cos(ang)
        sinT[p] = np.sin(ang) * (-1.0 if pair == 0 else 1.0)
        partner = p + 16 if pair == 0 else p - 16
        pmat[partner, p] = 1.0
    return cosT, sinT, pmat


def attn_proj(self, UT, UTB, w_qkv, b_qkv, cos_in, sin_in, pmat_in, QTd, KTd, VAd, B_att):
    S = self.S
    self.stage()
    P, PB = self.ps, self.psB
    ug, B_ug = self.sbb([128, NCH, T], BF16)
    UTv = UT.rearrange("c p t -> p c t")
    for g, (t0, n) in enumerate(GROUPS):
        S.dma("sp", ug[:, :, t0:t0 + n], UTv[:, :, t0:t0 + n], [UTB[g]], [B_ug])
    cosT, B_tab = self.sbb([128, SEQ], F32)
    sinT = self.sb([128, SEQ], F32)
    pm = self.sb([128, 128], F32)
    S.dma("sp", cosT[:], cos_in, (), [B_tab])
    S.dma("sp", sinT[:], sin_in, (), [B_tab])
    S.dma("sp", pm[:], pmat_in, (), [B_tab])
    brow, B_brow = self.sbb([1, 2560], F32)
    S.dma("sp", brow[:], b_qkv, (), [B_brow])
    bkd, B_bkd = self.sbb([1, 4, 128], F32)
    for kv in range(4):
        for hh in range(2):
            S.dma("sp", bkd[0:1, kv, hh * 64:(hh + 1) * 64], b_qkv[0:1, 2048 + kv * 64:2048 + (kv + 1) * 64], (), [B_bkd])
    wrot = self.rot(2, [128, NCH, 512], BF16)
    wkd, B_wkd = self.sbb([128, NCH, 4, 128], BF16)
    q32rot = self.rot(2, [128, 512], F32)
    t1rot = self.rot(2, [128, 512], F32)
    qsrot = self.rot(3, [128, 512], BF16)
    vaug, B_va = self.sbb([128, NT, 4, 65], BF16)
    S.op("pool", lambda e: e.memset(vaug[:, :, :, 64:65], 1.0), (), [B_va])
    wv = w_qkv.rearrange("(k p) n -> p k n", p=128)
    pi = 0

    def project(lhs_fn, bias_ap, dst, c, Bw):
        nonlocal pi
        for g, (t0, n) in enumerate(GROUPS):
            pt, B_pt = P[pi], PB[pi]
            pi = (pi + 1) % 4
            for k in range(NCH):
                self.mm(pt[:, 0:n], lhs_fn(k), ug[:, k, t0:t0 + n], k == 0, False, [Bw, B_ug], [B_pt])
            self.mm(pt[:, 0:n], bias_ap, self.onesrow[0:1, 0:n], False, True, [B_brow, B_bkd, self.B_const], [B_pt])
            qs, B_qs = qsrot.next()
            if g == 0:
                self.cp("act", qs[:, 0:n], pt[:, 0:n], [B_pt], [B_qs])
            else:
                q32, B_q32 = q32rot.next()
                t1, B_t1 = t1rot.next()
                self.cp("act", q32[:, 0:n], pt[:, 0:n], [B_pt], [B_q32])
                p2, B_p2 = P[4 + (pi % 2)], PB[4 + (pi % 2)]
                self.mm(p2[:, 0:n], pm[:], q32[:, 0:n], True, True, [B_tab, B_q32], [B_p2])
                l0 = t0 - CTX
                self.tt("pool", t1[:, 0:n], q32[:, 0:n], cosT[:, l0:l0 + n], ALU.mult, [B_q32, B_tab], [B_t1])
                self.tt("dve", q32[:, 0:n], p2[:, 0:n], sinT[:, l0:l0 + n], ALU.mult, [B_p2, B_tab], [B_q32])
                self.tt("pool", qs[:, 0:n], t1[:, 0:n], q32[:, 0:n], ALU.add, [B_t1, B_q32], [B_qs])
            S.dma("sp", dst[c][:, t0:t0 + n], qs[:, 0:n], [B_qs], [B_att])

    for wb in range(5):
        w, B_w = wrot.next()
        S.dma("pool", w[:], wv[:, :, wb * 512:(wb + 1) * 512], (), [B_w])
        if wb < 4:
            for f in range(4):
                c = wb * 4 + f
                project(lambda k, w=w, f=f: w[:, k, f * 128:(f + 1) * 128], brow[0:1, c * 128:(c + 1) * 128], QTd, c, B_w)
        else:
            for hh in range(2):
                self.cp("dve", wkd[:, :, :, hh * 64:(hh + 1) * 64], w[:, :, 0:256].rearrange("p k (v d) -> p k v d", d=64),
                        [B_w], [B_wkd])
            for kv in range(4):
                project(lambda k, kv=kv: wkd[:, k, kv, :], bkd[0:1, kv, :], KTd, kv, B_wkd)
            for tt in range(NT):
                pt, B_pt = P[6 + tt % 2], PB[6 + tt % 2]
                for k in range(NCH):
                    self.mm(pt[:, 0:256], ug[:, k, tt * 128:(tt + 1) * 128], w[:, k, 256:512], k == 0, False, [B_w, B_ug], [B_pt])
                self.mm(pt[:, 0:256], self.ones[0:1, :], brow[0:1, 2304:2560], False, True, [B_brow, self.B_const], [B_pt])
                self.cp("act", vaug[:, tt, :, 0:64], pt[:, 0:256].rearrange("p (v d) -> p v d", d=64), [B_pt], [B_va])
            S.dma("sp", VAd, vaug[:].rearrange("p t v d -> p (t v d)"), [B_va], [B_att])


def attn_core(self, QTd, KTd, VAd, B_att, sink, w_o, b_o, YT, YTB):
    S = self.S
    self.stage()
    P, PB = self.ps, self.psB
    kt, B_kt = self.sbb([128, 4, T], BF16)
    S.dma("sp", kt[:], KTd.rearrange("v p t -> p v t"), [B_att], [B_kt])
    vaug, B_va = self.sbb([128, NT, 4, 65], BF16)
    S.dma("sp", vaug[:].rearrange("p t v d -> p (t v d)"), VAd, [B_att], [B_va])
    mask3, B_mk = self.sbb([128, 384], BF16)
    self.cp("dve", mask3[:, 0:128], self.m_ge[:], [self.B_const], [B_mk])
    self.cp("dve", mask3[:, 128:256], self.ones[:], [self.B_const], [B_mk])
    self.cp("dve", mask3[:, 256:384], self.m_le[:], [self.B_const], [B_mk])
    srow, B_srow = self.sbb([1, 32], F32)
    S.dma("sp", srow[:], sink, (), [B_srow])
    esink, B_es = self.sbb([128, 32], F32)
    self.mm(P[0][:, 0:32], self.ones[0:1, :], srow[:], True, True, [B_srow, self.B_const], [PB[0]])
    self.act(esink[:], P[0][:, 0:32], AF.Exp, [PB[0]], [B_es])
    borow, B_bo = self.sbb([1, D], F32)
    S.dma("sp", borow[:], b_o, (), [B_bo])
    qrot = self.rot(2, [128, NCH, 512], BF16)
    pcrot = self.rot(3, [128, 2, 512], BF16)
    plrot = self.rot(9, [128, 384], BF16)
    ot, B_ot = self.sbb([128, 4, D], F32)
    otg, B_otg = self.sbb([128, NCH, 512], BF16)
    worot = self.rot(3, [128, NCH, 128], BF16)
    yrot = self.rot(2, [128, 512], F32)
    den, B_den = self.sbb([128, 8], F32)
    QTv = QTd.rearrange("c p t -> p c t")
    wov = w_o.rearrange("(k p) n -> p k n", p=128)
    i_c = i_l = i_o = i_t = 0
    for g, (t0, n) in enumerate(GROUPS):
        nb = n // 128
        qg, B_qg = qrot.next()
        S.dma("sp", qg[:, :, 0:n], QTv[:, :, t0:t0 + n], [B_att], [B_qg])
        def stage1(h):
            nonlocal i_c, i_l
            qc, r0, kv = h // 2, (h % 2) * 64, h // 8
            pc, B_pc = pcrot.next()
            for ct in range(2):
                pt, B_pt = P[i_c], PB[i_c]
                i_c = (i_c + 1) % 2
                self.mm(pt[:, 0:n], kt[r0:r0 + 64, kv, ct * 128:(ct + 1) * 128], qg[r0:r0 + 64, qc, 0:n], True, True, [B_kt, B_qg], [B_pt])
                self.act(pc[:, ct, 0:n], pt[:, 0:n], AF.Exp, [B_pt], [B_pc], scale=0.125)
            pls = []
            for j in range(nb):
                if g > 0:
                    qb = (g - 1) * 4 + j
                    slots = [s_ for s_ in range(3) if 0 <= qb + s_ - 1 <= 15]
                    pt, B_pt = P[2 + i_l], PB[2 + i_l]
                    i_l = (i_l + 1) % 2
                    for s_ in slots:
                        k0 = CTX + (qb + s_ - 1) * 128
                        self.mm(pt[:, s_ * 128:(s_ + 1) * 128], kt[r0:r0 + 64, kv, k0:k0 + 128], qg[r0:r0 + 64, qc, j * 128:(j + 1) * 128],
                                True, True, [B_kt, B_qg], [B_pt])
                    pl, B_pl = plrot.next()
                    a_, b_ = slots[0] * 128, (slots[-1] + 1) * 128
                    self.act(pl[:, a_:b_], pt[:, a_:b_], AF.Exp, [B_pt], [B_pl], scale=0.125)
                    self.tt("pool", pl[:, a_:b_], pl[:, a_:b_], mask3[:, a_:b_], ALU.mult, [B_pl, B_mk], [B_pl])
                    pls.append((pl, B_pl, slots, qb))
                else:
                    pls.append(None)
            return (h, pc, B_pc, pls)

        def stage2(st):
            nonlocal i_o
            h, pc, B_pc, pls = st
            kv = h // 8
            po, B_po = P[4 + i_o], PB[4 + i_o]
            i_o = (i_o + 1) % 2
            for j in range(nb):
                mms = []
                if pls[j] is not None:
                    pl, B_pl, slots, qb = pls[j]
                    for s_ in slots:
                        mms.append((pl[:, s_ * 128:(s_ + 1) * 128], vaug[:, 2 + qb + s_ - 1, kv, :], B_pl))
                for ct in range(2):
                    mms.append((pc[:, ct, j * 128:(j + 1) * 128], vaug[:, ct, kv, :], B_pc))
                for i, (l_, r_, B_) in enumerate(mms):
                    self.mm(po[:, j * 65:(j + 1) * 65], l_, r_, i == 0, i == len(mms) - 1, [B_, B_va], [B_po])
            pov = po[:, 0:nb * 65].rearrange("p (j d) -> p j d", d=65)
            self.ts("dve", den[:, 0:nb], pov[:, :, 64], esink[:, h:h + 1], None, ALU.add, None, [B_po, B_es], [B_den])
            S.op("dve", lambda e, nb=nb: e.reciprocal(out=den[:, 0:nb], in_=den[:, 0:nb]), [B_den], [B_den])
            self.tt("dve", ot[:, 0:nb, h * 64:(h + 1) * 64], pov[:, :, 0:64], den[:, 0:nb].unsqueeze(2).broadcast_to([128, nb, 64]),
                    ALU.mult, [B_po, B_den], [B_ot])

        prev = None
        for h in range(33):
            cur_st = stage1(h) if h < 32 else None
            if prev is not None:
                stage2(prev)
            prev = cur_st
        for j in range(nb):
            for cq in range(4):
                pt, B_pt = P[6 + i_t], PB[6 + i_t]
                i_t = (i_t + 1) % 2
                for f in range(4):
                    c = cq * 4 + f
                    self.tr(pt[:, f * 128:(f + 1) * 128], ot[:, j, c * 128:(c + 1) * 128], [B_ot], [B_pt])
                self.cp("act" if cq % 2 else "dve", otg[:, cq * 4:(cq + 1) * 4, j * 128:(j + 1) * 128],
                        pt[:].rearrange("p (f t) -> p f t", f=4), [B_pt], [B_otg])
        for dc in range(NCH):
            wo, B_wo = worot.next()
            S.dma("pool", wo[:], wov[:, :, dc * 128:(dc + 1) * 128], (), [B_wo])
            pt, B_pt = P[6 + i_t], PB[6 + i_t]
            i_t = (i_t + 1) % 2
            for c in range(NCH):
                self.mm(pt[:, 0:n], wo[:, c, :], otg[:, c, 0:n], c == 0, False, [B_wo, B_otg], [B_pt])
            self.mm(pt[:, 0:n], borow[0:1, dc * 128:(dc + 1) * 128], self.onesrow[0:1, 0:n], False, True, [B_bo, self.B_const], [B_pt])
            y, B_y = yrot.next()
            self.cp("act", y[:, 0:n], pt[:, 0:n], [B_pt], [B_y])
            S.dma("sp", YT[dc][:, t0:t0 + n], y[:, 0:n], [B_y], [YTB[g]])


K.attn_proj = attn_proj
K.attn_core = attn_core


L2_EPS = 1e-6
RMS_EPS = 1e-6


def dn_proj(self, UT, UTB, w_in, conv_w, a_log, dt_bias, QTn, KTn, KTOK, VTOK, ZSd, GBd, B_dn):
    S = self.S
    self.stage()
    P, PB = self.ps, self.psB
    ug, B_ug = self.sbb([128, NCH, T], BF16)
    UTv = UT.rearrange("c p t -> p c t")
    for g, (t0, n) in enumerate(GROUPS):
        S.dma("sp", ug[:, :, t0:t0 + n], UTv[:, :, t0:t0 + n], [UTB[g]], [B_ug])
    w5l, B_w5l = self.sbb([128, 3, 128], F32)
    w5T, B_w5 = self.sbb([128, 320], F32)
    cwv = conv_w.rearrange("j (c p) -> (j c) p", p=128)
    for i, (a, b) in enumerate(((0, 128), (128, 256), (256, 320))):
        S.dma("sp", w5l[0:b - a, i, :], cwv[a:b, :], (), [B_w5l])
        self.tr(P[0][:, a:b], w5l[0:b - a, i, :], [B_w5l], [PB[0]])
    self.cp("dve", w5T[:], P[0][:, 0:320], [PB[0]], [B_w5])
    wv = w_in.rearrange("(k p) n -> p k n", p=128)
    wrot = self.rot(2, [128, NCH, 512], BF16)
    xcrot = self.rot(2, [128, T], F32)
    ycrot = self.rot(2, [128, T], F32)
    sqrot = self.rot(2, [128, T], F32)
    rnrot = self.rot(2, [128, 512], F32)
    qsrot = self.rot(2, [128, 512], BF16)
    ktok, B_ktok = self.sbb([128, NT, 128], BF16)
    vtok, B_vtok = self.sbb([128, NT, 128], F32)
    zsrot = self.rot(2, [128, 512], F32)
    pi = 0
    for wb in range(16):
        w, B_w = wrot.next()
        S.dma("pool", w[:], wv[:, :, wb * 512:(wb + 1) * 512], (), [B_w])
        for f in range(4):
            cc = wb * 4 + f
            xc, B_xc = xcrot.next()
            yc, B_yc = ycrot.next()
            sq, B_sq = sqrot.next()
            for g, (t0, n) in enumerate(GROUPS):
                pt, B_pt = P[pi], PB[pi]
                pi = (pi + 1) % 4
                for k in range(NCH):
                    self.mm(pt[:, 0:n], w[:, k, f * 128:(f + 1) * 128], ug[:, k, t0:t0 + n], k == 0, k == NCH - 1, [B_w, B_ug], [B_pt])
                self.cp("act", xc[:, t0:t0 + n], pt[:, 0:n], [B_pt], [B_xc])
            ce = "dve"
            for (s0, s1) in ((0, CTX), (CTX, T)):
                self.ts(ce, yc[:, s0:s1], xc[:, s0:s1], w5T[:, 2 * 64 + cc:2 * 64 + cc + 1], None, ALU.mult, None, [B_xc, B_w5], [B_yc])
                for j in (0, 1, 3, 4):
                    sh = j - 2
                    a, b = s0 + max(0, -sh), s1 - max(0, sh)
                    self.stt(ce, yc[:, a:b], xc[:, a + sh:b + sh], w5T[:, j * 64 + cc:j * 64 + cc + 1], yc[:, a:b], ALU.mult, ALU.add,
                             [B_xc, B_w5, B_yc], [B_yc])
            self.act(yc[:], yc[:], AF.Silu, [B_yc], [B_yc])
            if cc < 32:
                hq = cc % 16
                isq = cc < 16
                self.act(sq[:], yc[:], AF.Square, [B_yc], [B_sq])
                for g, (t0, n) in enumerate(GROUPS):
                    pt, B_pt = P[4 + g % 2], PB[4 + g % 2]
                    self.mm(pt[:, 0:n], self.ones[:], sq[:, t0:t0 + n], True, True, [B_sq, self.B_const], [B_pt])
                    rn, B_rn = rnrot.next()
                    self.act(rn[:, 0:n], pt[:, 0:n], AF.Sqrt, [B_pt], [B_rn], bias=L2_EPS)
                    S.op("dve", lambda e, rn=rn, n=n: e.reciprocal(out=rn[:, 0:n], in_=rn[:, 0:n]), [B_rn], [B_rn])
                    if isq:
                        self.stt("dve", rn[:, 0:n], yc[:, t0:t0 + n], 128.0 ** -0.5, rn[:, 0:n], ALU.mult, ALU.mult, [B_yc, B_rn], [B_rn])
                    else:
                        self.tt("dve", rn[:, 0:n], yc[:, t0:t0 + n], rn[:, 0:n], ALU.mult, [B_yc, B_rn], [B_rn])
                    qs, B_qs = qsrot.next()
                    self.cp("act", qs[:, 0:n], rn[:, 0:n], [B_rn], [B_qs])
                    S.dma("sp", (QTn if isq else KTn)[hq][:, t0:t0 + n], qs[:, 0:n], [B_qs], [B_dn])
                    if not isq:
                        pt2, B_pt2 = P[6 + g % 2], PB[6 + g % 2]
                        for tl in range(n // 128):
                            self.tr(pt2[:, tl * 128:(tl + 1) * 128], rn[:, tl * 128:(tl + 1) * 128], [B_rn], [B_pt2])
                        tt0 = t0 // 128
                        self.cp("dve", ktok[:, tt0:tt0 + n // 128, :], pt2[:, 0:n].rearrange("p (t d) -> p t d", d=128), [B_pt2], [B_ktok])
                if not isq:
                    S.dma("sp", KTOK[hq], ktok[:], [B_ktok], [B_dn])
            else:
                hv = cc - 32
                for g, (t0, n) in enumerate(GROUPS):
                    pt2, B_pt2 = P[6 + g % 2], PB[6 + g % 2]
                    for tl in range(n // 128):
                        self.tr(pt2[:, tl * 128:(tl + 1) * 128], yc[:, t0 + tl * 128:t0 + (tl + 1) * 128], [B_yc], [B_pt2])
                    tt0 = t0 // 128
                    self.cp("dve", vtok[:, tt0:tt0 + n // 128, :], pt2[:, 0:n].rearrange("p (t d) -> p t d", d=128), [B_pt2], [B_vtok])
                S.dma("sp", VTOK[hv], vtok[:], [B_vtok], [B_dn])
    ZSv = ZSd.rearrange("h p t e -> p h t e")
    for zb in range(8):
        w, B_w = wrot.next()
        S.dma("pool", w[:], wv[:, :, 8192 + zb * 512:8192 + (zb + 1) * 512], (), [B_w])
        for tt in range(NT):
            pt, B_pt = P[pi], PB[pi]
            pi = (pi + 1) % 4
            for k in range(NCH):
                self.mm(pt[:], ug[:, k, tt * 128:(tt + 1) * 128], w[:, k, :], k == 0, k == NCH - 1, [B_w, B_ug], [B_pt])
            zs, B_zs = zsrot.next()
            self.act(zs[:], pt[:], AF.Silu, [B_pt], [B_zs])
            S.dma("sp", ZSv[:, zb * 4:(zb + 1) * 4, tt, :], zs[:].rearrange("p (h e) -> p h e", e=128), [B_zs], [B_dn])
    w, B_w = wrot.next()
    S.dma("pool", w[:, :, 0:128], wv[:, :, 12288:12416], (), [B_w])
    arow, B_arow = self.sbb([1, 128], F32)
    S.dma("sp", arow[0:1, 0:64], a_log, (), [B_arow])
    S.dma("sp", arow[0:1, 64:128], dt_bias, (), [B_arow])
    abc, B_abc = self.sbb([128, 128], F32)
    self.mm(P[4][:, 0:128], self.ones[0:1, :], arow[:], True, True, [B_arow, self.B_const], [PB[4]])
    self.act(abc[:, 0:64], P[4][:, 0:64], AF.Exp, [PB[4]], [B_abc])
    self.ts("dve", abc[:, 0:64], abc[:, 0:64], -1.0, None, ALU.mult, None, [B_abc], [B_abc])
    self.cp("dve", abc[:, 64:128], P[4][:, 64:128], [PB[4]], [B_abc])
    gt_, B_g = self.sbb([128, NT, 64], F32)
    bt_, B_b = self.sbb([128, NT, 64], F32)
    tmp, B_tmp = self.sbb([128, 2, 2, 32], F32)
    for tt in range(NT):
        pt, B_pt = P[pi], PB[pi]
        pi = (pi + 1) % 4
        for k in range(NCH):
            self.mm(pt[:, 0:128], ug[:, k, tt * 128:(tt + 1) * 128], w[:, k, 0:128], k == 0, k == NCH - 1, [B_w, B_ug], [B_pt])
        pv = pt[:, 0:128].rearrange("p (d a h) -> p d a h", d=2, a=2)
        self.tt("dve", tmp[:, :, 0, :], pv[:, :, 0, :], abc[:, 64:128].rearrange("p (d h) -> p d h", d=2), ALU.add, [B_pt, B_abc], [B_tmp])
        self.act(tmp[:, :, 0, :], tmp[:, :, 0, :], AF.Exp, [B_tmp], [B_tmp])
        self.act(tmp[:, :, 0, :], tmp[:, :, 0, :], AF.Ln, [B_tmp], [B_tmp], bias=1.0)
        self.tt("dve", gt_[:, tt, :].rearrange("p (d h) -> p d h", d=2), tmp[:, :, 0, :], abc[:, 0:64].rearrange("p (d h) -> p d h", d=2),
                ALU.mult, [B_tmp, B_abc], [B_g])
        self.act(tmp[:, :, 1, :], pv[:, :, 1, :], AF.Exp, [B_pt], [B_tmp], scale=-1.0)
        self.ts("dve", tmp[:, :, 1, :], tmp[:, :, 1, :], 1.0, None, ALU.add, None, [B_tmp], [B_tmp])
        S.op("dve", lambda e, tt=tt: e.reciprocal(out=bt_[:, tt, :].rearrange("p (d h) -> p d h", d=2), in_=tmp[:, :, 1, :]), [B_tmp], [B_b])
    S.dma("sp", GBd[0], gt_[:], [B_g], [B_dn])
    S.dma("sp", GBd[1], bt_[:], [B_b], [B_dn])


def dn_core(self, QTn, KTn, KTOK, VTOK, ZSd, GBd, B_dn, norm_w, OGd, B_og):
    S = self.S
    self.stage()
    P, PB = self.ps, self.psB
    G, B_G = self.sbb([128, NT, 64], F32)
    Bt, B_Bt = self.sbb([128, NT, 64], F32)
    S.dma("sp", G[:], GBd[0], [B_dn], [B_G])
    S.dma("sp", Bt[:], GBd[1], [B_dn], [B_Bt])
    GC, B_GC = self.sbb([128, NT, 64], F32)
    EG, B_EG = self.sbb([128, NT, 64], F32)
    NEG, B_NEG = self.sbb([128, NT, 64], F32)
    BDL, B_BDL = self.sbb([128, NT, 64], F32)
    EGL, B_EGL = self.sbb([128, NT, 64], F32)
    STOP = self.cfg.get("dn_stop", 99)
    if STOP <= 1:
        return
    nwr, B_nwr = self.sbb([1, 128], F32)
    S.dma("sp", nwr[:], norm_w, (), [B_nwr])
    nw, B_nw = self.sbb([128, 128], F32)
    self.mm(P[0][:, 0:128], self.ones[0:1, :], nwr[:], True, True, [B_nwr, self.B_const], [PB[0]])
    self.cp("dve", nw[:], P[0][:, 0:128], [PB[0]], [B_nw])
    if STOP <= 2:
        return
    for ch in range(NT):
        pa, B_pa = P[1 + ch % 2], PB[1 + ch % 2]
        pb, B_pb = P[3 + ch % 2], PB[3 + ch % 2]
        self.mm(pa[:, 0:32], self.m_le[:], G[:, ch, 0:32], True, True, [B_G, self.B_const], [B_pa])
        self.mm(pa[:, 32:64], self.m_ge[:], G[:, ch, 32:64], True, True, [B_G, self.B_const], [B_pa])
        self.mm(pb[:, 0:64], self.ones[:], G[:, ch, :], True, True, [B_G, self.B_const], [B_pb])
        self.cp("dve", GC[:, ch, :], pa[:, 0:64], [B_pa], [B_GC])
        self.act(EG[:, ch, :], pa[:, 0:64], AF.Exp, [B_pa], [B_EG])
        self.act(EGL[:, ch, :], pb[:, 0:64], AF.Exp, [B_pb], [B_EGL])
        self.tt("dve", BDL[:, ch, :], pb[:, 0:64], GC[:, ch, :], ALU.subtract, [B_pb, B_GC], [B_BDL])
    if STOP <= 3:
        return
    self.ts("dve", NEG[:], EG[:], -1.0, None, ALU.mult, None, [B_EG], [B_NEG])
    self.act(BDL[:], BDL[:], AF.Exp, [B_BDL], [B_BDL])
    self.tt("dve", BDL[:], BDL[:], Bt[:], ALU.mult, [B_BDL, B_Bt], [B_BDL])

    if STOP <= 4:
        return
    qT, B_qT = self.sbb([128, T], BF16)
    kT, B_kT = self.sbb([128, T], BF16)
    ktok, B_ktok = self.sbb([128, NT, 128], BF16)
    vtok, B_vtok = self.sbb([128, 2, NT, 128], F32)
    AT, B_AT = self.sbb([128, NT, 4, 128], BF16)
    QKD, B_QKD = self.sbb([128, NT, 4, 128], BF16)
    O, B_O = self.sbb([128, 2, NT, 128], F32)
    ss, B_ss = self.sbb([128, NT], F32)
    ogT, B_ogT = self.sbb([128, T], BF16)
    NCHAIN = 2
    ch_t = []
    setup_t = []
    off_chain = self.off
    for i in range(2):
        d = {}
        for nm in ("KQ", "Gm", "E"):
            shape = [128, 2, 256] if nm == "KQ" else [128, 4, 128]
            d[nm] = self.sbb(shape, F32)
        setup_t.append(d)
    for i in range(NCHAIN):
        d = dict(setup_t[i % 2])
        for nm in ("PRa", "PRb"):
            d[nm] = self.sbb([128, 4, 2, 128], F32)
        for nm in ("PTa", "PTb"):
            d[nm] = self.sbb([128, 4, 128], F32)
        ch_t.append(d)
    off_end = self.off
    self.off = off_chain
    zs, B_zs = self.sbb([128, NT, 128], F32)
    sq, B_sq = self.sbb([128, NT, 128], F32)
    assert self.off <= off_end
    self.off = off_end
    psi = [0]

    def nps():
        i = psi[0]
        psi[0] = (i + 1) % 8
        return P[i], PB[i]
    St, B_S = self.sbb([128, 4, 128], F32)
    Sb, B_Sb = self.sbb([128, 4, 128], BF16)
    rp, B_rp = self.sbb([128, 4, 128], BF16)
    vn, B_vn = self.sbb([128, 4, 128], BF16)
    vd, B_vd = self.sbb([128, 4, 128], BF16)
    oq1rot = self.rot(2, [128, 4, 128], F32)
    oq2rot = self.rot(2, [128, 4, 128], F32)
    strict = (self.m_lt, self.m_gt)
    incl = (self.m_le, self.m_ge)
    aft_k = (self.m_gt, self.m_lt)
    atb_k = (self.m_le, self.m_ge)
    identb = self.ident[:].unsqueeze(1).broadcast_to([128, 4, 128])
    order = ([c for c in range(NT)], [1, 0] + [c for c in range(NT - 1, 1, -1)])
    PH = self.cfg.get("dn_phases", "ABC")
    for hq in range(self.cfg.get("dn_nhq", 16)):
        S.fence()
        S.dma("sp", qT[:], QTn[hq], [B_dn], [B_qT])
        S.dma("sp", kT[:], KTn[hq], [B_dn], [B_kT])
        S.dma("sp", ktok[:], KTOK[hq], [B_dn], [B_ktok])
        for hvl in range(2):
            S.dma("sp", vtok[:, hvl], VTOK[2 * hq + hvl], [B_dn], [B_vtok])
        for c0 in (range(0, NT, NCHAIN) if "A" in PH else []):
            chains = list(range(c0, min(NT, c0 + NCHAIN)))
            for ci, ch in enumerate(chains):
                tmpd = ch_t[ci]
                cs = slice(ch * 128, (ch + 1) * 128)
                KQ, B_KQ = tmpd["KQ"]
                Gm, B_Gm = tmpd["Gm"]
                E, B_E = tmpd["E"]
                PRa_, B_X = tmpd["PRa"]
                X = PRa_[:, :, 0, :]
                pk, B_pk = nps()
                self.mm(pk[:, 0:128], kT[:, cs], kT[:, cs], True, True, [B_kT], [B_pk])
                self.mm(pk[:, 128:256], kT[:, cs], qT[:, cs], True, True, [B_kT, B_qT], [B_pk])
                for d in range(2):
                    self.tt("dve", KQ[:, d, 0:128], pk[:, 0:128], strict[d][:], ALU.mult, [B_pk, self.B_const], [B_KQ])
                    self.tt("dve", KQ[:, d, 128:256], pk[:, 128:256], incl[d][:], ALU.mult, [B_pk, self.B_const], [B_KQ])
                for q in range(4):
                    d, hvl = q // 2, q % 2
                    col = d * 32 + 2 * hq + hvl
                    self.ts("pool", Gm[:, q, :], aft_k[d][:], G[:, ch, col:col + 1], None, ALU.mult, None, [B_G, self.B_const], [B_Gm])
                pd, B_pd = nps()
                for q in range(4):
                    self.mm(pd[:, q * 128:(q + 1) * 128], Gm[:, q, :], atb_k[q // 2][:], True, True, [B_Gm, self.B_const], [B_pd])
                self.act(E[:].rearrange("p q i -> p (q i)"), pd[:], AF.Exp, [B_pd], [B_E])
                for q in range(4):
                    d, hvl = q // 2, q % 2
                    col = d * 32 + 2 * hq + hvl
                    self.stt("dve", X[:, q, :], E[:, q, :], Bt[:, ch, col:col + 1], KQ[:, d, 0:128], ALU.mult, ALU.mult,
                             [B_E, B_Bt, B_KQ], [B_X])
                for d in range(2):
                    self.tt("pool", QKD[:, ch, 2 * d:2 * d + 2, :], E[:, 2 * d:2 * d + 2, :],
                            KQ[:, d, 128:256].unsqueeze(1).broadcast_to([128, 2, 128]), ALU.mult, [B_E, B_KQ], [B_QKD])
            for ci, ch in enumerate(chains):
                tmpd = ch_t[ci]
                PRa_, B_PRa = tmpd["PRa"]
                PTa_, B_PTa = tmpd["PTa"]
                pt, B_pt = nps()
                for q in range(4):
                    self.tr(pt[:, q * 128:(q + 1) * 128], PRa_[:, q, 0, :], [B_PRa], [B_pt])
                self.cp("act", PTa_[:].rearrange("p q i -> p (q i)"), pt[:], [B_pt], [B_PTa])
            for s_ in range(7):
                for ci, ch in enumerate(chains):
                    tmpd = ch_t[ci]
                    (PRc, B_PRc), (PTc, B_PTc) = (tmpd["PRa"], tmpd["PTa"]) if s_ % 2 == 0 else (tmpd["PRb"], tmpd["PTb"])
                    (PRn, B_PRn), (PTn, B_PTn) = (tmpd["PRb"], tmpd["PTb"]) if s_ % 2 == 0 else (tmpd["PRa"], tmpd["PTa"])
                    lo, hi = (0, 128) if s_ == 0 else ((128, 256) if s_ >= 5 else (0, 256))
                    banks = [nps(), nps()]
                    for q in range(4):
                        pb_, B_pb_ = banks[q // 2]
                        o0 = (q % 2) * 256
                        self.mm(pb_[:, o0 + lo:o0 + hi], PTc[:, q, :], PRc[:, q].rearrange("p a i -> p (a i)")[:, lo:hi], True, True,
                                [B_PTc, B_PRc], [B_pb_])
                    if s_ < 6:
                        pq, B_pq = nps()
                        for q in range(4):
                            self.mm(pq[:, q * 128:(q + 1) * 128], PRc[:, q, 0, :], PTc[:, q, :], True, True, [B_PRc, B_PTc], [B_pq])
                    for hb in range(2):
                        pb_, B_pb_ = banks[hb]
                        pv = pb_[:].rearrange("p (q a i) -> p q a i", q=2, a=2)
                        qs = slice(2 * hb, 2 * hb + 2)
                        if s_ < 5:
                            self.cp("act", PRn[:, qs, 0, :], pv[:, :, 0, :], [B_pb_], [B_PRn])
                        if s_ == 0:
                            self.tt("pool", PRn[:, qs, 1, :], self.ident[:].unsqueeze(1).broadcast_to([128, 2, 128]), PRc[:, qs, 0, :],
                                    ALU.subtract, [B_PRc, self.B_const], [B_PRn])
                        elif s_ < 6:
                            self.tt("dve", PRn[:, qs, 1, :], PRc[:, qs, 1, :], pv[:, :, 1, :], ALU.add, [B_pb_, B_PRc], [B_PRn])
                        else:
                            self.tt("dve", AT[:, ch, qs, :], PRc[:, qs, 1, :], pv[:, :, 1, :], ALU.add, [B_pb_, B_PRc], [B_AT])
                    if s_ < 6:
                        self.cp("act" if s_ % 2 else "dve", PTn[:].rearrange("p q i -> p (q i)"), pq[:], [B_pq], [B_PTn])
        S.op("pool", lambda e: e.memset(St[:], 0.0), (), [B_S])
        S.op("pool", lambda e: e.memset(Sb[:], 0.0), (), [B_Sb])
        S.op("pool", lambda e: e.memset(O[:], 0.0), (), [B_O])
        for s in (range(NT) if "B" in PH else []):
            pA, B_pA = P[0], PB[0]
            pB_, B_pB = (P[1], PB[1]) if s % 2 == 0 else (P[4], PB[4])
            pC, B_pC = P[2], PB[2]
            pD, B_pD = P[3], PB[3]
            pE, B_pE = (P[6], PB[6]) if s % 2 == 0 else (P[7], PB[7])
            info = []
            for q in range(4):
                d, hvl = q // 2, q % 2
                ch = order[d][s]
                info.append((d, hvl, ch, d * 32 + 2 * hq + hvl, slice(ch * 128, (ch + 1) * 128)))
            for q, (d, hvl, ch, col, cs) in enumerate(info):
                self.mm(pA[:, q * 128:(q + 1) * 128], kT[:, cs], Sb[:, q, :], True, True, [B_kT, B_Sb], [B_pA])
            for q, (d, hvl, ch, col, cs) in enumerate(info):
                self.mm(pB_[:, q * 128:(q + 1) * 128], qT[:, cs], Sb[:, q, :], True, True, [B_qT, B_Sb], [B_pB])
            for q, (d, hvl, ch, col, cs) in enumerate(info):
                self.stt("dve", rp[:, q, :], pA[:, q * 128:(q + 1) * 128], NEG[:, ch, col:col + 1], vtok[:, hvl, ch, :], ALU.mult, ALU.add,
                         [B_pA, B_NEG, B_vtok], [B_rp])
            for q, (d, hvl, ch, col, cs) in enumerate(info):
                self.mm(pC[:, q * 128:(q + 1) * 128], AT[:, ch, q, :], rp[:, q, :], True, True, [B_AT, B_rp], [B_pC])
            for q, (d, hvl, ch, col, cs) in enumerate(info):
                self.act(vn[:, q, :], pC[:, q * 128:(q + 1) * 128], AF.Identity, [B_pC, B_Bt], [B_vn], scale=Bt[:, ch, col:col + 1])
                self.ts("dve", vd[:, q, :], pC[:, q * 128:(q + 1) * 128], BDL[:, ch, col:col + 1], None, ALU.mult, None, [B_pC, B_BDL], [B_vd])
            for q, (d, hvl, ch, col, cs) in enumerate(info):
                self.mm(pD[:, q * 128:(q + 1) * 128], ktok[:, ch, :], vd[:, q, :], True, True, [B_ktok, B_vd], [B_pD])
            for q, (d, hvl, ch, col, cs) in enumerate(info):
                self.mm(pE[:, q * 128:(q + 1) * 128], QKD[:, ch, q, :], vn[:, q, :], True, True, [B_QKD, B_vn], [B_pE])
            for q, (d, hvl, ch, col, cs) in enumerate(info):
                self.stt("dve", St[:, q, :], St[:, q, :], EGL[:, ch, col:col + 1], pD[:, q * 128:(q + 1) * 128], ALU.mult, ALU.add,
                         [B_S, B_EGL, B_pD], [B_S])
            self.cp("act", Sb[:], St[:], [B_S], [B_Sb])
            oq1, B_oq1 = oq1rot.next()
            oq2, B_oq2 = oq2rot.next()
            for q, (d, hvl, ch, col, cs) in enumerate(info):
                self.act(oq1[:, q, :], pB_[:, q * 128:(q + 1) * 128], AF.Identity, [B_pB, B_EG], [B_oq1], scale=EG[:, ch, col:col + 1])
            self.cp("act", oq2[:].rearrange("p q i -> p (q i)"), pE[:], [B_pE], [B_oq2])
            for q, (d, hvl, ch, col, cs) in enumerate(info):
                self.tt("pool", O[:, hvl, ch, :], O[:, hvl, ch, :], oq1[:, q, :], ALU.add, [B_oq1, B_O], [B_O])
                self.tt("pool", O[:, hvl, ch, :], O[:, hvl, ch, :], oq2[:, q, :], ALU.add, [B_oq2, B_O], [B_O])
        S.fence()
        for hvl in (range(2) if "C" in PH else []):
            hv = 2 * hq + hvl
            S.dma("sp", zs[:], ZSd[hv], [B_dn], [B_zs])
            Ov = O[:, hvl]
            self.tt("pool", sq[:], Ov, Ov, ALU.mult, [B_O], [B_sq])
            S.op("dve", lambda e: e.tensor_reduce(out=ss[:], in_=sq[:], axis=AX.X, op=ALU.add), [B_sq], [B_ss])
            self.act(ss[:], ss[:], AF.Sqrt, [B_ss], [B_ss], bias=RMS_EPS, scale=1.0 / 128.0)
            S.op("dve", lambda e: e.reciprocal(out=ss[:], in_=ss[:]), [B_ss], [B_ss])
            self.tt("dve", sq[:], Ov, ss[:].unsqueeze(2).broadcast_to([128, NT, 128]), ALU.mult, [B_O, B_ss], [B_sq])
            self.tt("pool", sq[:], sq[:], nw[:].unsqueeze(1).broadcast_to([128, NT, 128]), ALU.mult, [B_sq, B_nw], [B_sq])
            self.tt("dve", sq[:], sq[:], zs[:], ALU.mult, [B_sq, B_zs], [B_sq])
            for tq in range(0, NT, 4):
                nt_ = min(4, NT - tq)
                pt, B_pt = P[4 + (tq // 4) % 2], PB[4 + (tq // 4) % 2]
                for i in range(nt_):
                    self.tr(pt[:, i * 128:(i + 1) * 128], sq[:, tq + i, :], [B_sq], [B_pt])
                self.cp("act", ogT[:, tq * 128:(tq + nt_) * 128], pt[:, 0:nt_ * 128], [B_pt], [B_ogT])
            S.dma("sp", OGd[hv], ogT[:], [B_ogT], [B_og])


def dn_out(self, OGd, B_og, w_o, YT, YTB):
    S = self.S
    self.stage()
    P, PB = self.ps, self.psB
    grot = self.rot(2, [128, 32, 512], BF16)
    worot = self.rot(3, [128, 32, 128], BF16)
    yrot = self.rot(3, [128, 512], F32)
    OGv = OGd.rearrange("h p t -> p h t")
    wov = w_o.rearrange("(k p) n -> p k n", p=128)
    pi = 0
    for g, (t0, n) in enumerate(GROUPS):
        og, B_g = grot.next()
        S.dma("sp", og[:, :, 0:n], OGv[:, :, t0:t0 + n], [B_og], [B_g])
        for dc in range(NCH):
            wo, B_wo = worot.next()
            S.dma("pool", wo[:], wov[:, :, dc * 128:(dc + 1) * 128], (), [B_wo])
            pt, B_pt = P[pi], PB[pi]
            pi = (pi + 1) % 4
            for k in range(32):
                self.mm(pt[:, 0:n], wo[:, k, :], og[:, k, 0:n], k == 0, k == 31, [B_wo, B_g], [B_pt])
            y, B_y = yrot.next()
            self.cp("act" if dc % 2 else "dve", y[:, 0:n], pt[:, 0:n], [B_pt], [B_y])
            S.dma("sp", YT[dc][:, t0:t0 + n], y[:, 0:n], [B_y], [YTB[g]])


K.dn_proj = dn_proj
K.dn_core = dn_core
K.dn_out = dn_out


def build_program():
    k = K({})
    I = {}
    I["c"] = k.ext_in("c", [16, 128]); I["c_ctx"] = k.ext_in("c_ctx", [16, 128])
    I["x"] = k.ext_in("x", [SEQ, D]); I["ctx"] = k.ext_in("ctx", [CTX, D])
    I["w_mod"] = k.ext_in("w_mod", [DEPTH, D, 6 * D]); I["b_mod"] = k.ext_in("b_mod", [DEPTH, 6 * D])
    I["ln_g"] = k.ext_in("ln_g", [DEPTH, 2, D]); I["ln_b"] = k.ext_in("ln_b", [DEPTH, 2, D])
    I["att_w_qkv"] = k.ext_in("att_w_qkv", [2, D, 2560]); I["att_b_qkv"] = k.ext_in("att_b_qkv", [2, 1, 2560])
    I["att_sink"] = k.ext_in("att_sink", [2, 1, 32]); I["att_w_o"] = k.ext_in("att_w_o", [2, D, D]); I["att_b_o"] = k.ext_in("att_b_o", [2, 1, D])
    I["cosT"] = k.ext_in("cosT", [128, SEQ]); I["sinT"] = k.ext_in("sinT", [128, SEQ]); I["pmat"] = k.ext_in("pmat", [128, 128])
    I["dn_w_in"] = k.ext_in("dn_w_in", [2, D, 12416]); I["dn_conv_w"] = k.ext_in("dn_conv_w", [2, 5, 8192])
    I["dn_a_log"] = k.ext_in("dn_a_log", [2, 1, 64]); I["dn_dt_bias"] = k.ext_in("dn_dt_bias", [2, 1, 64])
    I["dn_norm_w"] = k.ext_in("dn_norm_w", [2, 1, 128]); I["dn_w_o"] = k.ext_in("dn_w_o", [2, 4096, D])
    I["moe_w_r"] = k.ext_in("moe_w_r", [DEPTH, D, NE]); I["moe_b_r"] = k.ext_in("moe_b_r", [DEPTH, 1, NE])
    I["moe_wgu"] = k.ext_in("moe_wgu", [DEPTH, NE, 6, 128, 16, 256]); I["moe_bgu"] = k.ext_in("moe_bgu", [DEPTH, NE, 1536])
    I["moe_wdn"] = k.ext_in("moe_wdn", [DEPTH, NE, DEXP, D]); I["moe_bdn"] = k.ext_in("moe_bdn", [DEPTH, NE, D])
    out = k.ext_out("out", [SEQ, D])
    HT = k.dram("HT", [NCH, 128, T], F32); YT = k.dram("YT", [NCH, 128, T], F32); UT = k.dram("UT", [NCH, 128, T], BF16)
    GT = k.dram("GT", [NE, T], F32)
    QTd = k.dram("QTd", [NCH, 128, T], BF16); KTd = k.dram("KTd", [4, 128, T], BF16); VAd = k.dram("VAd", [128, NT * 4 * 65], BF16)
    QTn = k.dram("QTn", [16, 128, T], BF16); KTn = k.dram("KTn", [16, 128, T], BF16); KTOK = k.dram("KTOK", [16, 128, NT, 128], BF16)
    VTOK = k.dram("VTOK", [32, 128, NT, 128], F32); ZSd = k.dram("ZSd", [32, 128, NT, 128], F32); GBd = k.dram("GBd", [2, 128, NT, 64], F32)
    OGd = k.dram("OGd", [32, 128, T], BF16)
    HTB = [Buf() for _ in GROUPS]; YTB = [Buf() for _ in GROUPS]; UTB = [Buf() for _ in GROUPS]; GTB = [Buf() for _ in GROUPS]
    B_att = Buf(); B_dn = Buf(); B_og = Buf()
    k.setup()
    k.prologue_mod(I["c"], I["c_ctx"], I["w_mod"], I["b_mod"], I["ln_g"], I["ln_b"])
    k.prologue_x(I["x"], I["ctx"], HT, HTB)
    k.ln_mod(0, HT, HTB, None, None, None, None, (0, 1), UT, UTB)
    for i in range(DEPTH):
        j = i // 2
        if i % 2 == 0:
            k.attn_proj(UT, UTB, I["att_w_qkv"][j], I["att_b_qkv"][j], I["cosT"], I["sinT"], I["pmat"], QTd, KTd, VAd, B_att)
            k.attn_core(QTd, KTd, VAd, B_att, I["att_sink"][j], I["att_w_o"][j], I["att_b_o"][j], YT, YTB)
        else:
            k.dn_proj(UT, UTB, I["dn_w_in"][j], I["dn_conv_w"][j], I["dn_a_log"][j], I["dn_dt_bias"][j], QTn, KTn, KTOK, VTOK, ZSd, GBd, B_dn)
            k.dn_core(QTn, KTn, KTOK, VTOK, ZSd, GBd, B_dn, I["dn_norm_w"][j], OGd, B_og)
            k.dn_out(OGd, B_og, I["dn_w_o"][j], YT, YTB)
        k.ln_mod(i, HT, HTB, YT, YTB, 2, 0, (3, 4), UT, UTB, router=(I["moe_w_r"][i], I["moe_b_r"][i], GT, GTB))
        k.moe(i, UT, UTB, GT, GTB, YT, YTB, I["moe_wgu"][i], I["moe_bgu"][i], I["moe_wdn"][i], I["moe_bdn"][i], skip_ctx=(i == DEPTH - 1))
        k.ln_mod(i, HT, HTB, YT, YTB, 5, 1, (0, 1), UT, UTB, l_mod=min(i + 1, DEPTH - 1))
    B_out = k.epilogue(HT, HTB, out)
    k.finish([B_out])
    return k


_PROG = None


def kernel(x, c, ctx, c_ctx, w_mod, b_mod, ln_g, ln_b, att_w_qkv, att_b_qkv, att_sink, att_w_o, att_b_o,
           dn_w_in, dn_conv_w, dn_a_log, dn_dt_bias, dn_norm_w, dn_w_o, moe_w_router, moe_b_router,
           moe_w_gu, moe_b_gu, moe_w_down, moe_b_down):
    global _PROG
    if _PROG is None:
        _PROG = build_program()
    k = _PROG
    f = lambda a: np.ascontiguousarray(np.asarray(a, dtype=np.float32))
    x, c, ctx, c_ctx = f(x), f(c), f(ctx), f(c_ctx)
    B = x.shape[0]
    cosT, sinT, pmat = rope_tables()
    wg = np.asarray(moe_w_gu, dtype=np.float32)
    g = wg[..., 0::2].reshape(DEPTH, NE, 16, 128, 6, 128)
    u = wg[..., 1::2].reshape(DEPTH, NE, 16, 128, 6, 128)
    wgu = np.ascontiguousarray(np.concatenate([g, u], axis=-1).transpose(0, 1, 4, 3, 2, 5))
    del g, u, wg
    bg = np.asarray(moe_b_gu, dtype=np.float32)
    bgu = np.ascontiguousarray(np.concatenate([bg[..., 0::2], bg[..., 1::2]], axis=-1))
    shared = {
        "c_ctx": c_ctx.reshape(16, 128), "w_mod": f(w_mod), "b_mod": f(b_mod), "ln_g": f(ln_g), "ln_b": f(ln_b),
        "att_w_qkv": f(att_w_qkv), "att_b_qkv": f(att_b_qkv).reshape(2, 1, 2560), "att_sink": f(att_sink).reshape(2, 1, 32),
        "att_w_o": f(att_w_o), "att_b_o": f(att_b_o).reshape(2, 1, D), "cosT": cosT, "sinT": sinT, "pmat": pmat,
        "dn_w_in": f(dn_w_in), "dn_conv_w": f(dn_conv_w), "dn_a_log": f(dn_a_log).reshape(2, 1, 64),
        "dn_dt_bias": f(dn_dt_bias).reshape(2, 1, 64), "dn_norm_w": f(dn_norm_w).reshape(2, 1, 128), "dn_w_o": f(dn_w_o),
        "moe_w_r": f(moe_w_router), "moe_b_r": f(moe_b_router).reshape(DEPTH, 1, NE), "moe_wgu": wgu, "moe_bgu": bgu,
        "moe_wdn": f(moe_w_down), "moe_bdn": f(moe_b_down),
    }
    in_maps = []
    for b in range(B):
        m = dict(shared)
        m["x"] = x[b]
        m["ctx"] = ctx[b]
        m["c"] = c[b].reshape(16, 128)
        in_maps.append(m)
    res = run_bass_kernel_spmd(k.nc, in_maps, core_ids=list(range(B)))
    return np.stack([np.asarray(r["out"], dtype=np.float32) for r in res.results], axis=0)
```

```python
import contextlib
import numpy as np
import concourse.bass as bass
import concourse.mybir as mybir
from concourse.bass_utils import run_bass_kernel_spmd

F32 = mybir.dt.float32
BF16 = mybir.dt.bfloat16
ALU = mybir.AluOpType
AF = mybir.ActivationFunctionType
AX = mybir.AxisListType

D = 2048
NCH = 16
CTX = 256
SEQ = 2048
T = CTX + SEQ
NT = T // 128
DEPTH = 4
ALPHA = (2.0 * DEPTH) ** 0.25
LN_EPS = 1e-5
GROUPS = [(0, 256), (256, 512), (768, 512), (1280, 512), (1792, 512)]
NE = 32
DEXP = 768

EPOCH = 30000
N_DMA_SEMS = 12


class Buf:
    __slots__ = ("name", "lw", "rd", "excl")

    def __init__(self, name="", excl=False):
        self.name = name
        self.lw = None
        self.rd = {}
        self.excl = excl


class Sched:
    def __init__(self, nc):
        self.nc = nc
        self.ops = {"pe": [], "act": [], "dve": [], "pool": [], "sp": []}
        self.cnt = {e: 0 for e in self.ops}
        self.epoch = {e: 0 for e in self.ops}
        self.seen = {e: {} for e in self.ops}
        self.semkeys = set()
        self.dma_tot = {}
        self.dma_rr = {"sp": 0, "pool": 0, "act": 0}
        self.n_ops = 0

    def _deps(self, eng, reads, writes):
        deps = {}
        for b in reads:
            if b.lw is not None and deps.get(b.lw[0], 0) < b.lw[1]:
                deps[b.lw[0]] = b.lw[1]
            if b.excl:
                for k, v in b.rd.items():
                    if k[1] != eng and deps.get(k, 0) < v:
                        deps[k] = v
        for b in writes:
            if b.lw is not None and deps.get(b.lw[0], 0) < b.lw[1]:
                deps[b.lw[0]] = b.lw[1]
            for k, v in b.rd.items():
                if deps.get(k, 0) < v:
                    deps[k] = v
        out = []
        seen = self.seen[eng]
        for k, v in deps.items():
            if eng == "pe" and k[0] == "e" and k[1] == "pe":
                continue
            if seen.get(k, 0) >= v:
                continue
            seen[k] = v
            out.append((k, v))
        return out

    def op(self, eng, fn, reads=(), writes=()):
        if self.cnt[eng] >= EPOCH:
            self.epoch[eng] += 1
            self.cnt[eng] = 0
        waits = self._deps(eng, reads, writes)
        key = ("e", eng, self.epoch[eng])
        self.semkeys.add(key)
        self.cnt[eng] += 1
        val = self.cnt[eng]
        self.ops[eng].append((waits, fn, key, 1))
        for b in writes:
            b.lw = (key, val)
            b.rd = {}
        for b in reads:
            if b.rd.get(key, 0) < val:
                b.rd[key] = val
        self.n_ops += 1

    def dma(self, q, out_ap, in_ap, reads=(), writes=()):
        i = self.dma_rr[q]
        self.dma_rr[q] = (i + 1) % N_DMA_SEMS
        key = ("d", q, i)
        self.semkeys.add(key)
        prev = self.dma_tot.get(key, 0)
        waits = self._deps(q, reads, writes)
        if prev > 0 and self.seen[q].get(key, 0) < prev:
            self.seen[q][key] = prev
            waits.append((key, prev))
        tot = prev + 16
        self.dma_tot[key] = tot

        def fn(e, out_ap=out_ap, in_ap=in_ap):
            return e.dma_start(out=out_ap, in_=in_ap)
        self.ops[q].append((waits, fn, key, 16))
        for b in writes:
            b.lw = (key, tot)
            b.rd = {}
        for b in reads:
            if b.rd.get(key, 0) < tot:
                b.rd[key] = tot
        self.n_ops += 1

    def fence(self):
        allk = []
        for x in self.ops:
            if self.cnt[x] > 0:
                allk.append((("e", x, self.epoch[x]), self.cnt[x]))
        for k, v in self.dma_tot.items():
            allk.append((k, v))
        for e in self.ops:
            waits = []
            for k, v in allk:
                if self.seen[e].get(k, 0) < v:
                    self.seen[e][k] = v
                    waits.append((k, v))
            if waits:
                self.ops[e].append((waits, None, None, 0))

    def emit(self):
        nc = self.nc
        with contextlib.ExitStack() as st:
            sems = {}
            for k in sorted(self.semkeys):
                sems[k] = st.enter_context(nc.semaphore("s_" + "_".join(str(x) for x in k)))
            block = st.enter_context(nc.Block())

            def run(eng_name):
                def body(e):
                    for waits, fn, key, inc in self.ops[eng_name]:
                        for (k, v) in waits:
                            e.wait_ge(sems[k], v)
                        if fn is not None:
                            fn(e).then_inc(sems[key], inc)
                return body
            block.sync(run("sp"))
            block.tensor(run("pe"))
            block.scalar(run("act"))
            block.vector(run("dve"))
            block.gpsimd(run("pool"))


class Rot:
    def __init__(self, items):
        self.items = items
        self.i = 0

    def next(self):
        it = self.items[self.i]
        self.i = (self.i + 1) % len(self.items)
        return it


class K:
    def __init__(self, cfg):
        self.cfg = cfg
        self.nc = bass.Bass("TRN2", target_bir_lowering=False)
        self.S = Sched(self.nc)
        self.uid = 0
        self.base = 16640
        self.off = 16640
        self.LIMIT = 229000

    def sb(self, shape, dtype, persistent=False):
        self.uid += 1
        esz = 2 if dtype == BF16 else 4
        n = 1
        for s in shape[1:]:
            n *= s
        nbytes = (n * esz + 63) // 64 * 64
        t = self.nc.alloc_sbuf_tensor_at("sb%d" % self.uid, list(shape), dtype, offset=self.off)
        self.off += nbytes
        assert self.off <= self.LIMIT, ("SBUF overflow", self.off)
        if persistent:
            self.base = self.off
        return t

    def sbb(self, shape, dtype):
        return self.sb(shape, dtype), Buf()

    def rot(self, n, shape, dtype):
        return Rot([self.sbb(shape, dtype) for _ in range(n)])

    def stage(self):
        self.S.fence()
        self.off = self.base

    def dram(self, name, shape, dtype):
        return self.nc.dram_tensor(name, list(shape), dtype).ap()

    def ext_in(self, name, shape, dtype=F32):
        return self.nc.dram_tensor(name, list(shape), dtype, kind="ExternalInput").ap()

    def ext_out(self, name, shape, dtype=F32):
        return self.nc.dram_tensor(name, list(shape), dtype, kind="ExternalOutput").ap()

    def mm(self, out, lhsT, rhs, start, stop, reads, writes):
        self.S.op("pe", lambda e: e.matmul(out, lhsT, rhs, start=start, stop=stop), reads, writes)

    def tr(self, out, in_, reads, writes):
        ident = self.ident
        n = in_.shape[0]
        self.S.op("pe", lambda e: e.transpose(out, in_, ident[0:n, 0:n]), list(reads) + [self.B_const], writes)

    def act(self, out, in_, func, reads, writes, bias=0.0, scale=1.0):
        self.S.op("act", lambda e: e.activation(out=out, in_=in_, func=func, bias=bias, scale=scale), reads, writes)

    def ts(self, eng, out, in0, s1, s2, op0, op1, reads, writes):
        if op1 is None:
            self.S.op(eng, lambda e: e.tensor_scalar(out=out, in0=in0, scalar1=s1, scalar2=None, op0=op0), reads, writes)
        else:
            self.S.op(eng, lambda e: e.tensor_scalar(out=out, in0=in0, scalar1=s1, scalar2=s2, op0=op0, op1=op1), reads, writes)

    def tt(self, eng, out, in0, in1, op, reads, writes):
        self.S.op(eng, lambda e: e.tensor_tensor(out=out, in0=in0, in1=in1, op=op), reads, writes)

    def stt(self, eng, out, in0, scalar, in1, op0, op1, reads, writes):
        self.S.op(eng, lambda e: e.scalar_tensor_tensor(out=out, in0=in0, scalar=scalar, in1=in1, op0=op0, op1=op1), reads, writes)

    def cp(self, eng, out, in_, reads, writes):
        if eng == "act":
            self.S.op("act", lambda e: e.copy(out=out, in_=in_), reads, writes)
        else:
            self.S.op(eng, lambda e: e.tensor_copy(out=out, in_=in_), reads, writes)

    def setup(self):
        nc, S = self.nc, self.S
        self.ps = [nc.alloc_psum_tensor("ps%d" % i, [128, 512], F32) for i in range(8)]
        self.psB = [Buf("ps%d" % i, excl=True) for i in range(8)]
        self.B_const = Buf("const")
        self.ident = self.sb([128, 128], F32, True)
        self.ones = self.sb([128, 128], F32, True)
        ident, ones = self.ident, self.ones
        self.onesrow = self.sb([1, 512], F32, True)
        onesrow = self.onesrow
        S.op("pool", lambda e: e.memset(onesrow[:], 1.0), (), [self.B_const])
        S.op("pool", lambda e: e.memset(ones[:], 1.0), (), [self.B_const])
        S.op("pool", lambda e: e.memset(ident[:], 1.0), (), [self.B_const])
        S.op("pool", lambda e: e.affine_select(out=ident[:], in_=ident[:], pattern=[[-1, 128]],
                                               compare_op=ALU.is_equal, fill=0.0, base=0, channel_multiplier=1),
             [self.B_const], [self.B_const])
        self.m_le = self.sb([128, 128], F32, True)
        self.m_ge = self.sb([128, 128], F32, True)
        self.m_lt = self.sb([128, 128], F32, True)
        self.m_gt = self.sb([128, 128], F32, True)
        for m, step, cm, base in ((self.m_le, 1, -1, 0), (self.m_ge, -1, 1, 0), (self.m_lt, 1, -1, -1), (self.m_gt, -1, 1, -1)):
            S.op("pool", lambda e, m=m: e.memset(m[:], 1.0), (), [self.B_const])
            S.op("pool", lambda e, m=m, step=step, cm=cm, base=base: e.affine_select(
                out=m[:], in_=m[:], pattern=[[step, 128]], compare_op=ALU.is_ge, fill=0.0, base=base, channel_multiplier=cm),
                 [self.B_const], [self.B_const])
        self.MT = self.sb([128, DEPTH, 96, 2], F32, True)
        self.B_MT = Buf("MT")
        self.lnG = self.sb([128, 128], F32, True)
        self.lnB = self.sb([128, 128], F32, True)
        self.B_ln = Buf("ln")

    def prologue_mod(self, c_in, cctx_in, w_mod, b_mod, ln_g, ln_b):
        S = self.S
        self.stage()
        P = self.ps
        cs, B_cs = self.sbb([32, 128], F32)
        csT, B_csT = self.sbb([128, 32], F32)
        bm, B_bm = self.sbb([128, 3, 128], F32)
        bmT, B_bmT = self.sbb([128, 384], F32)
        lt, B_lt = self.sbb([128, 2, 128], F32)
        S.dma("sp", cs[0:16, :], c_in, (), [B_cs])
        S.dma("sp", cs[16:32, :], cctx_in, (), [B_cs])
        self.act(cs[:], cs[:], AF.Silu, [B_cs], [B_cs])
        self.tr(P[0][:, 0:32], cs[:], [B_cs], [self.psB[0]])
        self.cp("dve", csT[:], P[0][:, 0:32], [self.psB[0]], [B_csT])
        bview = b_mod.rearrange("l (f p) -> (l f) p", p=128)
        for i in range(3):
            S.dma("sp", bm[:, i, :], bview[i * 128:(i + 1) * 128, :], (), [B_bm])
        for i in range(3):
            self.tr(P[1][:, i * 128:(i + 1) * 128], bm[:, i, :], [B_bm], [self.psB[1]])
        self.cp("dve", bmT[:], P[1][:, 0:384], [self.psB[1]], [B_bmT])
        S.dma("sp", lt[:, 0, :], ln_g.rearrange("l s (k p) -> (l s k) p", p=128), (), [B_lt])
        S.dma("sp", lt[:, 1, :], ln_b.rearrange("l s (k p) -> (l s k) p", p=128), (), [B_lt])
        self.tr(P[2][:, 0:128], lt[:, 0, :], [B_lt], [self.psB[2]])
        self.tr(P[2][:, 128:256], lt[:, 1, :], [B_lt], [self.psB[2]])
        self.cp("dve", self.lnG[:], P[2][:, 0:128], [self.psB[2]], [self.B_ln])
        self.cp("dve", self.lnB[:], P[2][:, 128:256], [self.psB[2]], [self.B_ln])
        wrot = self.rot(3, [128, 16, 512], F32)
        csv = csT[:].rearrange("p (j k) -> p j k", j=2)
        pi = 0
        rowrot = self.rot(2, [2, 512], F32)
        for l in range(DEPTH):
            wv = w_mod[l].rearrange("(k p) n -> p k n", p=128)
            for nb in range(24):
                w, B_w = wrot.next()
                S.dma("sp", w[:], wv[:, :, nb * 512:(nb + 1) * 512], (), [B_w])
                pr_, B_pr = P[3 + pi], self.psB[3 + pi]
                pt, B_pt = P[5 + pi], self.psB[5 + pi]
                pi = (pi + 1) % 2
                for k in range(16):
                    self.mm(pr_[0:2, :], csv[:, :, k], w[:, k, :], k == 0, k == 15, [B_w, B_csT], [B_pr])
                row, B_row = rowrot.next()
                self.cp("act", row[:], pr_[0:2, :], [B_pr], [B_row])
                for f in range(4):
                    self.tr(pt[:, f * 2:f * 2 + 2], row[0:2, f * 128:(f + 1) * 128], [B_row], [B_pt])
                a = l * 96 + nb * 4
                self.tt("dve", self.MT[:, l, nb * 4:nb * 4 + 4, :], pt[:, 0:8].rearrange("p (f j) -> p f j", j=2),
                        bmT[:, a:a + 4].unsqueeze(2).broadcast_to([128, 4, 2]), ALU.add, [B_pt, B_bmT], [self.B_MT])
        for l in range(DEPTH):
            for sec in (1, 4):
                v = self.MT[:, l, sec * 16:(sec + 1) * 16, :]
                self.ts("dve", v, v, 1.0, None, ALU.add, None, [self.B_MT], [self.B_MT])
            for sec in (2, 5):
                v = self.MT[:, l, sec * 16:(sec + 1) * 16, :]
                self.ts("dve", v, v, 1.0 / ALPHA, None, ALU.mult, None, [self.B_MT], [self.B_MT])

    def prologue_x(self, x_in, ctx_in, HT, HTB):
        S = self.S
        self.stage()
        P = self.ps
        xrot = self.rot(2, [128, D], F32)
        srot = self.rot(2, [128, NCH, 128], F32)
        HTv = HT.rearrange("c p t -> p c t")
        pi = 0
        for tt in range(NT):
            xt, B_x = xrot.next()
            src = ctx_in[tt * 128:(tt + 1) * 128, :] if tt < 2 else x_in[(tt - 2) * 128:(tt - 1) * 128, :]
            S.dma("sp", xt[:], src, (), [B_x])
            st, B_st = srot.next()
            for cq in range(4):
                pt, B_pt = P[pi], self.psB[pi]
                pi = (pi + 1) % 8
                for j in range(4):
                    c = cq * 4 + j
                    self.tr(pt[:, j * 128:(j + 1) * 128], xt[:, c * 128:(c + 1) * 128], [B_x], [B_pt])
                self.cp("act" if cq % 2 else "dve", st[:, cq * 4:(cq + 1) * 4, :],
                        pt[:].rearrange("p (j t) -> p j t", j=4), [B_pt], [B_st])
            g = self.group_of(tt * 128)
            S.dma("sp", HTv[:, :, tt * 128:(tt + 1) * 128], st[:], [B_st], [HTB[g]])

    @staticmethod
    def group_of(t):
        for g, (s, n) in enumerate(GROUPS):
            if s <= t < s + n:
                return g
        raise ValueError

    def ln_mod(self, l, HT, HTB, YT, YTB, gate_sec, ln_idx, mod_sec, UT, UTB, router=None, l_mod=None):
        S = self.S
        self.stage()
        P, PB = self.ps, self.psB
        has_ln = YT is not None
        zrot = Rot([(self.sb([128, NCH, 512], F32), [Buf() for _ in range(NCH)]) for _ in range(2)])
        yrot = self.rot(4, [128, 512], F32)
        qrot = self.rot(2, [128, 512], F32)
        urot = self.rot(2, [128, NCH, 512], BF16)
        ufrot = self.rot(2, [128, 512], F32)
        mean, B_mean = self.sbb([128, 512], F32)
        msq, B_msq = self.sbb([128, 512], F32)
        rstd, B_rstd = self.sbb([128, 512], F32)
        HTv = HT.rearrange("c p t -> p c t")
        UTv = UT.rearrange("c p t -> p c t")
        if router is not None:
            w_r, b_r, GT, GTB = router
            wr, B_wr = self.sbb([128, NCH, NE], F32)
            br, B_br = self.sbb([1, NE], F32)
            S.dma("sp", wr[:], w_r.rearrange("(k p) e -> p k e", p=128), (), [B_wr])
            S.dma("sp", br[:], b_r, (), [B_br])
            lg, B_lg = self.sbb([128, 4, NE], F32)
            ex, B_ex = self.sbb([128, 4, NE], F32)
            mk, B_mk = self.sbb([128, 4, NE], F32)
            m8, B_m8 = self.sbb([128, 4, 8], F32)
            sm, B_sm = self.sbb([128, 4, 4], F32)
            gts, B_gts = self.sbb([NE, 512], F32)
        sh_sec, sc_sec = mod_sec
        lm = l if l_mod is None else l_mod
        eps = LN_EPS / (ALPHA * ALPHA)
        for g, (t0, n) in enumerate(GROUPS):
            j = 1 if g == 0 else 0
            z, BZ = zrot.next()
            u, B_u = urot.next()
            S.dma("sp", z[:, :, 0:n], HTv[:, :, t0:t0 + n], [HTB[g]], BZ)
            if has_ln:
                for c in range(NCH):
                    B_z = BZ[c]
                    y, B_y = yrot.next()
                    S.dma("sp", y[:, 0:n], YT[c][:, t0:t0 + n], [YTB[g]], [B_y])
                    self.stt("dve", z[:, c, 0:n], y[:, 0:n], self.MT[:, l, gate_sec * 16 + c, j:j + 1], z[:, c, 0:n],
                             ALU.mult, ALU.add, [B_y, B_z, self.B_MT], [B_z])
                    q, B_q = qrot.next()
                    self.act(q[:, 0:n], z[:, c, 0:n], AF.Square, [B_z], [B_q])
                    self.mm(P[0][:, 0:n], self.ones[:], z[:, c, 0:n], c == 0, c == NCH - 1, [B_z, self.B_const], [PB[0]])
                    self.mm(P[1][:, 0:n], self.ones[:], q[:, 0:n], c == 0, c == NCH - 1, [B_q, self.B_const], [PB[1]])
                self.ts("dve", mean[:, 0:n], P[0][:, 0:n], 1.0 / D, None, ALU.mult, None, [PB[0]], [B_mean])
                self.tt("pool", msq[:, 0:n], mean[:, 0:n], mean[:, 0:n], ALU.mult, [B_mean], [B_msq])
                self.stt("dve", rstd[:, 0:n], P[1][:, 0:n], 1.0 / D, msq[:, 0:n], ALU.mult, ALU.subtract, [PB[1], B_msq], [B_rstd])
                self.act(rstd[:, 0:n], rstd[:, 0:n], AF.Sqrt, [B_rstd], [B_rstd], bias=eps)
                S.op("dve", lambda e, n=n: e.reciprocal(out=rstd[:, 0:n], in_=rstd[:, 0:n]), [B_rstd], [B_rstd])
            for c in range(NCH):
                B_z = BZ[c]
                zc = z[:, c, 0:n]
                if has_ln:
                    self.tt("pool", zc, zc, mean[:, 0:n], ALU.subtract, [B_z, B_mean], [B_z])
                    self.tt("dve", zc, zc, rstd[:, 0:n], ALU.mult, [B_z, B_rstd], [B_z])
                    col = l * 32 + ln_idx * 16 + c
                    self.act(zc, zc, AF.Identity, [B_z, self.B_ln], [B_z], bias=self.lnB[:, col:col + 1], scale=self.lnG[:, col:col + 1])
                sc = self.MT[:, lm, sc_sec * 16 + c, j:j + 1]
                sh = self.MT[:, lm, sh_sec * 16 + c, j:j + 1]
                self.act(u[:, c, 0:n], zc, AF.Identity, [B_z, self.B_MT], [B_u], bias=sh, scale=sc)
                if router is not None:
                    uf, B_uf = ufrot.next()
                    self.ts("dve", uf[:, 0:n], zc, sc, sh, ALU.mult, ALU.add, [B_z, self.B_MT], [B_uf])
                    for tl in range(n // 128):
                        self.mm(P[2 + tl][:, 0:NE], uf[:, tl * 128:(tl + 1) * 128], wr[:, c, :],
                                c == 0, False, [B_uf, B_wr], [PB[2 + tl]])
            if has_ln:
                S.dma("sp", HTv[:, :, t0:t0 + n], z[:, :, 0:n], BZ, [HTB[g]])
            S.dma("sp", UTv[:, :, t0:t0 + n], u[:, :, 0:n], [B_u], [UTB[g]])
            if router is not None:
                ntl = n // 128
                for tl in range(ntl):
                    self.mm(P[2 + tl][:, 0:NE], self.ones[0:1, :], br[:], False, True, [B_br, self.B_const], [PB[2 + tl]])
                    self.cp("dve", lg[:, tl, :], P[2 + tl][:, 0:NE], [PB[2 + tl]], [B_lg])
                for tl in range(ntl):
                    S.op("dve", lambda e, tl=tl: e.max(out=m8[:, tl, :], in_=lg[:, tl, :]), [B_lg], [B_m8])
                self.ts("dve", sm[:, 0:ntl, 0:1], m8[:, 0:ntl, 0:1], -1.0, None, ALU.mult, None, [B_m8], [B_sm])
                for tl in range(ntl):
                    self.act(ex[:, tl, :], lg[:, tl, :], AF.Exp, [B_lg, B_sm], [B_ex], bias=sm[:, tl, 0:1])
                    self.ts("dve", mk[:, tl, :], lg[:, tl, :], m8[:, tl, 3:4], None, ALU.is_ge, None, [B_lg, B_m8], [B_mk])
                self.tt("dve", ex[:, 0:ntl, :], ex[:, 0:ntl, :], mk[:, 0:ntl, :], ALU.mult, [B_ex, B_mk], [B_ex])
                S.op("dve", lambda e, ntl=ntl: e.tensor_reduce(out=sm[:, 0:ntl, 1:2], in_=ex[:, 0:ntl, :], axis=AX.X, op=ALU.add),
                     [B_ex], [B_sm])
                S.op("dve", lambda e, ntl=ntl: e.reciprocal(out=sm[:, 0:ntl, 2:3], in_=sm[:, 0:ntl, 1:2]), [B_sm], [B_sm])
                for tl in range(ntl):
                    self.ts("dve", ex[:, tl, :], ex[:, tl, :], sm[:, tl, 2:3], None, ALU.mult, None, [B_ex, B_sm], [B_ex])
                    self.tr(P[6][0:NE, tl * 128:(tl + 1) * 128], ex[:, tl, :], [B_ex], [PB[6]])
                self.cp("dve", gts[:, 0:n], P[6][0:NE, 0:n], [PB[6]], [B_gts])
                S.dma("sp", GT[:, t0:t0 + n], gts[:, 0:n], [B_gts], [GTB[g]])

    def moe(self, l, UT, UTB, GT, GTB, YT, YTB, wgu, bgu, wdn, bdn, skip_ctx=False):
        S = self.S
        self.stage()
        P, PB = self.ps, self.psB
        TG = 768
        SG = 384
        ug, B_ug = self.sbb([128, NCH, TG], BF16)
        acc, B_acc = self.sbb([128, NCH, TG], F32)
        gt, B_gt = self.sbb([NE, TG], F32)
        sel, B_sel = self.sbb([NE, NE, 128], F32)
        bdt, B_bdt = self.sbb([NE, D], F32)
        bgT, B_bgT = self.sbb([128, 12, NE], F32)
        bgl, B_bgl = self.sbb([NE, 1536], F32)
        wrot = self.rot(3, [128, 16, 256], BF16)
        drot = self.rot(3, [128, 6, 1024], BF16)
        arot = self.rot(2, [128, 6, TG], BF16)
        t1rot = self.rot(2, [128, SG], F32)
        t2rot = self.rot(2, [128, SG], F32)
        t3rot = self.rot(2, [128, SG], F32)
        S.op("dve", lambda e: e.tensor_copy(out=sel[:], in_=self.ident[0:NE, 0:NE].unsqueeze(2).broadcast_to([NE, NE, 128])),
             [self.B_const], [B_sel])
        S.dma("sp", bdt[:], bdn, (), [B_bdt])
        S.dma("sp", bgl[:], bgu, (), [B_bgl])
        for fc in range(12):
            self.tr(P[0][:, fc * NE:(fc + 1) * NE], bgl[:, fc * 128:(fc + 1) * 128], [B_bgl], [PB[0]])
        self.cp("dve", bgT[:], P[0][:, 0:12 * NE].rearrange("p (f e) -> p f e", e=NE), [PB[0]], [B_bgT])
        UTv = UT.rearrange("c p t -> p c t")
        YTv = YT.rearrange("c p t -> p c t")
        pgu = 0
        py = 0
        for tg in range(T // TG):
            t0 = tg * TG
            rg = sorted(set(self.group_of(t0 + a) for a in range(0, TG, 128)))
            S.dma("sp", ug[:], UTv[:, :, t0:t0 + TG], [UTB[g] for g in rg], [B_ug])
            S.dma("sp", gt[:], GT[:, t0:t0 + TG], [GTB[g] for g in rg], [B_gt])
            sgs = [(256, 256), (512, 256)] if (skip_ctx and tg == 0) else [(0, SG), (SG, SG)]
            if skip_ctx and tg == 0:
                S.op("pool", lambda e: e.memset(acc[:, :, 0:256], 0.0), (), [B_acc])
            for ex in range(NE):
                for sg, (so, sn) in enumerate(sgs):
                    self.mm(P[4 + sg][:, 0:sn], sel[:, ex, :], gt[:, so:so + sn], True, True,
                            [B_sel, B_gt], [PB[4 + sg]])
                a, B_a = arot.next()
                for j in range(6):
                    w, B_w = wrot.next()
                    S.dma("pool", w[:], wgu[ex, j], (), [B_w])
                    for sg, (so, sn) in enumerate(sgs):
                        pg, B_pg = P[pgu], PB[pgu]
                        pu, B_pu = P[pgu + 1], PB[pgu + 1]
                        pgu = (pgu + 2) % 4
                        for k in range(16):
                            self.mm(pg[:, 0:sn], w[:, k, 0:128], ug[:, k, so:so + sn], k == 0, k == 15, [B_w, B_ug], [B_pg])
                        for k in range(16):
                            self.mm(pu[:, 0:sn], w[:, k, 128:256], ug[:, k, so:so + sn], k == 0, k == 15, [B_w, B_ug], [B_pu])
                        g1, B_g1 = t1rot.next()
                        u1, B_u1 = t2rot.next()
                        s1, B_s1 = t3rot.next()
                        self.ts("dve", g1[:, 0:sn], pg[:, 0:sn], bgT[:, j, ex:ex + 1], 7.0, ALU.add, ALU.min, [B_pg, B_bgT], [B_g1])
                        self.ts("dve", u1[:, 0:sn], pu[:, 0:sn], bgT[:, 6 + j, ex:ex + 1], 7.0, ALU.add, ALU.min, [B_pu, B_bgT], [B_u1])
                        self.act(s1[:, 0:sn], g1[:, 0:sn], AF.Sigmoid, [B_g1], [B_s1], scale=1.702)
                        self.ts("dve", u1[:, 0:sn], u1[:, 0:sn], -7.0, 1.0, ALU.max, ALU.add, [B_u1], [B_u1])
                        self.tt("dve", g1[:, 0:sn], g1[:, 0:sn], s1[:, 0:sn], ALU.mult, [B_g1, B_s1], [B_g1])
                        self.tt("dve", g1[:, 0:sn], g1[:, 0:sn], u1[:, 0:sn], ALU.mult, [B_g1, B_u1], [B_g1])
                        self.tt("dve", a[:, j, so:so + sn], g1[:, 0:sn], P[4 + sg][:, 0:sn], ALU.mult, [B_g1, PB[4 + sg]], [B_a])
                for dh in range(2):
                    wd, B_wd = drot.next()
                    S.dma("pool", wd[:], wdn[ex].rearrange("(f p) d -> p f d", p=128)[:, :, dh * 1024:(dh + 1) * 1024], (), [B_wd])
                    for dc in range(8):
                        cc = dh * 8 + dc
                        for sg, (so, sn) in enumerate(sgs):
                            pyb, B_py = P[6 + py], PB[6 + py]
                            py = (py + 1) % 2
                            if ex == 0:
                                self.mm(pyb[:, 0:sn], bdt[:, cc * 128:(cc + 1) * 128], gt[:, so:so + sn], True, False,
                                        [B_bdt, B_gt], [B_py])
                            for f in range(6):
                                self.mm(pyb[:, 0:sn], wd[:, f, dc * 128:(dc + 1) * 128], a[:, f, so:so + sn],
                                        (f == 0 and ex != 0), f == 5, [B_wd, B_a], [B_py])
                            av = acc[:, cc, so:so + sn]
                            if ex == 0:
                                self.cp("dve", av, pyb[:, 0:sn], [B_py], [B_acc])
                            else:
                                self.tt("dve", av, av, pyb[:, 0:sn], ALU.add, [B_py, B_acc], [B_acc])
            S.dma("sp", YTv[:, :, t0:t0 + TG], acc[:], [B_acc], [YTB[g] for g in rg])

    def epilogue(self, HT, HTB, out):
        S = self.S
        self.stage()
        P, PB = self.ps, self.psB
        hrot = self.rot(2, [128, NCH, 128], F32)
        orot = self.rot(2, [128, D], F32)
        HTv = HT.rearrange("c p t -> p c t")
        B_out = Buf("out")
        pi = 0
        for tt in range(2, NT):
            h, B_h = hrot.next()
            g = self.group_of(tt * 128)
            S.dma("sp", h[:], HTv[:, :, tt * 128:(tt + 1) * 128], [HTB[g]], [B_h])
            o, B_o = orot.next()
            for cq in range(4):
                pt, B_pt = P[pi], PB[pi]
                pi = (pi + 1) % 8
                for j in range(4):
                    self.tr(pt[:, j * 128:(j + 1) * 128], h[:, cq * 4 + j, :], [B_h], [B_pt])
                self.cp("act" if cq % 2 else "dve", o[:, cq * 512:(cq + 1) * 512], pt[:], [B_pt], [B_o])
            S.dma("sp", out[(tt - 2) * 128:(tt - 1) * 128, :], o[:], [B_o], [B_out])
        return B_out

    def dump(self, src, dst, srcB):
        B = Buf("dump")
        self.S.dma("sp", dst, src, srcB, [B])
        return B

    def finish(self, bufs):
        S = self.S
        waits = S._deps("sp", bufs, ())
        S.ops["sp"].append((waits, None, None, 0))
        S.fence()
        S.emit()


def rope_tables():
    inv = (np.float32(10000.0) ** (-np.arange(0, 32, 2, dtype=np.float32) / np.float32(32))).astype(np.float32)
    t = np.arange(SEQ)
    pos = np.stack([t // 64, t % 64], 0).astype(np.float32)
    cosT = np.zeros((128, SEQ), np.float32)
    sinT = np.zeros((128, SEQ), np.float32)
    pmat = np.zeros((128, 128), np.float32)
    for p in range(128):
        d = p % 64
        axis, pair, f = d // 32, (d % 32) // 16, d % 16
        ang = (pos[axis] * inv[f]).astype(np.float32)
        cosT[p] = np.cos(ang)
        sinT[p] = np.sin(ang) * (-1.0 if pair == 0 else 1.0)
        partner = p + 16 if pair == 0 else p - 16
        pmat[partner, p] = 1.0
    return cosT, sinT, pmat


def attn_proj(self, UT, UTB, w_qkv, b_qkv, cos_in, sin_in, pmat_in, QTd, KTd, VAd, B_att):
    S = self.S
    self.stage()
    P, PB = self.ps, self.psB
    ug, B_ug = self.sbb([128, NCH, T], BF16)
    UTv = UT.rearrange("c p t -> p c t")
    for g, (t0, n) in enumerate(GROUPS):
        S.dma("sp", ug[:, :, t0:t0 + n], UTv[:, :, t0:t0 + n], [UTB[g]], [B_ug])
    cosT, B_tab = self.sbb([128, SEQ], F32)
    sinT = self.sb([128, SEQ], F32)
    pm = self.sb([128, 128], F32)
    S.dma("sp", cosT[:], cos_in, (), [B_tab])
    S.dma("sp", sinT[:], sin_in, (), [B_tab])
    S.dma("sp", pm[:], pmat_in, (), [B_tab])
    brow, B_brow = self.sbb([1, 2560], F32)
    S.dma("sp", brow[:], b_qkv, (), [B_brow])
    bkd, B_bkd = self.sbb([1, 4, 128], F32)
    for kv in range(4):
        for hh in range(2):
            S.dma("sp", bkd[0:1, kv, hh * 64:(hh + 1) * 64], b_qkv[0:1, 2048 + kv * 64:2048 + (kv + 1) * 64], (), [B_bkd])
    wrot = self.rot(2, [128, NCH, 512], BF16)
    wkd, B_wkd = self.sbb([128, NCH, 4, 128], BF16)
    q32rot = self.rot(2, [128, 512], F32)
    t1rot = self.rot(2, [128, 512], F32)
    qsrot = self.rot(3, [128, 512], BF16)
    vaug, B_va = self.sbb([128, NT, 4, 65], BF16)
    S.op("pool", lambda e: e.memset(vaug[:, :, :, 64:65], 1.0), (), [B_va])
    wv = w_qkv.rearrange("(k p) n -> p k n", p=128)
    pi = 0

    def project(lhs_fn, bias_ap, dst, c, Bw):
        nonlocal pi
        for g, (t0, n) in enumerate(GROUPS):
            pt, B_pt = P[pi], PB[pi]
            pi = (pi + 1) % 4
            for k in range(NCH):
                self.mm(pt[:, 0:n], lhs_fn(k), ug[:, k, t0:t0 + n], k == 0, False, [Bw, B_ug], [B_pt])
            self.mm(pt[:, 0:n], bias_ap, self.onesrow[0:1, 0:n], False, True, [B_brow, B_bkd, self.B_const], [B_pt])
            qs, B_qs = qsrot.next()
            if g == 0:
                self.cp("act", qs[:, 0:n], pt[:, 0:n], [B_pt], [B_qs])
            else:
                q32, B_q32 = q32rot.next()
                t1, B_t1 = t1rot.next()
                self.cp("act", q32[:, 0:n], pt[:, 0:n], [B_pt], [B_q32])
                p2, B_p2 = P[4 + (pi % 2)], PB[4 + (pi % 2)]
                self.mm(p2[:, 0:n], pm[:], q32[:, 0:n], True, True, [B_tab, B_q32], [B_p2])
                l0 = t0 - CTX
                self.tt("pool", t1[:, 0:n], q32[:, 0:n], cosT[:, l0:l0 + n], ALU.mult, [B_q32, B_tab], [B_t1])
                self.tt("dve", q32[:, 0:n], p2[:, 0:n], sinT[:, l0:l0 + n], ALU.mult, [B_p2, B_tab], [B_q32])
                self.tt("pool", qs[:, 0:n], t1[:, 0:n], q32[:, 0:n], ALU.add, [B_t1, B_q32], [B_qs])
            S.dma("sp", dst[c][:, t0:t0 + n], qs[:, 0:n], [B_qs], [B_att])

    for wb in range(5):
        w, B_w = wrot.next()
        S.dma("pool", w[:], wv[:, :, wb * 512:(wb + 1) * 512], (), [B_w])
        if wb < 4:
            for f in range(4):
                c = wb * 4 + f
                project(lambda k, w=w, f=f: w[:, k, f * 128:(f + 1) * 128], brow[0:1, c * 128:(c + 1) * 128], QTd, c, B_w)
        else:
            for hh in range(2):
                self.cp("dve", wkd[:, :, :, hh * 64:(hh + 1) * 64], w[:, :, 0:256].rearrange("p k (v d) -> p k v d", d=64),
                        [B_w], [B_wkd])
            for kv in range(4):
                project(lambda k, kv=kv: wkd[:, k, kv, :], bkd[0:1, kv, :], KTd, kv, B_wkd)
            for tt in range(NT):
                pt, B_pt = P[6 + tt % 2], PB[6 + tt % 2]
                for k in range(NCH):
                    self.mm(pt[:, 0:256], ug[:, k, tt * 128:(tt + 1) * 128], w[:, k, 256:512], k == 0, False, [B_w, B_ug], [B_pt])
                self.mm(pt[:, 0:256], self.ones[0:1, :], brow[0:1, 2304:2560], False, True, [B_brow, self.B_const], [B_pt])
                self.cp("act", vaug[:, tt, :, 0:64], pt[:, 0:256].rearrange("p (v d) -> p v d", d=64), [B_pt], [B_va])
            S.dma("sp", VAd, vaug[:].rearrange("p t v d -> p (t v d)"), [B_va], [B_att])


def attn_core(self, QTd, KTd, VAd, B_att, sink, w_o, b_o, YT, YTB):
    S = self.S
    self.stage()
    P, PB = self.ps, self.psB
    kt, B_kt = self.sbb([128, 4, T], BF16)
    S.dma("sp", kt[:], KTd.rearrange("v p t -> p v t"), [B_att], [B_kt])
    vaug, B_va = self.sbb([128, NT, 4, 65], BF16)
    S.dma("sp", vaug[:].rearrange("p t v d -> p (t v d)"), VAd, [B_att], [B_va])
    mask3, B_mk = self.sbb([128, 384], BF16)
    self.cp("dve", mask3[:, 0:128], self.m_ge[:], [self.B_const], [B_mk])
    self.cp("dve", mask3[:, 128:256], self.ones[:], [self.B_const], [B_mk])
    self.cp("dve", mask3[:, 256:384], self.m_le[:], [self.B_const], [B_mk])
    srow, B_srow = self.sbb([1, 32], F32)
    S.dma("sp", srow[:], sink, (), [B_srow])
    esink, B_es = self.sbb([128, 32], F32)
    self.mm(P[0][:, 0:32], self.ones[0:1, :], srow[:], True, True, [B_srow, self.B_const], [PB[0]])
    self.act(esink[:], P[0][:, 0:32], AF.Exp, [PB[0]], [B_es])
    borow, B_bo = self.sbb([1, D], F32)
    S.dma("sp", borow[:], b_o, (), [B_bo])
    qrot = self.rot(2, [128, NCH, 512], BF16)
    pcrot = self.rot(3, [128, 2, 512], BF16)
    plrot = self.rot(9, [128, 384], BF16)
    ot, B_ot = self.sbb([128, 4, D], F32)
    otg, B_otg = self.sbb([128, NCH, 512], BF16)
    worot = self.rot(3, [128, NCH, 128], BF16)
    yrot = self.rot(2, [128, 512], F32)
    den, B_den = self.sbb([128, 8], F32)
    QTv = QTd.rearrange("c p t -> p c t")
    wov = w_o.rearrange("(k p) n -> p k n", p=128)
    i_c = i_l = i_o = i_t = 0
    for g, (t0, n) in enumerate(GROUPS):
        nb = n // 128
        qg, B_qg = qrot.next()
        S.dma("sp", qg[:, :, 0:n], QTv[:, :, t0:t0 + n], [B_att], [B_qg])
        def stage1(h):
            nonlocal i_c, i_l
            qc, r0, kv = h // 2, (h % 2) * 64, h // 8
            pc, B_pc = pcrot.next()
            for ct in range(2):
                pt, B_pt = P[i_c], PB[i_c]
                i_c = (i_c + 1) % 2
                self.mm(pt[:, 0:n], kt[r0:r0 + 64, kv, ct * 128:(ct + 1) * 128], qg[r0:r0 + 64, qc, 0:n], True, True, [B_kt, B_qg], [B_pt])
                self.act(pc[:, ct, 0:n], pt[:, 0:n], AF.Exp, [B_pt], [B_pc], scale=0.125)
            pls = []
            for j in range(nb):
                if g > 0:
                    qb = (g - 1) * 4 + j
                    slots = [s_ for s_ in range(3) if 0 <= qb + s_ - 1 <= 15]
                    pt, B_pt = P[2 + i_l], PB[2 + i_l]
                    i_l = (i_l + 1) % 2
                    for s_ in slots:
                        k0 = CTX + (qb + s_ - 1) * 128
                        self.mm(pt[:, s_ * 128:(s_ + 1) * 128], kt[r0:r0 + 64, kv, k0:k0 + 128], qg[r0:r0 + 64, qc, j * 128:(j + 1) * 128],
                                True, True, [B_kt, B_qg], [B_pt])
                    pl, B_pl = plrot.next()
                    a_, b_ = slots[0] * 128, (slots[-1] + 1) * 128
                    self.act(pl[:, a_:b_], pt[:, a_:b_], AF.Exp, [B_pt], [B_pl], scale=0.125)
                    self.tt("pool", pl[:, a_:b_], pl[:, a_:b_], mask3[:, a_:b_], ALU.mult, [B_pl, B_mk], [B_pl])
                    pls.append((pl, B_pl, slots, qb))
                else:
                    pls.append(None)
            return (h, pc, B_pc, pls)

        def stage2(st):
            nonlocal i_o
            h, pc, B_pc, pls = st
            kv = h // 8
            po, B_po = P[4 + i_o], PB[4 + i_o]
            i_o = (i_o + 1) % 2
            for j in range(nb):
                mms = []
                if pls[j] is not None:
                    pl, B_pl, slots, qb = pls[j]
                    for s_ in slots:
                        mms.append((pl[:, s_ * 128:(s_ + 1) * 128], vaug[:, 2 + qb + s_ - 1, kv, :], B_pl))
                for ct in range(2):
                    mms.append((pc[:, ct, j * 128:(j + 1) * 128], vaug[:, ct, kv, :], B_pc))
                for i, (l_, r_, B_) in enumerate(mms):
                    self.mm(po[:, j * 65:(j + 1) * 65], l_, r_, i == 0, i == len(mms) - 1, [B_, B_va], [B_po])
            pov = po[:, 0:nb * 65].rearrange("p (j d) -> p j d", d=65)
            self.ts("dve", den[:, 0:nb], pov[:, :, 64], esink[:, h:h + 1], None, ALU.add, None, [B_po, B_es], [B_den])
            S.op("dve", lambda e, nb=nb: e.reciprocal(out=den[:, 0:nb], in_=den[:, 0:nb]), [B_den], [B_den])
            self.tt("dve", ot[:, 0:nb, h * 64:(h + 1) * 64], pov[:, :, 0:64], den[:, 0:nb].unsqueeze(2).broadcast_to([128, nb, 64]),
                    ALU.mult, [B_po, B_den], [B_ot])

        prev = None
        for h in range(33):
            cur_st = stage1(h) if h < 32 else None
            if prev is not None:
                stage2(prev)
            prev = cur_st
        for j in range(nb):
            for cq in range(4):
                pt, B_pt = P[6 + i_t], PB[6 + i_t]
                i_t = (i_t + 1) % 2
                for f in range(4):
                    c = cq * 4 + f
                    self.tr(pt[:, f * 128:(f + 1) * 128], ot[:, j, c * 128:(c + 1) * 128], [B_ot], [B_pt])
                self.cp("act" if cq % 2 else "dve", otg[:, cq * 4:(cq + 1) * 4, j * 128:(j + 1) * 128],
                        pt[:].rearrange("p (f t) -> p f t", f=4), [B_pt], [B_otg])
        for dc in range(NCH):
            wo, B_wo = worot.next()
            S.dma("pool", wo[:], wov[:, :, dc * 128:(dc + 1) * 128], (), [B_wo])
            pt, B_pt = P[6 + i_t], PB[6 + i_t]
            i_t = (i_t + 1) % 2
            for c in range(NCH):
                self.mm(pt[:, 0:n], wo[:, c, :], otg[:, c, 0:n], c == 0, False, [B_wo, B_otg], [B_pt])
            self.mm(pt[:, 0:n], borow[0:1, dc * 128:(dc + 1) * 128], self.onesrow[0:1, 0:n], False, True, [B_bo, self.B_const], [B_pt])
            y, B_y = yrot.next()
            self.cp("act", y[:, 0:n], pt[:, 0:n], [B_pt], [B_y])
            S.dma("sp", YT[dc][:, t0:t0 + n], y[:, 0:n], [B_y], [YTB[g]])


K.attn_proj = attn_proj
K.attn_core = attn_core


L2_EPS = 1e-6
RMS_EPS = 1e-6


def dn_proj(self, UT, UTB, w_in, conv_w, a_log, dt_bias, QTn, KTn, KTOK, VTOK, ZSd, GBd, B_dn):
    S = self.S
    self.stage()
    P, PB = self.ps, self.psB
    ug, B_ug = self.sbb([128, NCH, T], BF16)
    UTv = UT.rearrange("c p t -> p c t")
    for g, (t0, n) in enumerate(GROUPS):
        S.dma("sp", ug[:, :, t0:t0 + n], UTv[:, :, t0:t0 + n], [UTB[g]], [B_ug])
    w5l, B_w5l = self.sbb([128, 3, 128], F32)
    w5T, B_w5 = self.sbb([128, 320], F32)
    cwv = conv_w.rearrange("j (c p) -> (j c) p", p=128)
    for i, (a, b) in enumerate(((0, 128), (128, 256), (256, 320))):
        S.dma("sp", w5l[0:b - a, i, :], cwv[a:b, :], (), [B_w5l])
        self.tr(P[0][:, a:b], w5l[0:b - a, i, :], [B_w5l], [PB[0]])
    self.cp("dve", w5T[:], P[0][:, 0:320], [PB[0]], [B_w5])
    wv = w_in.rearrange("(k p) n -> p k n", p=128)
    wrot = self.rot(2, [128, NCH, 512], BF16)
    xcrot = self.rot(2, [128, T], F32)
    ycrot = self.rot(2, [128, T], F32)
    sqrot = self.rot(2, [128, T], F32)
    rnrot = self.rot(2, [128, 512], F32)
    qsrot = self.rot(2, [128, 512], BF16)
    ktok, B_ktok = self.sbb([128, NT, 128], BF16)
    vtok, B_vtok = self.sbb([128, NT, 128], F32)
    zsrot = self.rot(2, [128, 512], F32)
    pi = 0
    def part2(cc, xc, B_xc, yc, B_yc, sq, B_sq):
        ce = "dve"
        for (s0, s1) in ((0, CTX), (CTX, T)):
            self.ts(ce, yc[:, s0:s1], xc[:, s0:s1], w5T[:, 2 * 64 + cc:2 * 64 + cc + 1], None, ALU.mult, None, [B_xc, B_w5], [B_yc])
            for j in (0, 1, 3, 4):
                sh = j - 2
                a, b = s0 + max(0, -sh), s1 - max(0, sh)
                self.stt(ce, yc[:, a:b], xc[:, a + sh:b + sh], w5T[:, j * 64 + cc:j * 64 + cc + 1], yc[:, a:b], ALU.mult, ALU.add,
                         [B_xc, B_w5, B_yc], [B_yc])
        self.act(yc[:], yc[:], AF.Silu, [B_yc], [B_yc])
        if cc < 32:
            hq = cc % 16
            isq = cc < 16
            self.act(sq[:], yc[:], AF.Square, [B_yc], [B_sq])
            for g, (t0, n) in enumerate(GROUPS):
                pt, B_pt = P[4 + g % 2], PB[4 + g % 2]
                self.mm(pt[:, 0:n], self.ones[:], sq[:, t0:t0 + n], True, True, [B_sq, self.B_const], [B_pt])
                rn, B_rn = rnrot.next()
                self.act(rn[:, 0:n], pt[:, 0:n], AF.Sqrt, [B_pt], [B_rn], bias=L2_EPS)
                S.op("dve", lambda e, rn=rn, n=n: e.reciprocal(out=rn[:, 0:n], in_=rn[:, 0:n]), [B_rn], [B_rn])
                if isq:
                    self.stt("dve", rn[:, 0:n], yc[:, t0:t0 + n], 128.0 ** -0.5, rn[:, 0:n], ALU.mult, ALU.mult, [B_yc, B_rn], [B_rn])
                else:
                    self.tt("dve", rn[:, 0:n], yc[:, t0:t0 + n], rn[:, 0:n], ALU.mult, [B_yc, B_rn], [B_rn])
                qs, B_qs = qsrot.next()
                self.cp("act", qs[:, 0:n], rn[:, 0:n], [B_rn], [B_qs])
                S.dma("sp", (QTn if isq else KTn)[hq][:, t0:t0 + n], qs[:, 0:n], [B_qs], [B_dn])
                if not isq:
                    pt2, B_pt2 = P[6 + g % 2], PB[6 + g % 2]
                    for tl in range(n // 128):
                        self.tr(pt2[:, tl * 128:(tl + 1) * 128], rn[:, tl * 128:(tl + 1) * 128], [B_rn], [B_pt2])
                    tt0 = t0 // 128
                    self.cp("dve", ktok[:, tt0:tt0 + n // 128, :], pt2[:, 0:n].rearrange("p (t d) -> p t d", d=128), [B_pt2], [B_ktok])
            if not isq:
                S.dma("sp", KTOK[hq], ktok[:], [B_ktok], [B_dn])
        else:
            hv = cc - 32
            for g, (t0, n) in enumerate(GROUPS):
                pt2, B_pt2 = P[6 + g % 2], PB[6 + g % 2]
                for tl in range(n // 128):
                    self.tr(pt2[:, tl * 128:(tl + 1) * 128], yc[:, t0 + tl * 128:t0 + (tl + 1) * 128], [B_yc], [B_pt2])
                tt0 = t0 // 128
                self.cp("dve", vtok[:, tt0:tt0 + n // 128, :], pt2[:, 0:n].rearrange("p (t d) -> p t d", d=128), [B_pt2], [B_vtok])
            S.dma("sp", VTOK[hv], vtok[:], [B_vtok], [B_dn])

    pending = None
    for wb in range(16):
        w, B_w = wrot.next()
        S.dma("pool", w[:], wv[:, :, wb * 512:(wb + 1) * 512], (), [B_w])
        for f in range(4):
            cc = wb * 4 + f
            xc, B_xc = xcrot.next()
            yc, B_yc = ycrot.next()
            sq, B_sq = sqrot.next()
            for g, (t0, n) in enumerate(GROUPS):
                pt, B_pt = P[pi], PB[pi]
                pi = (pi + 1) % 4
                for k in range(NCH):
                    self.mm(pt[:, 0:n], w[:, k, f * 128:(f + 1) * 128], ug[:, k, t0:t0 + n], k == 0, k == NCH - 1, [B_w, B_ug], [B_pt])
                self.cp("act", xc[:, t0:t0 + n], pt[:, 0:n], [B_pt], [B_xc])
            if pending is not None:
                part2(*pending)
            pending = (cc, xc, B_xc, yc, B_yc, sq, B_sq)
    part2(*pending)
    ZSv = ZSd.rearrange("h p t e -> p h t e")
    for zb in range(8):
        w, B_w = wrot.next()
        S.dma("pool", w[:], wv[:, :, 8192 + zb * 512:8192 + (zb + 1) * 512], (), [B_w])
        for tt in range(NT):
            pt, B_pt = P[pi], PB[pi]
            pi = (pi + 1) % 4
            for k in range(NCH):
                self.mm(pt[:], ug[:, k, tt * 128:(tt + 1) * 128], w[:, k, :], k == 0, k == NCH - 1, [B_w, B_ug], [B_pt])
            zs, B_zs = zsrot.next()
            self.act(zs[:], pt[:], AF.Silu, [B_pt], [B_zs])
            S.dma("sp", ZSv[:, zb * 4:(zb + 1) * 4, tt, :], zs[:].rearrange("p (h e) -> p h e", e=128), [B_zs], [B_dn])
    w, B_w = wrot.next()
    S.dma("pool", w[:, :, 0:128], wv[:, :, 12288:12416], (), [B_w])
    arow, B_arow = self.sbb([1, 128], F32)
    S.dma("sp", arow[0:1, 0:64], a_log, (), [B_arow])
    S.dma("sp", arow[0:1, 64:128], dt_bias, (), [B_arow])
    abc, B_abc = self.sbb([128, 128], F32)
    self.mm(P[4][:, 0:128], self.ones[0:1, :], arow[:], True, True, [B_arow, self.B_const], [PB[4]])
    self.act(abc[:, 0:64], P[4][:, 0:64], AF.Exp, [PB[4]], [B_abc])
    self.ts("dve", abc[:, 0:64], abc[:, 0:64], -1.0, None, ALU.mult, None, [B_abc], [B_abc])
    self.cp("dve", abc[:, 64:128], P[4][:, 64:128], [PB[4]], [B_abc])
    gt_, B_g = self.sbb([128, NT, 64], F32)
    bt_, B_b = self.sbb([128, NT, 64], F32)
    tmp, B_tmp = self.sbb([128, 2, 2, 32], F32)
    for tt in range(NT):
        pt, B_pt = P[pi], PB[pi]
        pi = (pi + 1) % 4
        for k in range(NCH):
            self.mm(pt[:, 0:128], ug[:, k, tt * 128:(tt + 1) * 128], w[:, k, 0:128], k == 0, k == NCH - 1, [B_w, B_ug], [B_pt])
        pv = pt[:, 0:128].rearrange("p (d a h) -> p d a h", d=2, a=2)
        self.tt("dve", tmp[:, :, 0, :], pv[:, :, 0, :], abc[:, 64:128].rearrange("p (d h) -> p d h", d=2), ALU.add, [B_pt, B_abc], [B_tmp])
        self.act(tmp[:, :, 0, :], tmp[:, :, 0, :], AF.Exp, [B_tmp], [B_tmp])
        self.act(tmp[:, :, 0, :], tmp[:, :, 0, :], AF.Ln, [B_tmp], [B_tmp], bias=1.0)
        self.tt("dve", gt_[:, tt, :].rearrange("p (d h) -> p d h", d=2), tmp[:, :, 0, :], abc[:, 0:64].rearrange("p (d h) -> p d h", d=2),
                ALU.mult, [B_tmp, B_abc], [B_g])
        self.act(tmp[:, :, 1, :], pv[:, :, 1, :], AF.Exp, [B_pt], [B_tmp], scale=-1.0)
        self.ts("dve", tmp[:, :, 1, :], tmp[:, :, 1, :], 1.0, None, ALU.add, None, [B_tmp], [B_tmp])
        S.op("dve", lambda e, tt=tt: e.reciprocal(out=bt_[:, tt, :].rearrange("p (d h) -> p d h", d=2), in_=tmp[:, :, 1, :]), [B_tmp], [B_b])
    S.dma("sp", GBd[0], gt_[:], [B_g], [B_dn])
    S.dma("sp", GBd[1], bt_[:], [B_b], [B_dn])


def dn_core(self, QTn, KTn, KTOK, VTOK, ZSd, GBd, B_dn, norm_w, OGd, B_og):
    S = self.S
    self.stage()
    P, PB = self.ps, self.psB
    G, B_G = self.sbb([128, NT, 64], F32)
    Bt, B_Bt = self.sbb([128, NT, 64], F32)
    S.dma("sp", G[:], GBd[0], [B_dn], [B_G])
    S.dma("sp", Bt[:], GBd[1], [B_dn], [B_Bt])
    GC, B_GC = self.sbb([128, NT, 64], F32)
    EG, B_EG = self.sbb([128, NT, 64], F32)
    NEG, B_NEG = self.sbb([128, NT, 64], F32)
    BDL, B_BDL = self.sbb([128, NT, 64], F32)
    EGL, B_EGL = self.sbb([128, NT, 64], F32)
    STOP = self.cfg.get("dn_stop", 99)
    if STOP <= 1:
        return
    nwr, B_nwr = self.sbb([1, 128], F32)
    S.dma("sp", nwr[:], norm_w, (), [B_nwr])
    nw, B_nw = self.sbb([128, 128], F32)
    self.mm(P[0][:, 0:128], self.ones[0:1, :], nwr[:], True, True, [B_nwr, self.B_const], [PB[0]])
    self.cp("dve", nw[:], P[0][:, 0:128], [PB[0]], [B_nw])
    if STOP <= 2:
        return
    for ch in range(NT):
        pa, B_pa = P[1 + ch % 2], PB[1 + ch % 2]
        pb, B_pb = P[3 + ch % 2], PB[3 + ch % 2]
        self.mm(pa[:, 0:32], self.m_le[:], G[:, ch, 0:32], True, True, [B_G, self.B_const], [B_pa])
        self.mm(pa[:, 32:64], self.m_ge[:], G[:, ch, 32:64], True, True, [B_G, self.B_const], [B_pa])
        self.mm(pb[:, 0:64], self.ones[:], G[:, ch, :], True, True, [B_G, self.B_const], [B_pb])
        self.cp("dve", GC[:, ch, :], pa[:, 0:64], [B_pa], [B_GC])
        self.act(EG[:, ch, :], pa[:, 0:64], AF.Exp, [B_pa], [B_EG])
        self.act(EGL[:, ch, :], pb[:, 0:64], AF.Exp, [B_pb], [B_EGL])
        self.tt("dve", BDL[:, ch, :], pb[:, 0:64], GC[:, ch, :], ALU.subtract, [B_pb, B_GC], [B_BDL])
    if STOP <= 3:
        return
    self.ts("dve", NEG[:], EG[:], -1.0, None, ALU.mult, None, [B_EG], [B_NEG])
    self.act(BDL[:], BDL[:], AF.Exp, [B_BDL], [B_BDL])
    self.tt("dve", BDL[:], BDL[:], Bt[:], ALU.mult, [B_BDL, B_Bt], [B_BDL])

    if STOP <= 4:
        return
    qT, B_qT = self.sbb([128, T], BF16)
    kT, B_kT = self.sbb([128, T], BF16)
    ktok, B_ktok = self.sbb([128, NT, 128], BF16)
    vtok, B_vtok = self.sbb([128, 2, NT, 128], F32)
    AT, B_AT = self.sbb([128, NT, 4, 128], BF16)
    QKD, B_QKD = self.sbb([128, NT, 4, 128], BF16)
    O, B_O = self.sbb([128, 2, NT, 128], F32)
    ss, B_ss = self.sbb([128, NT], F32)
    ogT, B_ogT = self.sbb([128, T], BF16)
    NCHAIN = 2
    ch_t = []
    setup_t = []
    off_chain = self.off
    for i in range(2):
        d = {}
        for nm in ("KQ", "Gm", "E"):
            shape = [128, 2, 256] if nm == "KQ" else [128, 4, 128]
            d[nm] = self.sbb(shape, F32)
        setup_t.append(d)
    for i in range(NCHAIN):
        d = dict(setup_t[i % 2])
        for nm in ("PRa", "PRb"):
            d[nm] = self.sbb([128, 4, 2, 128], F32)
        for nm in ("PTa", "PTb"):
            d[nm] = self.sbb([128, 4, 128], F32)
        ch_t.append(d)
    off_end = self.off
    self.off = off_chain
    zs, B_zs = self.sbb([128, NT, 128], F32)
    sq, B_sq = self.sbb([128, NT, 128], F32)
    assert self.off <= off_end
    self.off = off_end
    psi = [0]

    def nps():
        i = psi[0]
        psi[0] = (i + 1) % 8
        return P[i], PB[i]
    St, B_S = self.sbb([128, 4, 128], F32)
    Sb, B_Sb = self.sbb([128, 4, 128], BF16)
    rp, B_rp = self.sbb([128, 4, 128], BF16)
    vn, B_vn = self.sbb([128, 4, 128], BF16)
    vd, B_vd = self.sbb([128, 4, 128], BF16)
    oq1rot = self.rot(2, [128, 4, 128], F32)
    oq2rot = self.rot(2, [128, 4, 128], F32)
    strict = (self.m_lt, self.m_gt)
    incl = (self.m_le, self.m_ge)
    aft_k = (self.m_gt, self.m_lt)
    atb_k = (self.m_le, self.m_ge)
    identb = self.ident[:].unsqueeze(1).broadcast_to([128, 4, 128])
    order = ([c for c in range(NT)], [1, 0] + [c for c in range(NT - 1, 1, -1)])
    PH = self.cfg.get("dn_phases", "ABC")
    for hq in range(self.cfg.get("dn_nhq", 16)):
        S.fence()
        S.dma("sp", qT[:], QTn[hq], [B_dn], [B_qT])
        S.dma("sp", kT[:], KTn[hq], [B_dn], [B_kT])
        S.dma("sp", ktok[:], KTOK[hq], [B_dn], [B_ktok])
        for hvl in range(2):
            S.dma("sp", vtok[:, hvl], VTOK[2 * hq + hvl], [B_dn], [B_vtok])
        for c0 in (range(0, NT, NCHAIN) if "A" in PH else []):
            chains = list(range(c0, min(NT, c0 + NCHAIN)))
            for ci, ch in enumerate(chains):
                tmpd = ch_t[ci]
                cs = slice(ch * 128, (ch + 1) * 128)
                KQ, B_KQ = tmpd["KQ"]
                Gm, B_Gm = tmpd["Gm"]
                E, B_E = tmpd["E"]
                PRa_, B_X = tmpd["PRa"]
                X = PRa_[:, :, 0, :]
                pk, B_pk = nps()
                self.mm(pk[:, 0:128], kT[:, cs], kT[:, cs], True, True, [B_kT], [B_pk])
                self.mm(pk[:, 128:256], kT[:, cs], qT[:, cs], True, True, [B_kT, B_qT], [B_pk])
                for d in range(2):
                    self.tt("dve", KQ[:, d, 0:128], pk[:, 0:128], strict[d][:], ALU.mult, [B_pk, self.B_const], [B_KQ])
                    self.tt("dve", KQ[:, d, 128:256], pk[:, 128:256], incl[d][:], ALU.mult, [B_pk, self.B_const], [B_KQ])
                for q in range(4):
                    d, hvl = q // 2, q % 2
                    col = d * 32 + 2 * hq + hvl
                    self.ts("pool", Gm[:, q, :], aft_k[d][:], G[:, ch, col:col + 1], None, ALU.mult, None, [B_G, self.B_const], [B_Gm])
                pd, B_pd = nps()
                for q in range(4):
                    self.mm(pd[:, q * 128:(q + 1) * 128], Gm[:, q, :], atb_k[q // 2][:], True, True, [B_Gm, self.B_const], [B_pd])
                self.act(E[:].rearrange("p q i -> p (q i)"), pd[:], AF.Exp, [B_pd], [B_E])
                for q in range(4):
                    d, hvl = q // 2, q % 2
                    col = d * 32 + 2 * hq + hvl
                    self.stt("dve", X[:, q, :], E[:, q, :], Bt[:, ch, col:col + 1], KQ[:, d, 0:128], ALU.mult, ALU.mult,
                             [B_E, B_Bt, B_KQ], [B_X])
                for d in range(2):
                    self.tt("pool", QKD[:, ch, 2 * d:2 * d + 2, :], E[:, 2 * d:2 * d + 2, :],
                            KQ[:, d, 128:256].unsqueeze(1).broadcast_to([128, 2, 128]), ALU.mult, [B_E, B_KQ], [B_QKD])
            for ci, ch in enumerate(chains):
                tmpd = ch_t[ci]
                PRa_, B_PRa = tmpd["PRa"]
                PTa_, B_PTa = tmpd["PTa"]
                pt, B_pt = nps()
                for q in range(4):
                    self.tr(pt[:, q * 128:(q + 1) * 128], PRa_[:, q, 0, :], [B_PRa], [B_pt])
                self.cp("act", PTa_[:].rearrange("p q i -> p (q i)"), pt[:], [B_pt], [B_PTa])
            for s_ in range(7):
                for ci, ch in enumerate(chains):
                    tmpd = ch_t[ci]
                    (PRc, B_PRc), (PTc, B_PTc) = (tmpd["PRa"], tmpd["PTa"]) if s_ % 2 == 0 else (tmpd["PRb"], tmpd["PTb"])
                    (PRn, B_PRn), (PTn, B_PTn) = (tmpd["PRb"], tmpd["PTb"]) if s_ % 2 == 0 else (tmpd["PRa"], tmpd["PTa"])
                    lo, hi = (0, 128) if s_ == 0 else ((128, 256) if s_ >= 5 else (0, 256))
                    banks = [nps(), nps()]
                    for q in range(4):
                        pb_, B_pb_ = banks[q // 2]
                        o0 = (q % 2) * 256
                        self.mm(pb_[:, o0 + lo:o0 + hi], PTc[:, q, :], PRc[:, q].rearrange("p a i -> p (a i)")[:, lo:hi], True, True,
                                [B_PTc, B_PRc], [B_pb_])
                    if s_ < 6:
                        pq, B_pq = nps()
                        for q in range(4):
                            self.mm(pq[:, q * 128:(q + 1) * 128], PRc[:, q, 0, :], PTc[:, q, :], True, True, [B_PRc, B_PTc], [B_pq])
                    for hb in range(2):
                        pb_, B_pb_ = banks[hb]
                        pv = pb_[:].rearrange("p (q a i) -> p q a i", q=2, a=2)
                        qs = slice(2 * hb, 2 * hb + 2)
                        if s_ < 5:
                            self.cp("act", PRn[:, qs, 0, :], pv[:, :, 0, :], [B_pb_], [B_PRn])
                        if s_ == 0:
                            self.tt("pool", PRn[:, qs, 1, :], self.ident[:].unsqueeze(1).broadcast_to([128, 2, 128]), PRc[:, qs, 0, :],
                                    ALU.subtract, [B_PRc, self.B_const], [B_PRn])
                        elif s_ < 6:
                            self.tt("dve", PRn[:, qs, 1, :], PRc[:, qs, 1, :], pv[:, :, 1, :], ALU.add, [B_pb_, B_PRc], [B_PRn])
                        else:
                            self.tt("dve", AT[:, ch, qs, :], PRc[:, qs, 1, :], pv[:, :, 1, :], ALU.add, [B_pb_, B_PRc], [B_AT])
                    if s_ < 6:
                        self.cp("act" if s_ % 2 else "dve", PTn[:].rearrange("p q i -> p (q i)"), pq[:], [B_pq], [B_PTn])
        S.op("pool", lambda e: e.memset(St[:], 0.0), (), [B_S])
        S.op("pool", lambda e: e.memset(Sb[:], 0.0), (), [B_Sb])
        S.op("pool", lambda e: e.memset(O[:], 0.0), (), [B_O])
        for s in (range(NT) if "B" in PH else []):
            pA, B_pA = P[0], PB[0]
            pB_, B_pB = (P[1], PB[1]) if s % 2 == 0 else (P[4], PB[4])
            pC, B_pC = P[2], PB[2]
            pD, B_pD = P[3], PB[3]
            pE, B_pE = (P[6], PB[6]) if s % 2 == 0 else (P[7], PB[7])
            info = []
            for q in range(4):
                d, hvl = q // 2, q % 2
                ch = order[d][s]
                info.append((d, hvl, ch, d * 32 + 2 * hq + hvl, slice(ch * 128, (ch + 1) * 128)))
            for q, (d, hvl, ch, col, cs) in enumerate(info):
                self.mm(pA[:, q * 128:(q + 1) * 128], kT[:, cs], Sb[:, q, :], True, True, [B_kT, B_Sb], [B_pA])
            for q, (d, hvl, ch, col, cs) in enumerate(info):
                self.mm(pB_[:, q * 128:(q + 1) * 128], qT[:, cs], Sb[:, q, :], True, True, [B_qT, B_Sb], [B_pB])
            for q, (d, hvl, ch, col, cs) in enumerate(info):
                self.stt("dve", rp[:, q, :], pA[:, q * 128:(q + 1) * 128], NEG[:, ch, col:col + 1], vtok[:, hvl, ch, :], ALU.mult, ALU.add,
                         [B_pA, B_NEG, B_vtok], [B_rp])
            for q, (d, hvl, ch, col, cs) in enumerate(info):
                self.mm(pC[:, q * 128:(q + 1) * 128], AT[:, ch, q, :], rp[:, q, :], True, True, [B_AT, B_rp], [B_pC])
            for q, (d, hvl, ch, col, cs) in enumerate(info):
                self.act(vn[:, q, :], pC[:, q * 128:(q + 1) * 128], AF.Identity, [B_pC, B_Bt], [B_vn], scale=Bt[:, ch, col:col + 1])
                self.ts("dve", vd[:, q, :], pC[:, q * 128:(q + 1) * 128], BDL[:, ch, col:col + 1], None, ALU.mult, None, [B_pC, B_BDL], [B_vd])
            for q, (d, hvl, ch, col, cs) in enumerate(info):
                self.mm(pD[:, q * 128:(q + 1) * 128], ktok[:, ch, :], vd[:, q, :], True, True, [B_ktok, B_vd], [B_pD])
            for q, (d, hvl, ch, col, cs) in enumerate(info):
                self.mm(pE[:, q * 128:(q + 1) * 128], QKD[:, ch, q, :], vn[:, q, :], True, True, [B_QKD, B_vn], [B_pE])
            for q, (d, hvl, ch, col, cs) in enumerate(info):
                self.stt("dve", St[:, q, :], St[:, q, :], EGL[:, ch, col:col + 1], pD[:, q * 128:(q + 1) * 128], ALU.mult, ALU.add,
                         [B_S, B_EGL, B_pD], [B_S])
            self.cp("act", Sb[:], St[:], [B_S], [B_Sb])
            oq1, B_oq1 = oq1rot.next()
            oq2, B_oq2 = oq2rot.next()
            for q, (d, hvl, ch, col, cs) in enumerate(info):
                self.act(oq1[:, q, :], pB_[:, q * 128:(q + 1) * 128], AF.Identity, [B_pB, B_EG], [B_oq1], scale=EG[:, ch, col:col + 1])
            self.cp("act", oq2[:].rearrange("p q i -> p (q i)"), pE[:], [B_pE], [B_oq2])
            for q, (d, hvl, ch, col, cs) in enumerate(info):
                self.tt("pool", O[:, hvl, ch, :], O[:, hvl, ch, :], oq1[:, q, :], ALU.add, [B_oq1, B_O], [B_O])
                self.tt("pool", O[:, hvl, ch, :], O[:, hvl, ch, :], oq2[:, q, :], ALU.add, [B_oq2, B_O], [B_O])
        S.fence()
        for hvl in (range(2) if "C" in PH else []):
            hv = 2 * hq + hvl
            S.dma("sp", zs[:], ZSd[hv], [B_dn], [B_zs])
            Ov = O[:, hvl]
            self.tt("pool", sq[:], Ov, Ov, ALU.mult, [B_O], [B_sq])
            S.op("dve", lambda e: e.tensor_reduce(out=ss[:], in_=sq[:], axis=AX.X, op=ALU.add), [B_sq], [B_ss])
            self.act(ss[:], ss[:], AF.Sqrt, [B_ss], [B_ss], bias=RMS_EPS, scale=1.0 / 128.0)
            S.op("dve", lambda e: e.reciprocal(out=ss[:], in_=ss[:]), [B_ss], [B_ss])
            self.tt("dve", sq[:], Ov, ss[:].unsqueeze(2).broadcast_to([128, NT, 128]), ALU.mult, [B_O, B_ss], [B_sq])
            self.tt("pool", sq[:], sq[:], nw[:].unsqueeze(1).broadcast_to([128, NT, 128]), ALU.mult, [B_sq, B_nw], [B_sq])
            self.tt("dve", sq[:], sq[:], zs[:], ALU.mult, [B_sq, B_zs], [B_sq])
            for tq in range(0, NT, 4):
                nt_ = min(4, NT - tq)
                pt, B_pt = P[4 + (tq // 4) % 2], PB[4 + (tq // 4) % 2]
                for i in range(nt_):
                    self.tr(pt[:, i * 128:(i + 1) * 128], sq[:, tq + i, :], [B_sq], [B_pt])
                self.cp("act", ogT[:, tq * 128:(tq + nt_) * 128], pt[:, 0:nt_ * 128], [B_pt], [B_ogT])
            S.dma("sp", OGd[hv], ogT[:], [B_ogT], [B_og])


def dn_out(self, OGd, B_og, w_o, YT, YTB):
    S = self.S
    self.stage()
    P, PB = self.ps, self.psB
    grot = self.rot(2, [128, 32, 512], BF16)
    worot = self.rot(3, [128, 32, 128], BF16)
    yrot = self.rot(3, [128, 512], F32)
    OGv = OGd.rearrange("h p t -> p h t")
    wov = w_o.rearrange("(k p) n -> p k n", p=128)
    pi = 0
    for g, (t0, n) in enumerate(GROUPS):
        og, B_g = grot.next()
        S.dma("sp", og[:, :, 0:n], OGv[:, :, t0:t0 + n], [B_og], [B_g])
        for dc in range(NCH):
            wo, B_wo = worot.next()
            S.dma("pool", wo[:], wov[:, :, dc * 128:(dc + 1) * 128], (), [B_wo])
            pt, B_pt = P[pi], PB[pi]
            pi = (pi + 1) % 4
            for k in range(32):
                self.mm(pt[:, 0:n], wo[:, k, :], og[:, k, 0:n], k == 0, k == 31, [B_wo, B_g], [B_pt])
            y, B_y = yrot.next()
            self.cp("act" if dc % 2 else "dve", y[:, 0:n], pt[:, 0:n], [B_pt], [B_y])
            S.dma("sp", YT[dc][:, t0:t0 + n], y[:, 0:n], [B_y], [YTB[g]])


K.dn_proj = dn_proj
K.dn_core = dn_core
K.dn_out = dn_out


def build_program():
    k = K({})
    I = {}
    I["c"] = k.ext_in("c", [16, 128]); I["c_ctx"] = k.ext_in("c_ctx", [16, 128])
    I["x"] = k.ext_in("x", [SEQ, D]); I["ctx"] = k.ext_in("ctx", [CTX, D])
    I["w_mod"] = k.ext_in("w_mod", [DEPTH, D, 6 * D]); I["b_mod"] = k.ext_in("b_mod", [DEPTH, 6 * D])
    I["ln_g"] = k.ext_in("ln_g", [DEPTH, 2, D]); I["ln_b"] = k.ext_in("ln_b", [DEPTH, 2, D])
    I["att_w_qkv"] = k.ext_in("att_w_qkv", [2, D, 2560]); I["att_b_qkv"] = k.ext_in("att_b_qkv", [2, 1, 2560])
    I["att_sink"] = k.ext_in("att_sink", [2, 1, 32]); I["att_w_o"] = k.ext_in("att_w_o", [2, D, D]); I["att_b_o"] = k.ext_in("att_b_o", [2, 1, D])
    I["cosT"] = k.ext_in("cosT", [128, SEQ]); I["sinT"] = k.ext_in("sinT", [128, SEQ]); I["pmat"] = k.ext_in("pmat", [128, 128])
    I["dn_w_in"] = k.ext_in("dn_w_in", [2, D, 12416]); I["dn_conv_w"] = k.ext_in("dn_conv_w", [2, 5, 8192])
    I["dn_a_log"] = k.ext_in("dn_a_log", [2, 1, 64]); I["dn_dt_bias"] = k.ext_in("dn_dt_bias", [2, 1, 64])
    I["dn_norm_w"] = k.ext_in("dn_norm_w", [2, 1, 128]); I["dn_w_o"] = k.ext_in("dn_w_o", [2, 4096, D])
    I["moe_w_r"] = k.ext_in("moe_w_r", [DEPTH, D, NE]); I["moe_b_r"] = k.ext_in("moe_b_r", [DEPTH, 1, NE])
    I["moe_wgu"] = k.ext_in("moe_wgu", [DEPTH, NE, 6, 128, 16, 256]); I["moe_bgu"] = k.ext_in("moe_bgu", [DEPTH, NE, 1536])
    I["moe_wdn"] = k.ext_in("moe_wdn", [DEPTH, NE, DEXP, D]); I["moe_bdn"] = k.ext_in("moe_bdn", [DEPTH, NE, D])
    out = k.ext_out("out", [SEQ, D])
    HT = k.dram("HT", [NCH, 128, T], F32); YT = k.dram("YT", [NCH, 128, T], F32); UT = k.dram("UT", [NCH, 128, T], BF16)
    GT = k.dram("GT", [NE, T], F32)
    QTd = k.dram("QTd", [NCH, 128, T], BF16); KTd = k.dram("KTd", [4, 128, T], BF16); VAd = k.dram("VAd", [128, NT * 4 * 65], BF16)
    QTn = k.dram("QTn", [16, 128, T], BF16); KTn = k.dram("KTn", [16, 128, T], BF16); KTOK = k.dram("KTOK", [16, 128, NT, 128], BF16)
    VTOK = k.dram("VTOK", [32, 128, NT, 128], F32); ZSd = k.dram("ZSd", [32, 128, NT, 128], F32); GBd = k.dram("GBd", [2, 128, NT, 64], F32)
    OGd = k.dram("OGd", [32, 128, T], BF16)
    HTB = [Buf() for _ in GROUPS]; YTB = [Buf() for _ in GROUPS]; UTB = [Buf() for _ in GROUPS]; GTB = [Buf() for _ in GROUPS]
    B_att = Buf(); B_dn = Buf(); B_og = Buf()
    k.setup()
    k.prologue_mod(I["c"], I["c_ctx"], I["w_mod"], I["b_mod"], I["ln_g"], I["ln_b"])
    k.prologue_x(I["x"], I["ctx"], HT, HTB)
    k.ln_mod(0, HT, HTB, None, None, None, None, (0, 1), UT, UTB)
    for i in range(DEPTH):
        j = i // 2
        if i % 2 == 0:
            k.attn_proj(UT, UTB, I["att_w_qkv"][j], I["att_b_qkv"][j], I["cosT"], I["sinT"], I["pmat"], QTd, KTd, VAd, B_att)
            k.attn_core(QTd, KTd, VAd, B_att, I["att_sink"][j], I["att_w_o"][j], I["att_b_o"][j], YT, YTB)
        else:
            k.dn_proj(UT, UTB, I["dn_w_in"][j], I["dn_conv_w"][j], I["dn_a_log"][j], I["dn_dt_bias"][j], QTn, KTn, KTOK, VTOK, ZSd, GBd, B_dn)
            k.dn_core(QTn, KTn, KTOK, VTOK, ZSd, GBd, B_dn, I["dn_norm_w"][j], OGd, B_og)
            k.dn_out(OGd, B_og, I["dn_w_o"][j], YT, YTB)
        k.ln_mod(i, HT, HTB, YT, YTB, 2, 0, (3, 4), UT, UTB, router=(I["moe_w_r"][i], I["moe_b_r"][i], GT, GTB))
        k.moe(i, UT, UTB, GT, GTB, YT, YTB, I["moe_wgu"][i], I["moe_bgu"][i], I["moe_wdn"][i], I["moe_bdn"][i], skip_ctx=(i == DEPTH - 1))
        k.ln_mod(i, HT, HTB, YT, YTB, 5, 1, (0, 1), UT, UTB, l_mod=min(i + 1, DEPTH - 1))
    B_out = k.epilogue(HT, HTB, out)
    k.finish([B_out])
    return k


_PROG = None


def kernel(x, c, ctx, c_ctx, w_mod, b_mod, ln_g, ln_b, att_w_qkv, att_b_qkv, att_sink, att_w_o, att_b_o,
           dn_w_in, dn_conv_w, dn_a_log, dn_dt_bias, dn_norm_w, dn_w_o, moe_w_router, moe_b_router,
           moe_w_gu, moe_b_gu, moe_w_down, moe_b_down):
    global _PROG
    if _PROG is None:
        _PROG = build_program()
    k = _PROG
    f = lambda a: np.ascontiguousarray(np.asarray(a, dtype=np.float32))
    x, c, ctx, c_ctx = f(x), f(c), f(ctx), f(c_ctx)
    B = x.shape[0]
    cosT, sinT, pmat = rope_tables()
    wg = np.asarray(moe_w_gu, dtype=np.float32)
    g = wg[..., 0::2].reshape(DEPTH, NE, 16, 128, 6, 128)
    u = wg[..., 1::2].reshape(DEPTH, NE, 16, 128, 6, 128)
    wgu = np.ascontiguousarray(np.concatenate([g, u], axis=-1).transpose(0, 1, 4, 3, 2, 5))
    del g, u, wg
    bg = np.asarray(moe_b_gu, dtype=np.float32)
    bgu = np.ascontiguousarray(np.concatenate([bg[..., 0::2], bg[..., 1::2]], axis=-1))
    shared = {
        "c_ctx": c_ctx.reshape(16, 128), "w_mod": f(w_mod), "b_mod": f(b_mod), "ln_g": f(ln_g), "ln_b": f(ln_b),
        "att_w_qkv": f(att_w_qkv), "att_b_qkv": f(att_b_qkv).reshape(2, 1, 2560), "att_sink": f(att_sink).reshape(2, 1, 32),
        "att_w_o": f(att_w_o), "att_b_o": f(att_b_o).reshape(2, 1, D), "cosT": cosT, "sinT": sinT, "pmat": pmat,
        "dn_w_in": f(dn_w_in), "dn_conv_w": f(dn_conv_w), "dn_a_log": f(dn_a_log).reshape(2, 1, 64),
        "dn_dt_bias": f(dn_dt_bias).reshape(2, 1, 64), "dn_norm_w": f(dn_norm_w).reshape(2, 1, 128), "dn_w_o": f(dn_w_o),
        "moe_w_r": f(moe_w_router), "moe_b_r": f(moe_b_router).reshape(DEPTH, 1, NE), "moe_wgu": wgu, "moe_bgu": bgu,
        "moe_wdn": f(moe_w_down), "moe_bdn": f(moe_b_down),
    }
    in_maps = []
    for b in range(B):
        m = dict(shared)
        m["x"] = x[b]
        m["ctx"] = ctx[b]
        m["c"] = c[b].reshape(16, 128)
        in_maps.append(m)
    res = run_bass_kernel_spmd(k.nc, in_maps, core_ids=list(range(B)))
    return np.stack([np.asarray(r["out"], dtype=np.float32) for r in res.results], axis=0)
```
